# Optimizing a Trainium2 kernel written in Bass

```python
import jax, jax.numpy as jnp
from jax import lax
import numpy as np

D_MODEL = 1024
BATCH = 8
SEQ = 4096
DEPTH = 4

N_MIXERS = 4
GROUP_WIDTH = D_MODEL // N_MIXERS
HEAD_DIM = 64
N_GROUP_HEADS = GROUP_WIDTH // HEAD_DIM
D_FF = 4 * D_MODEL
N_MOD = 6
RMS_EPS = 1e-6
NEG_INF = -1e30

ROPE_THETA = 500000.0
ROPE_DIM = HEAD_DIM // 4

CONV_WIDTH = 3

CMP_BLOCK = 32
CMP_STRIDE = 16
CMP_HIDDEN = 2 * HEAD_DIM
SLC_BLOCK = 64
N_SLC = 16
N_INIT_BLOCKS = 1
N_LOCAL_BLOCKS = 2
FORCE_BONUS = 1e4
WINDOW = 512
Q_BLOCK = 128
N_BRANCH = 3

GLA_DK = HEAD_DIM // 2
GLA_DV = HEAD_DIM
GLA_RANK = 16
GLA_TAU = 16.0
GLA_CHUNK = 64

GMLP_CHUNK = 128
GMLP_DIM = GROUP_WIDTH // N_GROUP_HEADS

SPLIT_SIZES = (
    GROUP_WIDTH, GROUP_WIDTH, GROUP_WIDTH,
    GROUP_WIDTH, 2 * N_BRANCH * HEAD_DIM, N_BRANCH * N_GROUP_HEADS,
    N_GROUP_HEADS * GLA_DK, N_GROUP_HEADS * GLA_DK,
    N_GROUP_HEADS * GLA_DV, N_GROUP_HEADS * GLA_DV, GLA_RANK,
    GROUP_WIDTH, GROUP_WIDTH,
)
D_IN_PROJ = sum(SPLIT_SIZES)

kernel_name = 'hybrid_parallel_group_block'


def rms_norm(x, gain=None):
    xf = x.astype(jnp.float32)
    y = xf * lax.rsqrt(jnp.mean(xf * xf, axis=-1, keepdims=True) + RMS_EPS)
    if gain is not None:
        y = y * gain.astype(jnp.float32)
    return y.astype(x.dtype)


def layer_norm(x):
    xf = x.astype(jnp.float32)
    mu = jnp.mean(xf, axis=-1, keepdims=True)
    var = jnp.mean(jnp.square(xf - mu), axis=-1, keepdims=True)
    return ((xf - mu) * lax.rsqrt(var + RMS_EPS)).astype(x.dtype)


def split_cols(z, sizes):
    out, start = [], 0
    for s in sizes:
        out.append(z[..., start:start + s])
        start += s
    return out


def partial_rotary(x, positions):
    half = ROPE_DIM // 2
    inv_freq = ROPE_THETA ** (-jnp.arange(0, ROPE_DIM, 2, dtype=jnp.float32) / ROPE_DIM)
    ang = positions.astype(jnp.float32)[..., None] * inv_freq
    cos = jnp.cos(ang)[:, :, None, :]
    sin = jnp.sin(ang)[:, :, None, :]
    xr = x[..., :ROPE_DIM].astype(jnp.float32)
    x1, x2 = xr[..., :half], xr[..., half:]
    rot = jnp.concatenate([x1 * cos - x2 * sin, x2 * cos + x1 * sin], axis=-1).astype(x.dtype)
    return jnp.concatenate([rot, x[..., ROPE_DIM:]], axis=-1)


def masked_softmax(s, mask):
    p = jax.nn.softmax(jnp.where(mask, s, NEG_INF), axis=-1)
    return jnp.where(mask, p, 0.0)


def short_conv_mixer(h_in, b_gate, c_gate, conv_w):
    z = c_gate * h_in
    y = lax.conv_general_dilated(z, conv_w.astype(z.dtype)[:, None, :], (1,), ((CONV_WIDTH - 1, 0),),
                                 dimension_numbers=('NWC', 'WIO', 'NWC'),
                                 feature_group_count=GROUP_WIDTH)
    return b_gate * y


def cmp_to_slc_matrix(n_cmp, n_blk):
    cs = np.arange(n_cmp) * CMP_STRIDE
    ss = np.arange(n_blk) * SLC_BLOCK
    ov = np.minimum(cs[:, None] + CMP_BLOCK, ss[None, :] + SLC_BLOCK) - np.maximum(cs[:, None], ss[None, :])
    return jnp.asarray(np.clip(ov, 0, None) / CMP_STRIDE, dtype=jnp.float32)


def nsa_mixer(q_in, kv_in, gate_in, positions, cmp_pos, cmp_w1, cmp_w2):
    B, S, _ = q_in.shape
    H, HD = N_GROUP_HEADS, HEAD_DIM
    scale = HD ** -0.5
    q = q_in.reshape(B, S, H, HD)
    k_cmp, v_cmp, k_slc, v_slc, k_win, v_win = jnp.split(kv_in, 2 * N_BRANCH, axis=-1)

    n_cmp = (S - CMP_BLOCK) // CMP_STRIDE + 1
    blk = np.arange(n_cmp)[:, None] * CMP_STRIDE + np.arange(CMP_BLOCK)[None, :]

    def compress(t, j):
        z = (t[:, blk] + cmp_pos[j]).reshape(B, n_cmp, CMP_BLOCK * HD)
        return jax.nn.silu(z @ cmp_w1[j]) @ cmp_w2[j]

    kc, vc = compress(k_cmp, 0), compress(v_cmp, 1)
    t_idx = jnp.arange(S)
    cmp_end = jnp.arange(n_cmp) * CMP_STRIDE + CMP_BLOCK - 1
    mask_c = cmp_end[None, :] <= t_idx[:, None]
    s_c = jnp.einsum('bshd,bnd->bhsn', q, kc).astype(jnp.float32) * scale
    p_c = masked_softmax(s_c, mask_c)
    o_cmp = jnp.einsum('bhsn,bnd->bshd', p_c.astype(vc.dtype), vc)

    n_blk = S // SLC_BLOCK
    p_slc = jnp.einsum('bhsn,nj->bsj', p_c, cmp_to_slc_matrix(n_cmp, n_blk))
    cur = (t_idx // SLC_BLOCK)[:, None]
    j = jnp.arange(n_blk)[None, :]
    forced = (j < N_INIT_BLOCKS) | ((cur - j >= 0) & (cur - j < N_LOCAL_BLOCKS))
    score = jnp.where(j <= cur, p_slc + jnp.where(forced, FORCE_BONUS, 0.0), NEG_INF)
    n_top = min(N_SLC, n_blk)
    _, sel_idx = lax.top_k(score, n_top)

    q_r = partial_rotary(q, positions)
    rope_k = lambda t: partial_rotary(t[:, :, None, :], positions)[:, :, 0, :]
    k_s_blk = rope_k(k_slc).reshape(B, n_blk, SLC_BLOCK, HD)
    v_s_blk = v_slc.reshape(B, n_blk, SLC_BLOCK, HD)
    pad = ((0, 0), (WINDOW, 0), (0, 0))
    k_w_pad = jnp.pad(rope_k(k_win), pad)
    v_w_pad = jnp.pad(v_win, pad)

    n_qb = S // Q_BLOCK
    q_blocks = q_r.reshape(B, n_qb, Q_BLOCK, H, HD).transpose(1, 0, 3, 2, 4)
    idx_blocks = sel_idx.reshape(B, n_qb, Q_BLOCK, n_top).transpose(1, 0, 2, 3)
    take = jax.vmap(lambda blocks, idx: blocks[idx])

    def block_fn(args):
        qb, q_b, idx_b = args
        tq = qb * Q_BLOCK + jnp.arange(Q_BLOCK)
        k_sel = take(k_s_blk, idx_b).reshape(B, Q_BLOCK, n_top * SLC_BLOCK, HD)
        v_sel = take(v_s_blk, idx_b).reshape(B, Q_BLOCK, n_top * SLC_BLOCK, HD)
        kpos = (idx_b[..., None] * SLC_BLOCK + jnp.arange(SLC_BLOCK)).reshape(B, Q_BLOCK, n_top * SLC_BLOCK)
        s_s = jnp.einsum('bhqd,bqkd->bhqk', q_b, k_sel).astype(jnp.float32) * scale
        p_s = masked_softmax(s_s, (kpos <= tq[None, :, None])[:, None])
        o_s = jnp.einsum('bhqk,bqkd->bhqd', p_s.astype(v_sel.dtype), v_sel)
        start = qb * Q_BLOCK
        k_wb = lax.dynamic_slice_in_dim(k_w_pad, start, Q_BLOCK + WINDOW, axis=1)
        v_wb = lax.dynamic_slice_in_dim(v_w_pad, start, Q_BLOCK + WINDOW, axis=1)
        kpos_w = start - WINDOW + jnp.arange(Q_BLOCK + WINDOW)
        diff = tq[:, None] - kpos_w[None, :]
        m_w = (diff >= 0) & (diff < WINDOW) & (kpos_w[None, :] >= 0)
        s_w = jnp.einsum('bhqd,bkd->bhqk', q_b, k_wb).astype(jnp.float32) * scale
        p_w = masked_softmax(s_w, m_w)
        o_w = jnp.einsum('bhqk,bkd->bhqd', p_w.astype(v_wb.dtype), v_wb)
        return o_s, o_w

    o_sel, o_win = lax.map(block_fn, (jnp.arange(n_qb), q_blocks, idx_blocks))
    to_bshd = lambda o: o.transpose(1, 0, 3, 2, 4).reshape(B, S, H, HD)
    g = jax.nn.sigmoid(gate_in.astype(jnp.float32)).astype(q.dtype).reshape(B, S, H, N_BRANCH)
    o = g[..., 0:1] * o_cmp + g[..., 1:2] * to_bshd(o_sel) + g[..., 2:3] * to_bshd(o_win)
    return o.reshape(B, S, GROUP_WIDTH)


def gla_mixer(q_in, k_in, v_in, g_in, lr_in, gate_w2, gate_b):
    B, S, _ = q_in.shape
    H, C = N_GROUP_HEADS, GLA_CHUNK
    n_c = S // C
    log_a = jax.nn.log_sigmoid((lr_in @ gate_w2 + gate_b).astype(jnp.float32)) / GLA_TAU

    def chunk(t, d):
        return t.reshape(B, n_c, C, H, d).transpose(0, 3, 1, 2, 4).astype(jnp.float32)

    q = chunk(q_in, GLA_DK) * GLA_DK ** -0.5
    k = chunk(k_in, GLA_DK)
    v = chunk(v_in, GLA_DV)
    b = jnp.cumsum(chunk(log_a, GLA_DK), axis=3)
    b_last = b[:, :, :, -1:, :]
    b_mid = b[:, :, :, C // 2:C // 2 + 1, :]
    causal = jnp.tril(jnp.ones((C, C), dtype=bool))
    att = jnp.einsum('bhntd,bhnsd->bhnts', q * jnp.exp(b - b_mid), k * jnp.exp(b_mid - b))
    o_intra = jnp.einsum('bhnts,bhnsv->bhntv', jnp.where(causal, att, 0.0), v)
    kv = jnp.einsum('bhnsd,bhnsv->bhndv', k * jnp.exp(b_last - b), v)
    decay = jnp.exp(b_last[:, :, :, 0, :])

    def step(state, inp):
        dec, kv_n = inp
        return dec[..., None] * state + kv_n, state

    init = jnp.zeros((B, H, GLA_DK, GLA_DV), jnp.float32)
    _, s_prev = lax.scan(step, init, (jnp.moveaxis(decay, 2, 0), jnp.moveaxis(kv, 2, 0)))
    o_inter = jnp.einsum('bhntd,nbhdv->bhntv', q * jnp.exp(b), s_prev)
    o = (o_intra + o_inter).transpose(0, 2, 3, 1, 4).reshape(B, S, H, GLA_DV)
    g = jax.nn.silu(g_in.astype(jnp.float32)).reshape(B, S, H, GLA_DV)
    return (g * rms_norm(o)).reshape(B, S, GROUP_WIDTH).astype(q_in.dtype)


def gmlp_mixer(u_in, v_in, w_s, b_s):
    B, S, _ = u_in.shape
    G, T = N_GROUP_HEADS, GMLP_CHUNK
    n_ch = S // T
    v = layer_norm(v_in.reshape(B, S, G, GMLP_DIM)).reshape(B, n_ch, T, G, GMLP_DIM)
    w = jnp.where(jnp.tril(jnp.ones((T, T), dtype=bool)), w_s, 0.0)
    mixed = jnp.einsum('gts,bnsgc->bntgc', w, v) + b_s.T[:, :, None]
    return (u_in.reshape(B, n_ch, T, G, GMLP_DIM) * mixed).reshape(B, S, GROUP_WIDTH)


def mixer_block(h, positions, w_in, conv_w, cmp_pos, cmp_w1, cmp_w2, gla_gate_w2, gla_gate_b,
                gmlp_ws, gmlp_b, grp_gain, w_o):
    B, S, _ = h.shape
    (a_h, a_b, a_c, n_q, n_kv, n_g, l_q, l_k, l_v, l_g, l_lr, m_u, m_v) = split_cols(h @ w_in, SPLIT_SIZES)
    outs = jnp.stack([
        short_conv_mixer(a_h, a_b, a_c, conv_w),
        nsa_mixer(n_q, n_kv, n_g, positions, cmp_pos, cmp_w1, cmp_w2),
        gla_mixer(l_q, l_k, l_v, l_g, l_lr, gla_gate_w2, gla_gate_b),
        gmlp_mixer(m_u, m_v, gmlp_ws, gmlp_b),
    ], axis=2)
    outs = rms_norm(outs, grp_gain.reshape(N_MIXERS, GROUP_WIDTH))
    return outs.reshape(B, S, D_MODEL) @ w_o


def setup_inputs(seed: int = 0) -> dict:
    key = jax.random.key(seed)
    ks = jax.random.split(key, 20)
    nrm = lambda k, shape, s: jax.random.normal(k, shape, jnp.float32) * s
    positions = (jax.random.randint(ks[2], (BATCH, 1), 0, SEQ, dtype=jnp.int32)
                 + jnp.arange(SEQ, dtype=jnp.int32)[None, :])
    return {
        'x': nrm(ks[0], (BATCH, SEQ, D_MODEL), 1.0),
        'c': nrm(ks[1], (BATCH, D_MODEL), 1.0),
        'positions': positions,
        'w_in': nrm(ks[3], (DEPTH, D_MODEL, D_IN_PROJ), D_MODEL ** -0.5),
        'conv_w': nrm(ks[4], (DEPTH, CONV_WIDTH, GROUP_WIDTH), CONV_WIDTH ** -0.5),
        'cmp_pos': nrm(ks[5], (DEPTH, 2, CMP_BLOCK, HEAD_DIM), 0.1),
        'cmp_w1': nrm(ks[6], (DEPTH, 2, CMP_BLOCK * HEAD_DIM, CMP_HIDDEN), (CMP_BLOCK * HEAD_DIM) ** -0.5),
        'cmp_w2': nrm(ks[7], (DEPTH, 2, CMP_HIDDEN, HEAD_DIM), CMP_HIDDEN ** -0.5),
        'gla_gate_w2': nrm(ks[8], (DEPTH, GLA_RANK, N_GROUP_HEADS * GLA_DK), GLA_RANK ** -0.5),
        'gla_gate_b': nrm(ks[9], (DEPTH, N_GROUP_HEADS * GLA_DK), 0.1),
        'gmlp_ws': nrm(ks[10], (DEPTH, N_GROUP_HEADS, GMLP_CHUNK, GMLP_CHUNK), 0.5 * GMLP_CHUNK ** -0.5),
        'gmlp_b': 1.0 + nrm(ks[11], (DEPTH, N_GROUP_HEADS, GMLP_CHUNK), 0.1),
        'grp_gain': 1.0 + nrm(ks[12], (DEPTH, D_MODEL), 0.1),
        'w_o': nrm(ks[13], (DEPTH, D_MODEL, D_MODEL), D_MODEL ** -0.5),
        'norm_g': 1.0 + nrm(ks[14], (DEPTH, 4, D_MODEL), 0.1),
        'w_mod': nrm(ks[15], (DEPTH, D_MODEL, N_MOD * D_MODEL), D_MODEL ** -0.5),
        'b_mod': nrm(ks[16], (DEPTH, N_MOD * D_MODEL), 0.02),
        'w_up': nrm(ks[17], (DEPTH, D_MODEL, D_FF), D_MODEL ** -0.5),
        'w_down': nrm(ks[18], (DEPTH, D_FF, D_MODEL), D_FF ** -0.5),
    }


def reference(x, c, positions, w_in, conv_w, cmp_pos, cmp_w1, cmp_w2, gla_gate_w2, gla_gate_b,
              gmlp_ws, gmlp_b, grp_gain, w_o, norm_g, w_mod, b_mod, w_up, w_down):
    cond = jax.nn.silu(c)
    for l in range(DEPTH):
        mod = cond @ w_mod[l] + b_mod[l]
        sh1, sc1, gt1, sh2, sc2, gt2 = [m[:, None, :] for m in jnp.split(mod, N_MOD, axis=-1)]
        h = rms_norm(x, norm_g[l, 0]) * (1.0 + sc1) + sh1
        y = mixer_block(h, positions, w_in[l], conv_w[l], cmp_pos[l], cmp_w1[l], cmp_w2[l],
                        gla_gate_w2[l], gla_gate_b[l], gmlp_ws[l], gmlp_b[l], grp_gain[l], w_o[l])
        x = x + gt1 * rms_norm(y, norm_g[l, 1])
        h = rms_norm(x, norm_g[l, 2]) * (1.0 + sc2) + sh2
        y = jnp.square(jax.nn.relu(h @ w_up[l])) @ w_down[l]
        x = x + gt2 * rms_norm(y, norm_g[l, 3])
    return x
```

```python
from contextlib import ExitStack
import numpy as np
import concourse.bass as bass
import concourse.mybir as mybir
from concourse.bass_utils import run_bass_kernel_spmd

F32 = mybir.dt.float32
BF16 = mybir.dt.bfloat16
I32 = mybir.dt.int32
ALU = mybir.AluOpType
AF = mybir.ActivationFunctionType
AX = mybir.AxisListType

D = 1024
S_ = 4096
DEPTH = 4
DFF = 4096
NT = S_ // 128
NB = S_ // 512
EPS = 1e-6
NEG = -30000.0
NCOL = 2972
FM_W = [128] * 15 + [16]
C_AH, C_AB, C_AC, C_Q, C_KVC, C_KS, C_KW, C_LQ, C_LK, C_LR = 0, 2, 4, 6, 10, 11, 12, 13, 14, 15
TM0 = 1936


class Res:
    __slots__ = ("w", "rs", "excl")

    def __init__(self, excl=False):
        self.w = None
        self.rs = {}
        self.excl = excl


class Sched:
    CE = ("pe", "act", "dve", "pool")

    def __init__(self, nc, stack, n_dma_sems=(("sp", 24), ("pool", 16), ("act", 4))):
        self.nc = nc
        self.prog = {k: [] for k in ("pe", "act", "dve", "pool", "sp")}
        self.semobj = {}
        self.cnt = {}
        self.ckey = {}
        self.keyeng = {}
        self.stack = stack
        self.epoch = 0
        for k in self.CE:
            self.semobj[k] = stack.enter_context(nc.semaphore("s_" + k))
            self.cnt[k] = 0
            self.ckey[k] = k
            self.keyeng[k] = k
        self.dq, self.dval, self.dnext = {}, {}, {}
        for q, n in n_dma_sems:
            keys = []
            for i in range(n):
                key = f"d_{q}{i}"
                self.semobj[key] = stack.enter_context(nc.semaphore(key))
                self.dval[key] = 0
                keys.append(key)
            self.dq[q] = keys
            self.dnext[q] = 0
        self.seen = {k: {} for k in self.prog}
        self.n_ops = 0

    def _wait(self, e, dep):
        key, val = dep
        if self.seen[e].get(key, 0) >= val:
            return
        self.seen[e][key] = val
        s = self.semobj[key]
        self.prog[e].append(lambda eng, s=s, v=val: eng.wait_ge(s, v))

    def _deps(self, e, reads, writes, is_dma):
        for r in reads:
            if r.w is not None:
                self._wait(e, r.w)
            if r.excl:
                for k, v in r.rs.items():
                    if self.keyeng.get(k) != e:
                        self._wait(e, (k, v))
        for w in writes:
            if w.w is not None and (is_dma or self.keyeng.get(w.w[0]) != e):
                self._wait(e, w.w)
            for k, v in w.rs.items():
                if is_dma or self.keyeng.get(k) != e:
                    self._wait(e, (k, v))

    def _commit(self, rec, reads, writes):
        for r in reads:
            if r.rs.get(rec[0], 0) < rec[1]:
                r.rs[rec[0]] = rec[1]
        for w in writes:
            w.w = rec
            w.rs = {}

    def op(self, e, fn, reads=(), writes=()):
        self._deps(e, reads, writes, False)
        self.cnt[e] += 1
        rec = (self.ckey[e], self.cnt[e])
        s = self.semobj[self.ckey[e]]
        self.prog[e].append(lambda eng, f=fn, s=s: f(eng).then_inc(s, 1))
        self._commit(rec, reads, writes)
        self.n_ops += 1
        return rec

    def dma(self, q, out, in_, reads=(), writes=(), **kw):
        self._deps(q, reads, writes, True)
        keys = self.dq[q]
        key = keys[self.dnext[q] % len(keys)]
        self.dnext[q] += 1
        prev = self.dval[key]
        if prev > 0:
            self._wait(q, (key, prev))
        tgt = prev + 16
        self.dval[key] = tgt
        s = self.semobj[key]
        self.prog[q].append(
            lambda eng, o=out, i=in_, s=s, kw=kw: eng.dma_start(out=o, in_=i, **kw).then_inc(s, 16))
        rec = (key, tgt)
        self._commit(rec, reads, writes)
        self.n_ops += 1
        return rec

    def barrier(self):
        for e in self.prog:
            for k in self.CE:
                if self.cnt[k] > 0:
                    self._wait(e, (self.ckey[k], self.cnt[k]))
            for key, v in self.dval.items():
                if v > 0:
                    self._wait(e, (key, v))

    def new_epoch(self):
        self.barrier()
        self.epoch += 1
        for k in self.CE:
            key = f"{k}@{self.epoch}"
            self.semobj[key] = self.stack.enter_context(self.nc.semaphore(f"s_{k}_{self.epoch}"))
            self.ckey[k] = key
            self.keyeng[key] = k
            self.cnt[k] = 0

    def emit(self):
        nc = self.nc
        with nc.Block() as block:
            @block.tensor
            def _(eng):
                for f in self.prog["pe"]:
                    f(eng)

            @block.scalar
            def _(eng):
                for f in self.prog["act"]:
                    f(eng)

            @block.vector
            def _(eng):
                for f in self.prog["dve"]:
                    f(eng)

            @block.gpsimd
            def _(eng):
                for f in self.prog["pool"]:
                    f(eng)

            @block.sync
            def _(eng):
                for f in self.prog["sp"]:
                    f(eng)


class Arena:
    def __init__(self, tile, nbytes, elsize):
        self.tile, self.nbytes, self.elsize = tile, nbytes, elsize
        self.off = 0

    def reset(self, off=0):
        self.off = off

    def get(self, shape, dt):
        es = 4 if dt in (F32, I32) else 2
        n = int(np.prod(shape[1:]))
        nb = (n * es + 31) // 32 * 32
        assert self.off + nb <= self.nbytes, f"arena overflow {self.off}+{nb}>{self.nbytes}"
        a, b = self.off // self.elsize, (self.off + nb) // self.elsize
        self.off += nb
        v = self.tile[:, a:b]
        if dt != self.tile.dtype:
            v = v.bitcast(dt)
        v = v[:, 0:n]
        if len(shape) == 3:
            v = v.rearrange("p (a b) -> p a b", a=shape[1])
        elif len(shape) == 4:
            v = v.rearrange("p (a b c) -> p a b c", a=shape[1], b=shape[2])
        if shape[0] < 128:
            v = v[0:shape[0]]
        return v


def _consts():
    c = {}
    kp = np.arange(128)[:, None]
    tq = np.arange(512)[None, :]
    masks = np.zeros((128, 12, 512), np.float32)
    for i in range(4):
        masks[:, i, :] = np.where(kp + 128 * i <= tq, 0.0, NEG)
    for n, i in enumerate(range(-4, 0)):
        masks[:, 4 + n, :] = np.where(tq < kp + 128 * i + 512, 0.0, NEG)
    for pat in range(4):
        masks[:, 8 + pat, :] = np.where(16 * kp + 15 - 512 * pat <= tq, 0.0, NEG)
    c["masks"] = masks
    c["ident"] = np.eye(128, dtype=np.float32)
    pm = np.zeros((128, 128), np.float32)
    for m in range(8):
        pm[m + 8, m] = 1.0
        pm[m, m + 8] = 1.0
    c["pm"] = pm
    inv_freq = (500000.0 ** (-np.arange(0, 16, 2, dtype=np.float32) / 16)).astype(np.float32)
    rc = np.zeros((128, 2), np.float32)
    rc[:16, 0] = np.concatenate([inv_freq, inv_freq])
    rc[:8, 1] = -1.0
    rc[8:16, 1] = 1.0
    c["ropec"] = rc
    ew = np.zeros((64, 4096), np.float32)
    for jb in range(64):
        ew[jb, jb * 64:(jb + 1) * 64] = 1.0
    c["ew"] = ew
    mo = np.zeros((256, 64), np.float32)
    for s in range(1, 256):
        n = s - 1
        cs = n * 16
        for j in range(64):
            ov = min(cs + 32, j * 64 + 64) - max(cs, j * 64)
            mo[s, j] = max(ov, 0) / 16.0
    c["mo"] = mo.reshape(2, 128, 64).transpose(1, 0, 2).copy()
    wd = np.zeros((128, 127), np.float32)
    cl = (np.arange(128) // 64)[:, None]
    m = (np.arange(127) - 62)[None, :]
    wd = np.where(m > cl, -1e30, np.where(m == cl, 2e4, np.where(m == cl - 1, 1e4, 0.0))).astype(np.float32)
    c["wd"] = wd
    s = np.arange(128)[:, None]
    t = np.arange(128)[None, :]
    same = (s // 64) == (t // 64)
    c["tinc"] = np.where(same & (s <= t), -1.0 / 16, 0.0).astype(np.float32)
    c["trev"] = np.where(same & (s > t), -1.0 / 16, 0.0).astype(np.float32)
    c["glam"] = np.where(same & (s <= t), 1.0, 0.0).astype(np.float32)
    c["triu"] = np.where(s <= t, 1.0, 0.0).astype(np.float32)
    p = np.arange(128)[:, None]
    c["bds"] = ((p // 32) == (np.arange(256)[None, :] // 64)).astype(np.float32)
    c["bdq"] = np.repeat(((p // 32) == np.arange(4)[None, :]).astype(np.float32)[:, :, None], 128, axis=2).copy()
    c["ones"] = np.ones((128, 128), np.float32)
    return c


def _perm():
    r = lambda a, b: list(range(a, b))
    cols = []
    cols += r(0, 256) + r(256, 512) + r(512, 768)
    for h in range(4):
        cols += r(768 + 64 * h, 832 + 64 * h) * 2
    cols += r(1024, 1152)
    cols += r(1152, 1216) * 2
    cols += r(1280, 1344) * 2
    cols += r(1420, 1548)
    cols += r(1548, 1676)
    cols += r(2188, 2204)
    assert len(cols) == TM0
    cols += r(1216, 1280) + r(1344, 1408) + r(1408, 1420)
    cols += r(1548, 1676) + r(1676, 1932)
    cols += r(1932, 2188) + r(2204, 2460)
    cols += r(2460, 2716)
    return np.array(cols, np.int64)


NCOL = TM0 + 140 + 384 + 512 + 256
FM_OFF = [128 * i for i in range(15)] + [1920]
T_OFF = [TM0, TM0 + 140, TM0 + 524, TM0 + 1036]
T_W = [140, 384, 512, 256]


def build(n_layers=DEPTH, taps=(), stop=None):
    nc = bass.Bass("TRN2", target_bir_lowering=False)
    CST = _consts()

    def din(name, shape, dt=F32):
        return nc.dram_tensor(name, list(shape), dt, kind="ExternalInput").ap()

    x_in = din("x", [S_, D])
    ccol_in = din("ccol", [128, 8])
    pos_in = din("pos", [1, S_], I32)
    w_in_d = din("w_in", [DEPTH, D, NCOL])
    w_o_d = din("w_o", [DEPTH, D, D])
    w_up_d = din("w_up", [DEPTH, D, DFF])
    w_dn_d = din("w_down", [DEPTH, DFF, D])
    w_mod_d = din("w_mod", [DEPTH, D, 6 * D])
    convw_d = din("convw", [DEPTH, 128, 2, 3])
    w1_d = din("cmp_w1", [DEPTH, 2, 2048, 128])
    w2_d = din("cmp_w2", [DEPTH, 2, 128, 64])
    posT_d = din("cmp_posT", [DEPTH, 128, 32])
    gw2b_d = din("gw2b", [DEPTH, 17, 128])
    gws_d = din("gmlp_wsT", [DEPTH, 128, 4, 128])
    gb_d = din("gmlp_bT", [DEPTH, 128, 4])
    gain_d = din("gaincol", [DEPTH, 128, 8])
    ngc_d = din("normg_col", [DEPTH, 128, 4, 8])
    ngr_d = din("normg_row", [DEPTH, 4, D])
    bmc_d = din("bmod_col", [DEPTH, 128, 48])
    bmr_d = din("bmod_row", [DEPTH, 6 * D])
    cst_d = {k: din("c_" + k, v.shape) for k, v in CST.items()}
    out_d = nc.dram_tensor("out", [S_, D], F32, kind="ExternalOutput").ap()
    xa_d = nc.dram_tensor("xa", [S_, D], F32).ap()
    ropeC_d = nc.dram_tensor("ropeC", [128, S_], F32).ap()
    ropeS_d = nc.dram_tensor("ropeS", [128, S_], F32).ap()
    tap_d = {}
    for name, shape in taps:
        tap_d[name] = nc.dram_tensor("tap_" + name, list(shape), F32, kind="ExternalOutput").ap()

    xview = lambda ap: ap.rearrange("(n p) d -> p n d", p=128)

    with ExitStack() as st:
        S = Sched(nc, st)
        build.last_sched = S
        sbt = lambda name, shape, dt: st.enter_context(nc.sbuf_tensor("sb_" + name, shape, dt))
        WB_t = sbt("WB", [128, 65536], BF16)
        PA_t = sbt("PA", [128, 14848], F32)
        WB = Arena(WB_t, 131072, 2)
        PA = Arena(PA_t, 59392, 4)

        def BUF(shape, dt):
            es = 4 if dt in (F32, I32) else 2
            nb = (int(np.prod(shape[1:])) * es + 31) // 32 * 32
            if WB.off + nb <= WB.nbytes:
                return WB.get(shape, dt)
            return PA.get(shape, dt)
        ps = [st.enter_context(nc.psum_tensor(f"ps{i}", [128, 512], F32)) for i in range(8)]
        psR = [Res(excl=True) for _ in range(8)]

        class Pool:
            def __init__(self, ids):
                self.ids, self.i = ids, 0

            def get(self):
                b = self.ids[self.i % len(self.ids)]
                self.i += 1
                return ps[b], psR[b]
        pMM = Pool([0, 1, 2])
        pACC = Pool([3, 4])
        pAUX = Pool([5, 6, 7])

        def mm(out, lhsT, rhs, start, stop, r, w, **kw):
            S.op("pe", lambda e: e.matmul(out, lhsT, rhs, start=start, stop=stop, **kw), r, w)

        def tr(out, in_, ident, r, w):
            S.op("pe", lambda e: e.transpose(out, in_, ident), r, w)

        def act(out, in_, func, r, w, **kw):
            S.op("act", lambda e: e.activation(out, in_, func, **kw), r, w)

        def ts(eng, out, in0, s1, s2, op0, op1, r, w):
            if op1 is None:
                S.op(eng, lambda e: e.tensor_scalar(out, in0, s1, None, op0), r, w)
            else:
                S.op(eng, lambda e: e.tensor_scalar(out, in0, s1, s2, op0, op1), r, w)

        def stt(eng, out, in0, sc, in1, op0, op1, r, w):
            S.op(eng, lambda e: e.scalar_tensor_tensor(out, in0, sc, in1, op0, op1), r, w)

        def tt(eng, out, in0, in1, op, r, w):
            S.op(eng, lambda e: e.tensor_tensor(out, in0, in1, op), r, w)

        def cp(eng, out, in_, r, w):
            if eng == "act":
                S.op("act", lambda e: e.copy(out, in_), r, w)
            else:
                S.op(eng, lambda e: e.tensor_copy(out, in_), r, w)

        def red(out, in_, r, w, op=ALU.add):
            S.op("dve", lambda e: e.tensor_reduce(out, in_, AX.X, op), r, w)

        def recip(out, in_, r, w):
            S.op("dve", lambda e: e.reciprocal(out, in_), r, w)

        def memset(eng, ap, v, w):
            S.op(eng, lambda e: e.memset(ap, v), (), w)

        def rsqrt(out, in_, scale, r, w):
            act(out, in_, AF.Sqrt, r, w, bias=EPS, scale=scale)
            recip(out, out, w, w)

        def tap(name, src_ap, res, dst_slice=None):
            if name in tap_d:
                dst = tap_d[name] if dst_slice is None else dst_slice(tap_d[name])
                S.dma("pool", dst, src_ap, reads=res)

        masks = sbt("masks", [128, 12, 512], BF16); Rc = Res()
        ident = sbt("ident", [128, 128], BF16)
        pm = sbt("pm", [128, 128], BF16)
        ones = sbt("ones", [128, 128], BF16)
        tinc = sbt("tinc", [128, 128], BF16)
        trev = sbt("trev", [128, 128], BF16)
        glam = sbt("glam", [128, 128], BF16)
        triu = sbt("triu", [128, 128], F32)
        bds = sbt("bds", [128, 256], BF16)
        bdq = sbt("bdq", [128, 4, 128], BF16)
        wd = sbt("wd", [128, 127], F32)
        ropec = sbt("ropec", [128, 2], F32)
        mo = sbt("mo", [128, 2, 64], BF16)
        for t_, k in ((masks, "masks"), (ident, "ident"), (pm, "pm"), (ones, "ones"), (tinc, "tinc"),
                      (trev, "trev"), (glam, "glam"), (bds, "bds"), (bdq, "bdq"), (mo, "mo")):
            S.dma("pool", t_[:], cst_d[k], writes=[Rc])
        for t_, k in ((triu, "triu"), (wd, "wd"), (ropec, "ropec")):
            S.dma("sp", t_[:], cst_d[k], writes=[Rc])
        condT = sbt("condT", [128, 8], BF16)
        cond_bc = sbt("cond_bc", [128, 8, 128], BF16)
        modcol = sbt("modcol", [128, 48], F32); Rmod = Res()
        A1 = sbt("A1", [128, 8], F32)
        A2 = sbt("A2", [128, 8], F32)
        ngc = sbt("ngc", [128, 4, 8], F32)
        bmc = sbt("bmc", [128, 48], F32)
        gainc = sbt("gainc", [128, 8], F32)
        convw = sbt("convw", [128, 2, 3], F32)
        gbT = sbt("gbT", [128, 4], F32)
        c1col = sbt("c1col", [128, 2], F32)
        small = sbt("small", [128, 64], F32); Rsm = Res()
        Rw = Res()

        PA.reset()
        posi = PA.get([128, 512], I32)
        ang = PA.get([128, 512], F32)
        kf = PA.get([128, 512], F32)
        tS = PA.get([128, 512], F32)
        tC = PA.get([128, 512], F32)
        Rs = Res()
        TWO_PI = float(2 * np.pi)
        for pc in range(8):
            cs = slice(pc * 512, (pc + 1) * 512)
            S.dma("sp", posi, pos_in[:, cs].partition_broadcast(128), writes=[Rs])
            cp("dve", ang, posi, [Rs], [Rs])
            ts("dve", ang, ang, ropec[:, 0:1], None, ALU.mult, None, [Rs, Rc], [Rs])
            ts("dve", kf, ang, 1.0 / TWO_PI, None, ALU.mult, None, [Rs], [Rs])
            cp("dve", posi, kf, [Rs], [Rs])
            cp("dve", kf, posi, [Rs], [Rs])
            stt("dve", ang, kf, -TWO_PI, ang, ALU.mult, ALU.add, [Rs], [Rs])
            ts("dve", ang, ang, 3.1415925, -3.1415925, ALU.min, ALU.max, [Rs], [Rs])
            act(tS, ang, AF.Sin, [Rs], [Rs])
            ts("dve", tS, tS, ropec[:, 1:2], None, ALU.mult, None, [Rs, Rc], [Rs])
            stt("dve", kf, ang, -1.0, ang, ALU.mult, ALU.max, [Rs], [Rs])
            act(tC, kf, AF.Sin, [Rs], [Rs], scale=-1.0, bias=float(np.pi / 2))
            S.dma("sp", ropeS_d[:, cs], tS, reads=[Rs])
            S.dma("sp", ropeC_d[:, cs], tC, reads=[Rs])
        ccol = PA.get([128, 8], F32)
        S.dma("sp", ccol, ccol_in, writes=[Rs])
        act(condT[:], ccol, AF.Silu, [Rs], [Rmod])
        cp("dve", cond_bc[:], condT[:].unsqueeze(2).to_broadcast([128, 8, 128]), [Rmod], [Rmod])
        Rrope = Res()
        S.barrier()

        def done():
            S.barrier()
            S.emit()
            return nc
        if stop == "setup":
            return done()

        xsrc = x_in
        for l in range(n_layers):
            if l > 0:
                S.new_epoch()
            xmid = xa_d
            xdst = out_d
            PA.reset()
            WB.reset()
            G1 = PA.get([128, 1024], F32)
            G2 = PA.get([128, 1024], F32)
            RG = Res()
            PA_base = PA.off
            wm = [WB.get([128, 8, 512], BF16) for _ in range(2)]
            Rwm = [Res(), Res()]
            brow = WB.get([128, 1024], F32)
            grow = WB.get([128, 1024], F32)
            Rrow = Res()
            for t_, src in ((ngc, ngc_d[l]), (bmc, bmc_d[l]), (gainc, gain_d[l]), (convw, convw_d[l]),
                            (gbT, gb_d[l])):
                S.dma("sp", t_[:], src, writes=[Rw])
            pcol, Rpcol = pAUX.get()
            wmv = w_mod_d[l].rearrange("(k p) n -> p k n", p=128)
            for pc in range(12):
                b = pc % 2
                S.dma("pool", wm[b], wmv[:, :, pc * 512:(pc + 1) * 512], writes=[Rwm[b]])
                for jj in range(4):
                    col = pc * 4 + jj
                    for k in range(8):
                        mm(pcol[:, col:col + 1], wm[b][:, k, jj * 128:(jj + 1) * 128], condT[:, k:k + 1],
                           k == 0, k == 7, [Rwm[b], Rmod], [Rpcol])
                if pc in (4, 5, 10, 11):
                    Gt = G1 if pc < 6 else G2
                    half = pc % 2
                    hs = slice(half * 512, (half + 1) * 512)
                    gi = 1 if pc < 6 else 3
                    pg, Rpg = pMM.get()
                    for k in range(8):
                        mm(pg[:], cond_bc[:, k, :], wm[b][:, k, :], k == 0, k == 7, [Rwm[b], Rmod], [Rpg])
                    if half == 0:
                        S.dma("sp", brow, bmr_d[l:l + 1, (pc // 2) * 1024:(pc // 2) * 1024 + 1024].partition_broadcast(128),
                              writes=[Rrow])
                        S.dma("sp", grow, ngr_d[l, gi:gi + 1, :].partition_broadcast(128), writes=[Rrow])
                    tt("dve", Gt[:, hs], pg[:], brow[:, hs], ALU.add, [Rpg, Rrow], [RG])
                    tt("dve", Gt[:, hs], Gt[:, hs], grow[:, hs], ALU.mult, [RG, Rrow], [RG])
            tt("dve", modcol[:], pcol[:, 0:48], bmc[:], ALU.add, [Rpcol, Rw], [Rmod])
            stt("dve", A1[:], modcol[:, 8:16], 1.0, ngc[:, 0, :], ALU.add, ALU.mult, [Rmod, Rw], [Rmod])
            stt("dve", A2[:], modcol[:, 32:40], 1.0, ngc[:, 2, :], ALU.add, ALU.mult, [Rmod, Rw], [Rmod])
            sh1 = modcol[:, 0:8]
            sh2 = modcol[:, 24:32]
            S.barrier()
            if stop == "mods":
                return done()

            WB.reset()
            PA.reset(PA_base)
            w_in = WB.get([128, 8, NCOL], BF16)
            w_o = WB.get([128, 8, D], BF16)
            RW1i = [Res() for _ in range(8)]
            RW1o = [Res() for _ in range(4)]
            wiv = w_in_d[l].rearrange("(k p) n -> p k n", p=128)
            for k in range(8):
                S.dma("pool", w_in[:, k, :], wiv[:, k, :], writes=[RW1i[k]])
            wov = w_o_d[l].rearrange("(k p) n -> p k n", p=128)
            for k in range(0, 8, 2):
                S.dma("pool", w_o[:, k:k + 2, :], wov[:, k:k + 2, :], writes=[RW1o[k // 2]])
            ksA = PA.get([128, 4096], BF16)
            kwT = PA.get([128, 1024], BF16)
            vs1 = PA.get([128, 32, 65], BF16)
            vw1 = PA.get([128, 8, 65], BF16)
            kvc = PA.get([128, 528], BF16)
            kcT = PA.get([128, 256], BF16)
            hTv = PA.get([128, 256], BF16)
            vcM = PA.get([128, 2, 128], BF16)
            W1 = PA.get([128, 32, 128], BF16)
            w2k = PA.get([128, 64], BF16)
            w2v = PA.get([128, 64], BF16)
            posT = PA.get([128, 32], BF16)
            gwT = PA.get([128, 4, 128], BF16)
            gw2b = PA.get([32, 128], BF16)
            Sst = PA.get([128, 256], F32)
            zbuf = [PA.get([128, 514], F32) for _ in range(2)]
            lr1T = PA.get([32, 512], BF16)
            ropeCt = PA.get([128, 512], F32)
            ropeSt = PA.get([128, 512], F32)
            Rks, Rkw, Rvs, Rvw, Rkvc, Rkc, RhTv, RvcM, RS, Rlr, Rrt = (Res() for _ in range(11))
            Rz = [Res(), Res()]
            xt = [BUF([128, 1024], F32)]; Rxt = [Res()]
            xs = BUF([128, 4, 1024], BF16); Rxs = Res()
            mixT = xs.rearrange("p a b -> p (a b)").rearrange("p (k t) -> p k t", k=8)
            hT = BUF([128, 8, 512], BF16); RhT = Res()
            QA = [BUF([128, 512], BF16) for _ in range(4)]; RQA = [Res() for _ in range(4)]
            qraw = BUF([128, 512], BF16); Rqr = Res()
            kraw = BUF([128, 512], BF16); Rkr = Res()
            pT = [BUF([128, 512], BF16) for _ in range(3)]; RpT = [Res() for _ in range(3)]
            cvy = BUF([128, 512], F32); Rcvy = Res()
            cvo12 = BUF([128, 1024], F32)
            cvo = [cvo12[:, 0:512], cvo12[:, 512:1024]]; Rcvo = [Res(), Res()]
            glq = BUF([128, 512], F32); glk = BUF([128, 512], F32); Rgl = Res()
            gts = BUF([128, 4, 12], F32); Rgts = Res()
            nrm = BUF([128, 4, 768], BF16); Rnrm = Res()
            nsa = BUF([128, 4, 256], F32); Rnsa = Res()
            pslc = BUF([128, 4, 64], F32); Rpslc = Res()
            selb = BUF([128, 128], BF16); Rselb = Res()
            t12 = BUF([128, 1024], F32)
            t1 = t12[:, 0:512]; t2 = t12[:, 512:1024]; Rt1 = Res(); Rt2 = Res()
            gwT32 = t1.rearrange("p (a b) -> p a b", a=4)
            g_e = BUF([128, 128], F32); g_lsp = BUF([128, 128], BF16)
            g_b = BUF([128, 128], F32); g_tmp = g_e
            g_e1 = BUF([128, 128], F32); g_e2 = BUF([128, 128], F32); g_e3 = g_e1
            g_qe = BUF([128, 128], BF16); g_ke = BUF([128, 128], BF16); g_qb = BUF([128, 128], BF16)
            g_qeb = BUF([128, 4, 128], BF16); g_ed = BUF([128, 128], F32); g_kd = BUF([128, 128], BF16)
            g_att = BUF([128, 4, 128], BF16); g_sbf = [BUF([128, 256], BF16) for _ in range(2)]
            g_kt = BUF([128, 128], F32); g_lv = BUF([128, 256], BF16); g_lg = BUF([128, 256], F32)
            g_sq = BUF([128, 256], F32); g_o = BUF([128, 256], F32)
            m_u = BUF([128, 256], F32); m_v = BUF([128, 256], F32); m_sq = g_sq
            m_vln = BUF([128, 256], BF16)
            Rg = Res()
            Rs1, RsG, RsA, Rs7 = Res(), Res(), Res(), Res()
            xbufs = [(xt[0], [Rxt[0]]), (t12, [Rt1, Rt2]), (nsa.rearrange("p a b -> p (a b)"), [Rnsa]),
                     (cvo12, [Rcvo[0], Rcvo[1]])]
            pY = Pool([3, 4, 5, 6, 7])
            ysb = BUF([128, 1024], F32); Rysb = Res()
            sqc = [BUF([128, 512], BF16) for _ in range(2)]; Rsqc = Res()
            S.dma("pool", ksA[64:128, :], cst_d["ew"], writes=[Rks])
            S.dma("pool", W1[0:64], w1_d[l, 0].rearrange("(p d) h -> d p h", d=64), writes=[Rw])
            S.dma("pool", W1[64:128], w1_d[l, 1].rearrange("(p d) h -> d p h", d=64), writes=[Rw])
            S.dma("pool", w2k, w2_d[l, 0], writes=[Rw])
            S.dma("pool", w2v, w2_d[l, 1], writes=[Rw])
            S.dma("pool", posT, posT_d[l], writes=[Rw])
            S.dma("pool", gw2b[0:17, :], gw2b_d[l], writes=[Rw])
            S.dma("sp", gwT32, gws_d[l], writes=[Rt1])
            tt("dve", gwT, gwT32, triu[:].unsqueeze(1).to_broadcast([128, 4, 128]), ALU.mult, [Rt1, Rc], [Rw])
            memset("dve", vs1[:, :, 64:65], 1.0, [Rvs])
            memset("dve", vw1[:, :, 64:65], 1.0, [Rvw])
            memset("dve", kvc[:, 0:16], 0.0, [Rkvc])
            memset("dve", kcT, 0.0, [Rkc])
            memset("dve", kwT[64:128, :], 0.0, [Rkw])
            memset("dve", hTv, 0.0, [RhTv])
            memset("dve", Sst, 0.0, [RS])
            memset("dve", lr1T, 1.0, [Rlr])
            for c2 in range(2):
                memset("dve", zbuf[c2][:, 0:2], 0.0, [Rz[c2]])
            cp("dve", vcM[:, :, 64:128], mo[:], [Rc], [RvcM])
            for jkv in range(2):
                pc1, Rpc1 = pAUX.get() if jkv == 0 else pACC.get()
                rs_ = slice(64 * jkv, 64 * jkv + 64)
                for p_ in range(32):
                    mm(pc1[:, 0:1], W1[rs_, p_, :], posT[rs_, p_:p_ + 1], p_ == 0, p_ == 31, [Rw], [Rpc1])
                cp("dve", c1col[:, jkv:jkv + 1], pc1[:, 0:1], [Rpc1], [Rw])
            if stop == "p1init":
                return done()

            xsv = xview(xsrc)
            xmv = xview(xmid)
            for tb in range(NB):
                bc = slice(tb * 512, (tb + 1) * 512)
                S.dma("sp", ropeCt, ropeC_d[:, bc], writes=[Rrt])
                S.dma("sp", ropeSt, ropeS_d[:, bc], writes=[Rrt])
                Rsm = Rs1
                for j in range(4):
                    S.dma("sp", xbufs[j][0], xsv[:, 4 * tb + j, :], writes=xbufs[j][1])
                for j in range(4):
                    xb, Rxb = xbufs[j]
                    act(xs[:, j, :], xb, AF.Square, Rxb, [Rxs, Rsm], accum_out=small[:, j:j + 1])
                    rsqrt(small[:, 4 + j:5 + j], small[:, j:j + 1], 1.0 / D, [Rsm], [Rsm])
                    ts("dve", xs[:, j, :], xb, small[:, 4 + j:5 + j], None, ALU.mult, None, Rxb + [Rsm], [Rxs])
                for k in range(8):
                    pb, Rpb = pMM.get()
                    pbv = pb[:].bitcast(BF16)
                    for j in range(4):
                        tr(pbv[:, j * 128:(j + 1) * 128], xs[:, j, k * 128:(k + 1) * 128], ident[:], [Rxs, Rc], [Rpb])
                    if k % 2 == 0:
                        act(hT[:, k, :], pbv[:, 0:512], AF.Identity, [Rpb, Rmod], [RhT],
                            scale=A1[:, k:k + 1], bias=sh1[:, k:k + 1])
                    else:
                        ts("dve", hT[:, k, :], pbv[:, 0:512], A1[:, k:k + 1], sh1[:, k:k + 1], ALU.mult, ALU.add,
                           [Rpb, Rmod], [RhT])
                if l == 0 and tb == 0:
                    tap("hT", hT.rearrange("p k t -> p (k t)"), [RhT])

                if stop == "b0s2":
                    return done()
                def fm(ch):
                    pb, Rpb = pMM.get()
                    w = FM_W[ch]
                    c0 = FM_OFF[ch]
                    for k in range(8):
                        mm(pb[0:w, :], w_in[:, k, c0:c0 + w], hT[:, k, :], k == 0, k == 7, [RW1i[k], RhT], [Rpb])
                    return pb, Rpb

                def tmc(ti, jc):
                    pb, Rpb = pMM.get()
                    for k in range(8):
                        mm(pb[:, 0:T_W[ti]], hT[:, k, jc], w_in[:, k, T_OFF[ti]:T_OFF[ti] + T_W[ti]],
                           k == 0, k == 7, [RW1i[k], RhT], [Rpb])
                    return pb, Rpb

                def rope(src_ps, Rsrc, raw, Rraw, dst, Rdst):
                    cp("act", raw[:], src_ps[:], [Rsrc], [Rraw])
                    p2, Rp2 = pMM.get()
                    mm(p2[:], pm[:], raw[:], True, True, [Rc, Rraw], [Rp2])
                    if stop == "q0c":
                        return "STOP"
                    tt("dve", t1, src_ps[:], ropeCt, ALU.mult, [Rsrc, Rrt], [Rt1])
                    if stop == "q0d":
                        return "STOP"
                    tt("dve", t2, p2[:], ropeSt, ALU.mult, [Rp2, Rrt], [Rt2])
                    if stop == "q0e":
                        return "STOP"
                    tt("dve", dst, t1[0:64], t2[0:64], ALU.add, [Rt1, Rt2], [Rdst])

                for c2 in range(2):
                    z = zbuf[c2]
                    pb, Rpb = fm(C_AH + c2)
                    cp("act", z[:, 2:514], pb[:], [Rpb], [Rz[c2]])
                    pb, Rpb = fm(C_AC + c2)
                    tt("dve", z[:, 2:514], pb[:], z[:, 2:514], ALU.mult, [Rpb, Rz[c2]], [Rz[c2]])
                    ts("pool", cvy, z[:, 2:514], convw[:, c2, 2:3], None, ALU.mult, None, [Rz[c2], Rw], [Rcvy])
                    stt("dve", cvy, z[:, 1:513], convw[:, c2, 1:2], cvy, ALU.mult, ALU.add, [Rz[c2], Rw, Rcvy], [Rcvy])
                    stt("dve", cvy, z[:, 0:512], convw[:, c2, 0:1], cvy, ALU.mult, ALU.add, [Rz[c2], Rw, Rcvy], [Rcvy])
                    cp("pool", z[:, 0:2], z[:, 512:514], [Rz[c2]], [Rz[c2]])
                    pb, Rpb = fm(C_AB + c2)
                    tt("dve", cvo[c2], pb[:], cvy, ALU.mult, [Rpb, Rcvy], [Rcvo[c2]])
                    act(sqc[c2], cvo[c2], AF.Square, [Rcvo[c2]], [Rsqc])
                if stop == "s3a":
                    return done()
                pb, Rpb = fm(C_KVC)
                cp("act", kvc[:, 16:528], pb[:], [Rpb], [Rkvc])
                if stop == "s3b1":
                    return done()
                phk, Rphk = pAUX.get()
                phv, Rphv = pACC.get()
                for jkv in range(2):
                    rs_ = slice(64 * jkv, 64 * jkv + 64)
                    for p_ in range(32):
                        mm((phk, phv)[jkv][:, 0:32], W1[rs_, p_, :], kvc[rs_, p_:p_ + 497:16], p_ == 0, p_ == 31,
                           [Rw, Rkvc], [(Rphk, Rphv)[jkv]])
                if stop == "s3b2":
                    return done()
                hk = t1[:, 0:16].bitcast(BF16)
                act(hk, phk[:, 0:32], AF.Silu, [Rphk, Rw], [Rt1], bias=c1col[:, 0:1])
                sl = slice(32 * tb, 32 * tb + 32)
                act(hTv[:, sl], phv[:, 0:32], AF.Silu, [Rphv, Rw], [RhTv], bias=c1col[:, 1:2])
                if tb == 0:
                    memset("dve", hTv[:, 0:1], 0.0, [RhTv])
                if stop == "s3b3":
                    return done()
                cp("pool", kvc[:, 0:16], kvc[:, 512:528], [Rkvc], [Rkvc])
                if stop == "s3b4":
                    return done()
                pk2, Rpk2 = pAUX.get()
                mm(pk2[0:64, 0:32], w2k, hk, True, True, [Rw, Rt1], [Rpk2])
                cch = tb // 4
                mm(pk2[:, 64:128], hTv[:, 128 * cch:128 * cch + 128], w2v, True, True, [RhTv, Rw], [Rpk2])
                cp("act", kcT[0:64, sl], pk2[0:64, 0:32], [Rpk2], [Rkc])
                cp("act", vcM[:, cch, 0:64], pk2[:, 64:128], [Rpk2], [RvcM])
                if stop == "s3b":
                    return done()
                for j in range(4):
                    J = 4 * tb + j
                    pb, Rpb = tmc(0, slice(j * 128, (j + 1) * 128))
                    cp("act", vs1[:, J, 0:64], pb[:, 0:64], [Rpb], [Rvs])
                    cp("act", vw1[:, J % 8, 0:64], pb[:, 64:128], [Rpb], [Rvw])
                    act(gts[:, j, :], pb[:, 128:140], AF.Sigmoid, [Rpb], [Rgts])
                if stop == "s3c":
                    return done()
                Rsm = RsA
                nch = 1 if tb < 4 else 2
                for h in range(4):
                    pb, Rpb = fm(C_Q + h)
                    if rope(pb, Rpb, qraw, Rqr, QA[h][0:64, :], RQA[h]) == "STOP":
                        return done()
                    if l == 0 and tb == 0 and h == 0:
                        tap("qa0", QA[0][0:64, :], [RQA[0]])
                        tap("qraw0", qraw[0:64, :], [Rqr])
                        tap("ropeC", ropeCt, [Rrt])
                        tap("ropeS", ropeSt, [Rrt])
                    if stop == "q1":
                        return done()
                    for c in range(nch):
                        pb, Rpb = pMM.get()
                        masked = (c == nch - 1)
                        mm(pb[:], kcT[:, 128 * c:128 * c + 128], qraw[:, :], True, not masked,
                           [Rkc, Rqr], [Rpb])
                        if masked:
                            mm(pb[:], ident[:], masks[:, 8 + (tb % 4), :], False, True, [Rc], [Rpb])
                        act(pT[c], pb[:], AF.Exp, [Rpb], [RpT[c]], scale=0.125)
                    if stop == "q2":
                        return done()
                    pacc, Rpacc = pACC.get()
                    pa3 = pacc[:].rearrange("p (j c) -> p j c", j=4)
                    for j in range(4):
                        for c in range(nch):
                            mm(pa3[:, j, :], pT[c][:, j * 128:(j + 1) * 128], vcM[:, c, :], c == 0, c == nch - 1,
                               [RpT[c], RvcM], [Rpacc])
                    if stop == "q3":
                        return done()
                    den = small[:, 32:36]
                    red(den, pa3[:, :, 64:128], [Rpacc], [Rsm])
                    ts("dve", den, den, 0.5, 1e-30, ALU.mult, ALU.max, [Rsm], [Rsm])
                    recip(den, den, [Rsm], [Rsm])
                    u3 = t1[:, 0:256].rearrange("p (j c) -> p j c", j=4)
                    tt("dve", u3, pa3[:, :, 64:128], den.unsqueeze(2).to_broadcast([128, 4, 64]), ALU.mult,
                       [Rpacc, Rsm], [Rt1])
                    if h == 0:
                        cp("pool", pslc, u3, [Rt1], [Rpslc])
                    else:
                        tt("pool", pslc, pslc, u3, ALU.add, [Rt1, Rpslc], [Rpslc])
                    tt("dve", small[:, 36:40], den, gts[:, :, 3 * h], ALU.mult, [Rsm, Rgts], [Rsm])
                    tt("dve", nsa[:, :, h * 64:(h + 1) * 64], pa3[:, :, 0:64],
                       small[:, 36:40].unsqueeze(2).to_broadcast([128, 4, 64]), ALU.mult, [Rpacc, Rsm], [Rnsa])
                if stop == "s3d":
                    return done()
                if l == 0:
                    for j in range(4):
                        tap("nsac", nsa[:, j, :], [Rnsa], lambda d, J=4 * tb + j: d[J * 128:(J + 1) * 128, :])
                pb, Rpb = fm(C_KS)
                rope(pb, Rpb, kraw, Rkr, ksA[0:64, bc], Rks)
                if l == 0 and tb == 0:
                    tap("ks0", ksA[0:64, 0:512], [Rks])
                pb, Rpb = fm(C_KW)
                rope(pb, Rpb, kraw, Rkr, kwT[0:64, (tb % 2) * 512:(tb % 2) * 512 + 512], Rkw)
                pb, Rpb = fm(C_LQ)
                cp("act", glq, pb[:], [Rpb], [Rgl])
                pb, Rpb = fm(C_LK)
                cp("act", glk, pb[:], [Rpb], [Rgl])
                pb, Rpb = fm(C_LR)
                cp("act", lr1T[0:16, :], pb[0:16, :], [Rpb], [Rlr])

                if stop == "b0s3":
                    return done()
                Rsm = RsA
                psl, Rpsl = pMM.get()
                pslv = psl[:].bitcast(BF16)
                for j in range(4):
                    J = 4 * tb + j
                    sc_ = t2[:, 0:64]
                    tt("dve", sc_, pslc[:, j, :], wd[:, 62 - 2 * J:126 - 2 * J], ALU.add, [Rpslc, Rc], [Rt2])
                    ts("dve", sc_[:, 0:1], sc_[:, 0:1], 3e4, None, ALU.add, None, [Rt2], [Rt2])
                    m8 = small[:, 40:56]
                    S.op("dve", lambda e, sc_=sc_, m8=m8: e.max(out=m8[:, 0:8], in_=sc_), [Rt2], [Rsm])
                    wk_ = t2[:, 64:128]
                    S.op("dve", lambda e, sc_=sc_, m8=m8, wk_=wk_: e.match_replace(
                        out=wk_, in_to_replace=m8[:, 0:8], in_values=sc_, imm_value=-3e38), [Rt2, Rsm], [Rt2])
                    S.op("dve", lambda e, m8=m8, wk_=wk_: e.max(out=m8[:, 8:16], in_=wk_), [Rt2], [Rsm])
                    if j == 0:
                        memset("dve", selb[:, 0:64], 0.0, [Rselb])
                    ts("dve", selb[:, 64:128], sc_, m8[:, 15:16], NEG, ALU.is_lt, ALU.mult, [Rt2, Rsm], [Rselb])
                    tr(pslv[:, j * 128:(j + 1) * 128], selb[:], ident[:], [Rselb, Rc], [Rpsl])
                for h in range(4):
                    cp("act" if h % 2 else "dve", QA[h][64:128, :], pslv[64:128, 0:512], [Rpsl], [RQA[h]])

                jobs = []
                for h in range(4):
                    jobs.append(dict(h=h, kts=list(range(0, 4 * tb + 4)), keyT=ksA, Rk=Rks, v1=vs1, Rv=Rvs,
                                     mask_of=(lambda kt: (kt - 4 * tb) if kt >= 4 * tb else None),
                                     rel_of=(lambda kt, j: kt <= 4 * tb + j), gcol=3 * h + 1, kmod=32))
                    jobs.append(dict(h=h, kts=list(range(max(0, 4 * tb - 4), 4 * tb + 4)), keyT=kwT, Rk=Rkw, v1=vw1, Rv=Rvw,
                                     mask_of=(lambda kt: (kt - 4 * tb) if kt >= 4 * tb else (4 + (kt - 4 * tb + 4))),
                                     rel_of=(lambda kt, j: (kt <= 4 * tb + j) and (kt >= 4 * tb + j - 4)),
                                     gcol=3 * h + 2, kmod=8))
                units = []
                for jb_ in jobs:
                    jb_["last"] = {}
                    for kt in jb_["kts"]:
                        for j in range(4):
                            if jb_["rel_of"](kt, j):
                                jb_["last"][j] = kt
                    jb_["started"] = False
                    for n, kt in enumerate(jb_["kts"]):
                        units.append((jb_, n, kt))
                LOOK = 2

                def emit_score(i):
                    jb_, n, kt = units[i]
                    h = jb_["h"]
                    js = [j for j in range(4) if jb_["rel_of"](kt, j)]
                    cs_ = slice(128 * js[0], 128 * (js[-1] + 1))
                    pb, Rpb = pMM.get()
                    mk = jb_["mask_of"](kt)
                    km = jb_["kmod"]
                    kc_ = slice((kt % km) * 128, (kt % km + 1) * 128)
                    mm(pb[:, cs_], jb_["keyT"][:, kc_], QA[h][:, cs_], True, mk is None, [jb_["Rk"], RQA[h]], [Rpb])
                    if mk is not None:
                        mm(pb[:, cs_], ident[:], masks[:, mk, cs_], False, True, [Rc], [Rpb])
                    act(pT[i % 3][:, cs_], pb[:, cs_], AF.Exp, [Rpb], [RpT[i % 3]], scale=0.125)

                def emit_pv(i):
                    jb_, n, kt = units[i]
                    h = jb_["h"]
                    if n == 0:
                        jb_["pacc"] = pACC.get()
                    pacc, Rpacc = jb_["pacc"]
                    pa3 = pacc[:, 0:260].rearrange("p (j c) -> p j c", j=4)
                    km = jb_["kmod"]
                    pt_ = pT[i % 3]
                    for j in range(4):
                        if jb_["rel_of"](kt, j):
                            mm(pa3[:, j, :], pt_[:, j * 128:(j + 1) * 128], jb_["v1"][:, kt % km, :],
                               not jb_["started"], kt == jb_["last"][j], [RpT[i % 3], jb_["Rv"]], [Rpacc],
                               skip_group_check=True)
                            jb_["started"] = True
                    if n == len(jb_["kts"]) - 1:
                        den = small[:, 32:36]
                        ts("dve", den, pa3[:, :, 64], 1e-30, None, ALU.max, None, [Rpacc], [Rsm])
                        recip(den, den, [Rsm], [Rsm])
                        tt("dve", small[:, 36:40], den, gts[:, :, jb_["gcol"]], ALU.mult, [Rsm, Rgts], [Rsm])
                        u3 = t1[:, 0:256].rearrange("p (j c) -> p j c", j=4)
                        tt("dve", u3, pa3[:, :, 0:64], small[:, 36:40].unsqueeze(2).to_broadcast([128, 4, 64]),
                           ALU.mult, [Rpacc, Rsm], [Rt1])
                        tt("pool", nsa[:, :, h * 64:(h + 1) * 64], nsa[:, :, h * 64:(h + 1) * 64], u3, ALU.add,
                           [Rt1, Rnsa], [Rnsa])

                def gen_B():
                    for j in range(4):
                        J = 4 * tb + j
                        yield
                        jc = slice(j * 128, (j + 1) * 128)
                        yield
                        pb, Rpb = tmc(1, jc)
                        yield
                        cp("dve", g_kt, pb[:, 0:128], [Rpb], [Rg])
                        yield
                        cp("dve", g_lv, pb[:, 128:384], [Rpb], [Rg])
                        yield
                        pb, Rpb = tmc(2, jc)
                        yield
                        act(g_lg, pb[:, 0:256], AF.Silu, [Rpb], [Rg])
                        yield
                        cp("dve", m_u, pb[:, 256:512], [Rpb], [Rg])
                        yield
                        pb, Rpb = tmc(3, jc)
                        yield
                        cp("dve", m_v, pb[:, 0:256], [Rpb], [Rg])
                        yield
                        pa, Rpa = pAUX.get()
                        yield
                        mm(pa[:, 0:128], lr1T[0:17, jc], gw2b[0:17, :], True, True, [Rlr, Rw], [Rpa])
                        yield
                        act(g_e, pa[:, 0:128], AF.Exp, [Rpa], [Rg], scale=-1.0)
                        yield
                        act(g_lsp, g_e, AF.Ln, [Rg], [Rg], bias=1.0)
                        yield
                        pbT, RpbT = pAUX.get()
                        yield
                        mm(pbT[:, 0:128], g_lsp, tinc[:], True, True, [Rg, Rc], [RpbT])
                        yield
                        mm(pbT[:, 128:256], trev[:], g_lsp, True, True, [Rg, Rc], [RpbT])
                        yield
                        cp("dve", g_b, pbT[:, 0:128], [RpbT], [Rg])
                        yield
                        gb3 = g_b.rearrange("p (c t) -> p c t", c=2)
                        yield
                        tt("dve", g_tmp.rearrange("p (c t) -> p c t", c=2), gb3, gb3[:, :, 32:33].to_broadcast([128, 2, 64]),
                           ALU.subtract, [Rg], [Rg])
                        yield
                        qsc = float(32 ** -0.5)
                        yield
                        act(g_e1, g_tmp, AF.Exp, [Rg], [Rg])
                        yield
                        act(g_e2, g_tmp, AF.Exp, [Rg], [Rg], scale=-1.0)
                        yield
                        stt("dve", g_qe, glq[:, jc], qsc, g_e1, ALU.mult, ALU.mult, [Rgl, Rg], [Rg])
                        yield
                        tt("dve", g_ke, glk[:, jc], g_e2, ALU.mult, [Rgl, Rg], [Rg])
                        yield
                        act(g_e3, g_b, AF.Exp, [Rg], [Rg])
                        yield
                        act(small[:, 8:10], gb3[:, :, 63], AF.Exp, [Rg], [RsG])
                        yield
                        act(g_ed, pbT[:, 128:256], AF.Exp, [RpbT], [Rg])
                        yield
                        stt("dve", g_qb, glq[:, jc], qsc, g_e3, ALU.mult, ALU.mult, [Rgl, Rg], [Rg])
                        yield
                        tt("dve", g_qeb, bdq[:], g_qe.unsqueeze(1).to_broadcast([128, 4, 128]), ALU.mult, [Rc, Rg], [Rg])
                        yield
                        tt("dve", g_kd, g_kt, g_ed, ALU.mult, [Rg], [Rg])
                        yield
                        patt, Rpatt = pMM.get()
                        yield
                        mm(patt[:], g_ke, g_qeb.rearrange("p a b -> p (a b)"), True, True, [Rg], [Rpatt])
                        yield
                        tt("dve", g_att, patt[:].rearrange("p (a b) -> p a b", a=4),
                           glam[:].unsqueeze(1).to_broadcast([128, 4, 128]), ALU.mult, [Rpatt, Rc], [Rg])
                        yield
                        pkvs = [pAUX.get(), pAUX.get()]
                        yield
                        for c in range(2):
                            rs_ = slice(64 * c, 64 * c + 64)
                            mm(pkvs[c][0][:, 0:256], g_kd[rs_, :], g_lv[rs_, :], True, True, [Rg], [pkvs[c][1]])
                        yield
                        for c in range(2):
                            tt("dve", g_sbf[c], Sst, bds[:], ALU.mult, [RS, Rc], [Rg])
                            stt("dve", Sst, Sst, small[:, 8 + c:9 + c], pkvs[c][0][:, 0:256], ALU.mult, ALU.add,
                                [RS, RsG, pkvs[c][1]], [RS])
                        yield
                        po, Rpo = pAUX.get()
                        yield
                        for c in range(2):
                            rs_ = slice(64 * c, 64 * c + 64)
                            mm(po[rs_, 0:256], g_qb[:, rs_], g_sbf[c], True, False, [Rg], [Rpo], skip_group_check=True)
                        yield
                        for hh in range(4):
                            mm(po[:, hh * 64:(hh + 1) * 64], g_att[:, hh, :], g_lv[:, hh * 64:(hh + 1) * 64], False, True,
                               [Rg], [Rpo], skip_group_check=True)
                        yield
                        act(g_sq, po[:, 0:256], AF.Square, [Rpo], [Rg])
                        yield
                        red(small[:, 12:16], g_sq.rearrange("p (h d) -> p h d", h=4), [Rg], [RsG])
                        yield
                        rsqrt(small[:, 12:16], small[:, 12:16], 1.0 / 64, [RsG], [RsG])
                        yield
                        tt("dve", g_o.rearrange("p (h d) -> p h d", h=4), po[:, 0:256].rearrange("p (h d) -> p h d", h=4),
                           small[:, 12:16].unsqueeze(2).to_broadcast([128, 4, 64]), ALU.mult, [Rpo, RsG], [Rg])
                        yield
                        tt("pool", g_o, g_o, g_lg, ALU.mult, [Rg], [Rg])
                        yield
                        if l == 0:
                            tap("gla", g_o, [Rg], lambda d, J=J: d[J * 128:(J + 1) * 128, :])
                        yield
                        act(g_sq, g_o, AF.Square, [Rg], [Rg, RsG], accum_out=small[:, 16:17])
                        yield
                        rsqrt(small[:, 16:17], small[:, 16:17], 1.0 / 256, [RsG], [RsG])
                        yield
                        ts("dve", nrm[:, j, 256:512], g_o, small[:, 16:17], None, ALU.mult, None, [Rg, RsG], [Rnrm])
                        yield
                        mv3 = m_v.rearrange("p (g d) -> p g d", g=4)
                        yield
                        red(small[:, 20:24], mv3, [Rg], [RsG])
                        yield
                        tt("pool", m_sq, m_v, m_v, ALU.mult, [Rg], [Rg])
                        yield
                        red(small[:, 24:28], m_sq.rearrange("p (g d) -> p g d", g=4), [Rg], [RsG])
                        yield
                        ts("dve", small[:, 20:24], small[:, 20:24], 1.0 / 64, None, ALU.mult, None, [RsG], [RsG])
                        yield
                        tt("dve", small[:, 28:32], small[:, 20:24], small[:, 20:24], ALU.mult, [RsG], [RsG])
                        yield
                        stt("dve", small[:, 24:28], small[:, 24:28], 1.0 / 64, small[:, 28:32], ALU.mult, ALU.subtract,
                            [RsG], [RsG])
                        yield
                        rsqrt(small[:, 24:28], small[:, 24:28], 1.0, [RsG], [RsG])
                        yield
                        tt("dve", mv3, mv3, small[:, 20:24].unsqueeze(2).to_broadcast([128, 4, 64]), ALU.subtract,
                           [Rg, RsG], [Rg])
                        yield
                        tt("dve", m_vln.rearrange("p (g d) -> p g d", g=4), mv3,
                           small[:, 24:28].unsqueeze(2).to_broadcast([128, 4, 64]), ALU.mult, [Rg, RsG], [Rg])
                        yield
                        pgm, Rpgm = pAUX.get()
                        yield
                        for g in range(4):
                            mm(pgm[:, g * 64:(g + 1) * 64], gwT[:, g, :], m_vln[:, g * 64:(g + 1) * 64], True, True,
                               [Rw, Rg], [Rpgm])
                        yield
                        mo3 = m_sq.rearrange("p (g d) -> p g d", g=4)
                        yield
                        tt("dve", mo3, pgm[:, 0:256].rearrange("p (g d) -> p g d", g=4),
                           gbT[:].unsqueeze(2).to_broadcast([128, 4, 64]), ALU.add, [Rpgm, Rw], [Rg])
                        yield
                        tt("pool", m_sq, m_sq, m_u, ALU.mult, [Rg], [Rg])
                        yield
                        if l == 0:
                            tap("gmlp", m_sq, [Rg], lambda d, J=J: d[J * 128:(J + 1) * 128, :])
                        yield
                        act(m_v, m_sq, AF.Square, [Rg], [Rg, RsG], accum_out=small[:, 17:18])
                        yield
                        rsqrt(small[:, 17:18], small[:, 17:18], 1.0 / 256, [RsG], [RsG])
                        yield
                        ts("dve", nrm[:, j, 512:768], m_sq, small[:, 17:18], None, ALU.mult, None, [Rg, RsG], [Rnrm])
                        yield
                def gen_A():
                    for i in range(len(units) + LOOK):
                        if i < len(units):
                            emit_score(i)
                        if i >= LOOK:
                            emit_pv(i - LOOK)
                        yield

                gA, gB = gen_A(), gen_B()
                per = max(1, -(-(4 * 68) // (len(units) + LOOK)))
                aliveA = aliveB = True
                while aliveA or aliveB:
                    if aliveA:
                        try:
                            next(gA)
                        except StopIteration:
                            aliveA = False
                    nb_ = per if aliveA else 1 << 30
                    while aliveB and nb_ > 0:
                        nb_ -= 1
                        try:
                            next(gB)
                        except StopIteration:
                            aliveB = False
                if stop == "b0s4":
                    return done()
                for j in range(4):
                    J = 4 * tb + j
                    if l == 0:
                        tap("nsa", nsa[:, j, :], [Rnsa], lambda d, J=J: d[J * 128:(J + 1) * 128, :])
                    act(t2[:, 0:256], nsa[:, j, :], AF.Square, [Rnsa], [Rt2, Rsm], accum_out=small[:, 18:19])
                    rsqrt(small[:, 18:19], small[:, 18:19], 1.0 / 256, [Rsm], [Rsm])
                    ts("dve", nrm[:, j, 0:256], nsa[:, j, :], small[:, 18:19], None, ALU.mult, None, [Rnsa, Rsm], [Rnrm])

                if stop == "b0s5":
                    return done()
                pss, Rpss = pMM.get()
                for c2 in range(2):
                    mm(pss[:], ones[:], sqc[c2], c2 == 0, c2 == 1, [Rc, Rsqc], [Rpss])
                rsqrt(t1, pss[:], 1.0 / 256, [Rpss], [Rt1])
                for c2 in range(2):
                    if l == 0:
                        tap("conv", cvo[c2], [Rcvo[c2]], lambda d, c2=c2, bc=bc: d[c2 * 128:(c2 + 1) * 128, bc])
                    stt("dve", mixT[:, c2, :], cvo[c2], gainc[:, c2:c2 + 1], t1, ALU.mult, ALU.mult,
                        [Rcvo[c2], Rw, Rt1], [Rxs])
                for cch6 in range(6):
                    pb, Rpb = pMM.get()
                    pbv = pb[:].bitcast(BF16)
                    for j in range(4):
                        tr(pbv[:, j * 128:(j + 1) * 128], nrm[:, j, cch6 * 128:(cch6 + 1) * 128], ident[:],
                           [Rnrm, Rc], [Rpb])
                    act(mixT[:, 2 + cch6, :], pbv[:, 0:512], AF.Copy, [Rpb, Rw], [Rxs], scale=gainc[:, 2 + cch6:3 + cch6])

                Rsm = Rs7
                for j in range(4):
                    S.dma("sp", xbufs[j][0], xsv[:, 4 * tb + j, :], writes=xbufs[j][1])
                for j in range(4):
                    J = 4 * tb + j
                    xb, Rxb = xbufs[j]
                    pys = []
                    for half in range(2):
                        py, Rpy = pY.get()
                        for k in range(8):
                            mm(py[:], mixT[:, k, j * 128:(j + 1) * 128], w_o[:, k, half * 512:(half + 1) * 512],
                               k == 0, k == 7, [Rxs, RW1o[k // 2]], [Rpy])
                        act(sqc[half], py[:], AF.Square, [Rpy], [Rsqc, Rsm],
                            accum_out=small[:, 56 + half:57 + half])
                        pys.append((py, Rpy))
                    tt("dve", small[:, 58:59], small[:, 56:57], small[:, 57:58], ALU.add, [Rsm], [Rsm])
                    rsqrt(small[:, 58:59], small[:, 58:59], 1.0 / D, [Rsm], [Rsm])
                    for half in range(2):
                        py, Rpy = pys[half]
                        hs = slice(half * 512, (half + 1) * 512)
                        stt("dve", ysb[:, hs], py[:], small[:, 58:59], G1[:, hs], ALU.mult, ALU.mult,
                            [Rpy, Rsm, RG], [Rysb])
                        tt("pool", xb[:, hs], xb[:, hs], ysb[:, hs], ALU.add, Rxb + [Rysb], Rxb)
                    S.dma("sp", xmv[:, J, :], xb, reads=Rxb)
                if stop == "b0":
                    return done()
            S.barrier()
            if stop == "p1":
                return done()

            WB.reset()
            PA.reset(PA_base)
            w_up = WB.get([128, 8, DFF], BF16)
            w_dn = WB.get([128, 32, D], BF16)
            RW2u = [Res() for _ in range(8)]
            RW2d = [Res() for _ in range(8)]
            wuv = w_up_d[l].rearrange("(k p) n -> p k n", p=128)
            for k in range(8):
                S.dma("pool", w_up[:, k, :], wuv[:, k, :], writes=[RW2u[k]])
            wdv = w_dn_d[l].rearrange("(k p) n -> p k n", p=128)
            for k in range(0, 32, 4):
                S.dma("pool", w_dn[:, k:k + 4, :], wdv[:, k:k + 4, :], writes=[RW2d[k // 4]])
            xt = [PA.get([128, 1024], F32) for _ in range(2)]; Rxt = [Res(), Res()]
            xs2 = PA.get([128, 2, 1024], BF16); Rxs = Res()
            hT2 = PA.get([128, 8, 256], BF16); RhT = Res()
            actT = PA.get([128, 32, 256], BF16); Ract = Res()
            ysb = PA.get([128, 1024], F32); Rysb = Res()
            xdv = xview(xdst)
            for tb in range(S_ // 256):
                Rsm = Rs1
                for j in range(2):
                    J = 2 * tb + j
                    S.dma("sp", xt[j], xmv[:, J, :], writes=[Rxt[j]])
                    act(xs2[:, j, :], xt[j], AF.Square, [Rxt[j]], [Rxs, Rsm], accum_out=small[:, j:j + 1])
                    rsqrt(small[:, 4 + j:5 + j], small[:, j:j + 1], 1.0 / D, [Rsm], [Rsm])
                    ts("dve", xs2[:, j, :], xt[j], small[:, 4 + j:5 + j], None, ALU.mult, None, [Rxt[j], Rsm], [Rxs])
                for k in range(8):
                    pb, Rpb = pMM.get()
                    pbv = pb[:].bitcast(BF16)
                    for j in range(2):
                        tr(pbv[:, j * 128:(j + 1) * 128], xs2[:, j, k * 128:(k + 1) * 128], ident[:], [Rxs, Rc], [Rpb])
                    if k % 2 == 0:
                        act(hT2[:, k, :], pbv[:, 0:256], AF.Identity, [Rpb, Rmod], [RhT],
                            scale=A2[:, k:k + 1], bias=sh2[:, k:k + 1])
                    else:
                        ts("dve", hT2[:, k, :], pbv[:, 0:256], A2[:, k:k + 1], sh2[:, k:k + 1], ALU.mult, ALU.add,
                           [Rpb, Rmod], [RhT])
                for hc in range(0, 32, 2):
                    pb, Rpb = pMM.get()
                    for u in range(2):
                        for k in range(8):
                            mm(pb[:, u * 256:(u + 1) * 256], w_up[:, k, (hc + u) * 128:(hc + u + 1) * 128], hT2[:, k, :],
                               k == 0, k == 7, [RW2u[k], RhT], [Rpb])
                    av = actT[:, hc:hc + 2, :].rearrange("p a b -> p (a b)")
                    act(av, pb[:], AF.Relu, [Rpb], [Ract])
                    tt("pool" if (hc // 2) % 2 else "dve", av, av, av, ALU.mult, [Ract], [Ract])
                Rsm = Rs7
                for j in range(2):
                    J = 2 * tb + j
                    pys = []
                    for half in range(2):
                        py, Rpy = pAUX.get() if half else pACC.get()
                        for k in range(32):
                            mm(py[:], actT[:, k, j * 128:(j + 1) * 128], w_dn[:, k, half * 512:(half + 1) * 512],
                               k == 0, k == 31, [Ract, RW2d[k // 4]], [Rpy])
                        act(ysb[:, half * 512:(half + 1) * 512], py[:], AF.Square, [Rpy], [Rysb, Rsm],
                            accum_out=small[:, 56 + half:57 + half])
                        pys.append((py, Rpy))
                    tt("dve", small[:, 58:59], small[:, 56:57], small[:, 57:58], ALU.add, [Rsm], [Rsm])
                    rsqrt(small[:, 58:59], small[:, 58:59], 1.0 / D, [Rsm], [Rsm])
                    for half in range(2):
                        py, Rpy = pys[half]
                        hs = slice(half * 512, (half + 1) * 512)
                        stt("dve", ysb[:, hs], py[:], small[:, 58:59], G2[:, hs], ALU.mult, ALU.mult,
                            [Rpy, Rsm, RG], [Rysb])
                        tt("pool", xt[j][:, hs], xt[j][:, hs], ysb[:, hs], ALU.add, [Rxt[j], Rysb], [Rxt[j]])
                    S.dma("sp", xdv[:, J, :], xt[j], reads=[Rxt[j]])
            S.barrier()
            xsrc = out_d
        S.barrier()
        S.emit()
    return nc


def prep_shared(inp):
    f = lambda a: np.ascontiguousarray(np.asarray(a, dtype=np.float32))
    perm = _perm()
    sh = {}
    sh["w_in"] = f(np.asarray(inp["w_in"])[:, :, perm])
    sh["w_o"] = f(inp["w_o"])
    sh["w_up"] = f(inp["w_up"])
    sh["w_down"] = f(inp["w_down"])
    sh["w_mod"] = f(inp["w_mod"])
    cw = np.asarray(inp["conv_w"])
    sh["convw"] = f(cw.reshape(DEPTH, 3, 2, 128).transpose(0, 3, 2, 1))
    sh["cmp_w1"] = f(inp["cmp_w1"])
    sh["cmp_w2"] = f(inp["cmp_w2"])
    cpz = np.asarray(inp["cmp_pos"])
    sh["cmp_posT"] = f(cpz.transpose(0, 1, 3, 2).reshape(DEPTH, 128, 32))
    sh["gw2b"] = f(np.concatenate([np.asarray(inp["gla_gate_w2"]), np.asarray(inp["gla_gate_b"])[:, None, :]], axis=1))
    sh["gmlp_wsT"] = f(np.asarray(inp["gmlp_ws"]).transpose(0, 3, 1, 2))
    sh["gmlp_bT"] = f(np.asarray(inp["gmlp_b"]).transpose(0, 2, 1))
    sh["gaincol"] = f(np.asarray(inp["grp_gain"]).reshape(DEPTH, 8, 128).transpose(0, 2, 1))
    ng = np.asarray(inp["norm_g"])
    sh["normg_col"] = f(ng.reshape(DEPTH, 4, 8, 128).transpose(0, 3, 1, 2))
    sh["normg_row"] = f(ng)
    bm = np.asarray(inp["b_mod"])
    sh["bmod_col"] = f(bm.reshape(DEPTH, 48, 128).transpose(0, 2, 1))
    sh["bmod_row"] = f(bm)
    for k, v in _consts().items():
        sh["c_" + k] = f(v)
    return sh


def core_inputs(inp, b, sh):
    d = dict(sh)
    d["x"] = np.ascontiguousarray(np.asarray(inp["x"][b], dtype=np.float32))
    d["ccol"] = np.ascontiguousarray(np.asarray(inp["c"][b], dtype=np.float32).reshape(8, 128).T)
    d["pos"] = np.ascontiguousarray(np.asarray(inp["positions"][b], dtype=np.int32).reshape(1, S_))
    return d


def kernel(**inputs):
    sh = prep_shared(inputs)
    nc = build()
    in_maps = [core_inputs(inputs, b, sh) for b in range(8)]
    res = run_bass_kernel_spmd(nc, in_maps, core_ids=list(range(8)))
    return np.stack([np.asarray(r["out"], dtype=np.float32) for r in res.results], axis=0)
```

```python
from contextlib import ExitStack
import numpy as np
import concourse.bass as bass
import concourse.mybir as mybir
from concourse.bass_utils import run_bass_kernel_spmd

F32 = mybir.dt.float32
BF16 = mybir.dt.bfloat16
I32 = mybir.dt.int32
ALU = mybir.AluOpType
AF = mybir.ActivationFunctionType
AX = mybir.AxisListType

D = 1024
S_ = 4096
DEPTH = 4
DFF = 4096
NT = S_ // 128
NB = S_ // 512
EPS = 1e-6
NEG = -30000.0
NCOL = 2972
FM_W = [128] * 15 + [16]
C_AH, C_AB, C_AC, C_Q, C_KVC, C_KS, C_KW, C_LQ, C_LK, C_LR = 0, 2, 4, 6, 10, 11, 12, 13, 14, 15
TM0 = 1936


class Res:
    __slots__ = ("w", "rs", "excl")

    def __init__(self, excl=False):
        self.w = None
        self.rs = {}
        self.excl = excl


class Sched:
    CE = ("pe", "act", "dve", "pool")

    def __init__(self, nc, stack, n_dma_sems=(("sp", 24), ("pool", 16), ("act", 4))):
        self.nc = nc
        self.prog = {k: [] for k in ("pe", "act", "dve", "pool", "sp")}
        self.semobj = {}
        self.cnt = {}
        self.ckey = {}
        self.keyeng = {}
        self.stack = stack
        self.epoch = 0
        for k in self.CE:
            self.semobj[k] = stack.enter_context(nc.semaphore("s_" + k))
            self.cnt[k] = 0
            self.ckey[k] = k
            self.keyeng[k] = k
        self.dq, self.dval, self.dnext = {}, {}, {}
        for q, n in n_dma_sems:
            keys = []
            for i in range(n):
                key = f"d_{q}{i}"
                self.semobj[key] = stack.enter_context(nc.semaphore(key))
                self.dval[key] = 0
                keys.append(key)
            self.dq[q] = keys
            self.dnext[q] = 0
        self.seen = {k: {} for k in self.prog}
        self.n_ops = 0

    def _wait(self, e, dep):
        key, val = dep
        if self.seen[e].get(key, 0) >= val:
            return
        self.seen[e][key] = val
        s = self.semobj[key]
        self.prog[e].append(lambda eng, s=s, v=val: eng.wait_ge(s, v))

    def _deps(self, e, reads, writes, is_dma):
        for r in reads:
            if r.w is not None:
                self._wait(e, r.w)
            if r.excl:
                for k, v in r.rs.items():
                    if self.keyeng.get(k) != e:
                        self._wait(e, (k, v))
        for w in writes:
            if w.w is not None and (is_dma or self.keyeng.get(w.w[0]) != e):
                self._wait(e, w.w)
            for k, v in w.rs.items():
                if is_dma or self.keyeng.get(k) != e:
                    self._wait(e, (k, v))

    def _commit(self, rec, reads, writes):
        for r in reads:
            if r.rs.get(rec[0], 0) < rec[1]:
                r.rs[rec[0]] = rec[1]
        for w in writes:
            w.w = rec
            w.rs = {}

    def op(self, e, fn, reads=(), writes=()):
        self._deps(e, reads, writes, False)
        self.cnt[e] += 1
        rec = (self.ckey[e], self.cnt[e])
        s = self.semobj[self.ckey[e]]
        self.prog[e].append(lambda eng, f=fn, s=s: f(eng).then_inc(s, 1))
        self._commit(rec, reads, writes)
        self.n_ops += 1
        return rec

    def dma(self, q, out, in_, reads=(), writes=(), **kw):
        self._deps(q, reads, writes, True)
        keys = self.dq[q]
        key = keys[self.dnext[q] % len(keys)]
        self.dnext[q] += 1
        prev = self.dval[key]
        if prev > 0:
            self._wait(q, (key, prev))
        tgt = prev + 16
        self.dval[key] = tgt
        s = self.semobj[key]
        self.prog[q].append(
            lambda eng, o=out, i=in_, s=s, kw=kw: eng.dma_start(out=o, in_=i, **kw).then_inc(s, 16))
        rec = (key, tgt)
        self._commit(rec, reads, writes)
        self.n_ops += 1
        return rec

    def barrier(self):
        for e in self.prog:
            for k in self.CE:
                if self.cnt[k] > 0:
                    self._wait(e, (self.ckey[k], self.cnt[k]))
            for key, v in self.dval.items():
                if v > 0:
                    self._wait(e, (key, v))

    def new_epoch(self):
        self.barrier()
        self.epoch += 1
        for k in self.CE:
            key = f"{k}@{self.epoch}"
            self.semobj[key] = self.stack.enter_context(self.nc.semaphore(f"s_{k}_{self.epoch}"))
            self.ckey[k] = key
            self.keyeng[key] = k
            self.cnt[k] = 0

    def emit(self):
        nc = self.nc
        with nc.Block() as block:
            @block.tensor
            def _(eng):
                for f in self.prog["pe"]:
                    f(eng)

            @block.scalar
            def _(eng):
                for f in self.prog["act"]:
                    f(eng)

            @block.vector
            def _(eng):
                for f in self.prog["dve"]:
                    f(eng)

            @block.gpsimd
            def _(eng):
                for f in self.prog["pool"]:
                    f(eng)

            @block.sync
            def _(eng):
                for f in self.prog["sp"]:
                    f(eng)


class Arena:
    def __init__(self, tile, nbytes, elsize):
        self.tile, self.nbytes, self.elsize = tile, nbytes, elsize
        self.off = 0

    def reset(self, off=0):
        self.off = off

    def get(self, shape, dt):
        es = 4 if dt in (F32, I32) else 2
        n = int(np.prod(shape[1:]))
        nb = (n * es + 31) // 32 * 32
        assert self.off + nb <= self.nbytes, f"arena overflow {self.off}+{nb}>{self.nbytes}"
        a, b = self.off // self.elsize, (self.off + nb) // self.elsize
        self.off += nb
        v = self.tile[:, a:b]
        if dt != self.tile.dtype:
            v = v.bitcast(dt)
        v = v[:, 0:n]
        if len(shape) == 3:
            v = v.rearrange("p (a b) -> p a b", a=shape[1])
        elif len(shape) == 4:
            v = v.rearrange("p (a b c) -> p a b c", a=shape[1], b=shape[2])
        if shape[0] < 128:
            v = v[0:shape[0]]
        return v


def _consts():
    c = {}
    kp = np.arange(128)[:, None]
    tq = np.arange(512)[None, :]
    masks = np.zeros((128, 12, 512), np.float32)
    for i in range(4):
        masks[:, i, :] = np.where(kp + 128 * i <= tq, 0.0, NEG)
    for n, i in enumerate(range(-4, 0)):
        masks[:, 4 + n, :] = np.where(tq < kp + 128 * i + 512, 0.0, NEG)
    for pat in range(4):
        masks[:, 8 + pat, :] = np.where(16 * kp + 15 - 512 * pat <= tq, 0.0, NEG)
    c["masks"] = masks
    c["ident"] = np.eye(128, dtype=np.float32)
    pm = np.zeros((128, 128), np.float32)
    for m in range(8):
        pm[m + 8, m] = 1.0
        pm[m, m + 8] = 1.0
    c["pm"] = pm
    inv_freq = (500000.0 ** (-np.arange(0, 16, 2, dtype=np.float32) / 16)).astype(np.float32)
    rc = np.zeros((128, 2), np.float32)
    rc[:16, 0] = np.concatenate([inv_freq, inv_freq])
    rc[:8, 1] = -1.0
    rc[8:16, 1] = 1.0
    c["ropec"] = rc
    ew = np.zeros((64, 4096), np.float32)
    for jb in range(64):
        ew[jb, jb * 64:(jb + 1) * 64] = 1.0
    c["ew"] = ew
    mo = np.zeros((256, 64), np.float32)
    for s in range(1, 256):
        n = s - 1
        cs = n * 16
        for j in range(64):
            ov = min(cs + 32, j * 64 + 64) - max(cs, j * 64)
            mo[s, j] = max(ov, 0) / 16.0
    c["mo"] = mo.reshape(2, 128, 64).transpose(1, 0, 2).copy()
    wd = np.zeros((128, 127), np.float32)
    cl = (np.arange(128) // 64)[:, None]
    m = (np.arange(127) - 62)[None, :]
    wd = np.where(m > cl, -1e30, np.where(m == cl, 2e4, np.where(m == cl - 1, 1e4, 0.0))).astype(np.float32)
    c["wd"] = wd
    s = np.arange(128)[:, None]
    t = np.arange(128)[None, :]
    same = (s // 64) == (t // 64)
    c["tinc"] = np.where(same & (s <= t), -1.0 / 16, 0.0).astype(np.float32)
    c["trev"] = np.where(same & (s > t), -1.0 / 16, 0.0).astype(np.float32)
    c["glam"] = np.where(same & (s <= t), 1.0, 0.0).astype(np.float32)
    c["triu"] = np.where(s <= t, 1.0, 0.0).astype(np.float32)
    p = np.arange(128)[:, None]
    c["bds"] = ((p // 32) == (np.arange(256)[None, :] // 64)).astype(np.float32)
    c["bdq"] = np.repeat(((p // 32) == np.arange(4)[None, :]).astype(np.float32)[:, :, None], 128, axis=2).copy()
    c["ones"] = np.ones((128, 128), np.float32)
    return c


def _perm():
    r = lambda a, b: list(range(a, b))
    cols = []
    cols += r(0, 256) + r(256, 512) + r(512, 768)
    for h in range(4):
        cols += r(768 + 64 * h, 832 + 64 * h) * 2
    cols += r(1024, 1152)
    cols += r(1152, 1216) * 2
    cols += r(1280, 1344) * 2
    cols += r(1420, 1548)
    cols += r(1548, 1676)
    cols += r(2188, 2204)
    assert len(cols) == TM0
    cols += r(1216, 1280) + r(1344, 1408) + r(1408, 1420)
    cols += r(1548, 1676) + r(1676, 1932)
    cols += r(1932, 2188) + r(2204, 2460)
    cols += r(2460, 2716)
    return np.array(cols, np.int64)


NCOL = TM0 + 140 + 384 + 512 + 256
FM_OFF = [128 * i for i in range(15)] + [1920]
T_OFF = [TM0, TM0 + 140, TM0 + 524, TM0 + 1036]
T_W = [140, 384, 512, 256]


def build(n_layers=DEPTH, taps=(), stop=None):
    nc = bass.Bass("TRN2", target_bir_lowering=False)
    CST = _consts()

    def din(name, shape, dt=F32):
        return nc.dram_tensor(name, list(shape), dt, kind="ExternalInput").ap()

    x_in = din("x", [S_, D])
    ccol_in = din("ccol", [128, 8])
    pos_in = din("pos", [1, S_], I32)
    w_in_d = din("w_in", [DEPTH, D, NCOL])
    w_o_d = din("w_o", [DEPTH, D, D])
    w_up_d = din("w_up", [DEPTH, D, DFF])
    w_dn_d = din("w_down", [DEPTH, DFF, D])
    w_mod_d = din("w_mod", [DEPTH, D, 6 * D])
    convw_d = din("convw", [DEPTH, 128, 2, 3])
    w1_d = din("cmp_w1", [DEPTH, 2, 2048, 128])
    w2_d = din("cmp_w2", [DEPTH, 2, 128, 64])
    posT_d = din("cmp_posT", [DEPTH, 128, 32])
    gw2b_d = din("gw2b", [DEPTH, 17, 128])
    gws_d = din("gmlp_wsT", [DEPTH, 128, 4, 128])
    gb_d = din("gmlp_bT", [DEPTH, 128, 4])
    gain_d = din("gaincol", [DEPTH, 128, 8])
    ngc_d = din("normg_col", [DEPTH, 128, 4, 8])
    ngr_d = din("normg_row", [DEPTH, 4, D])
    bmc_d = din("bmod_col", [DEPTH, 128, 48])
    bmr_d = din("bmod_row", [DEPTH, 6 * D])
    cst_d = {k: din("c_" + k, v.shape) for k, v in CST.items()}
    out_d = nc.dram_tensor("out", [S_, D], F32, kind="ExternalOutput").ap()
    xa_d = nc.dram_tensor("xa", [S_, D], F32).ap()
    ropeC_d = nc.dram_tensor("ropeC", [128, S_], F32).ap()
    ropeS_d = nc.dram_tensor("ropeS", [128, S_], F32).ap()
    tap_d = {}
    for name, shape in taps:
        tap_d[name] = nc.dram_tensor("tap_" + name, list(shape), F32, kind="ExternalOutput").ap()

    xview = lambda ap: ap.rearrange("(n p) d -> p n d", p=128)

    with ExitStack() as st:
        S = Sched(nc, st)
        build.last_sched = S
        sbt = lambda name, shape, dt: st.enter_context(nc.sbuf_tensor("sb_" + name, shape, dt))
        WB_t = sbt("WB", [128, 65536], BF16)
        PA_t = sbt("PA", [128, 14848], F32)
        WB = Arena(WB_t, 131072, 2)
        PA = Arena(PA_t, 59392, 4)

        def BUF(shape, dt):
            es = 4 if dt in (F32, I32) else 2
            nb = (int(np.prod(shape[1:])) * es + 31) // 32 * 32
            if WB.off + nb <= WB.nbytes:
                return WB.get(shape, dt)
            return PA.get(shape, dt)
        ps = [st.enter_context(nc.psum_tensor(f"ps{i}", [128, 512], F32)) for i in range(8)]
        psR = [Res(excl=True) for _ in range(8)]

        class Pool:
            def __init__(self, ids):
                self.ids, self.i = ids, 0

            def get(self):
                b = self.ids[self.i % len(self.ids)]
                self.i += 1
                return ps[b], psR[b]
        pMM = Pool([0, 1, 2])
        pACC = Pool([3, 4])
        pAUX = Pool([5, 6, 7])

        def mm(out, lhsT, rhs, start, stop, r, w, **kw):
            S.op("pe", lambda e: e.matmul(out, lhsT, rhs, start=start, stop=stop, **kw), r, w)

        def tr(out, in_, ident, r, w):
            S.op("pe", lambda e: e.transpose(out, in_, ident), r, w)

        def act(out, in_, func, r, w, **kw):
            S.op("act", lambda e: e.activation(out, in_, func, **kw), r, w)

        def ts(eng, out, in0, s1, s2, op0, op1, r, w):
            if op1 is None:
                S.op(eng, lambda e: e.tensor_scalar(out, in0, s1, None, op0), r, w)
            else:
                S.op(eng, lambda e: e.tensor_scalar(out, in0, s1, s2, op0, op1), r, w)

        def stt(eng, out, in0, sc, in1, op0, op1, r, w):
            S.op(eng, lambda e: e.scalar_tensor_tensor(out, in0, sc, in1, op0, op1), r, w)

        def tt(eng, out, in0, in1, op, r, w):
            S.op(eng, lambda e: e.tensor_tensor(out, in0, in1, op), r, w)

        def cp(eng, out, in_, r, w):
            if eng == "act":
                S.op("act", lambda e: e.copy(out, in_), r, w)
            else:
                S.op(eng, lambda e: e.tensor_copy(out, in_), r, w)

        def red(out, in_, r, w, op=ALU.add):
            S.op("dve", lambda e: e.tensor_reduce(out, in_, AX.X, op), r, w)

        def recip(out, in_, r, w):
            S.op("dve", lambda e: e.reciprocal(out, in_), r, w)

        def memset(eng, ap, v, w):
            S.op(eng, lambda e: e.memset(ap, v), (), w)

        def rsqrt(out, in_, scale, r, w):
            act(out, in_, AF.Sqrt, r, w, bias=EPS, scale=scale)
            recip(out, out, w, w)

        def tap(name, src_ap, res, dst_slice=None):
            if name in tap_d:
                dst = tap_d[name] if dst_slice is None else dst_slice(tap_d[name])
                S.dma("pool", dst, src_ap, reads=res)

        masks = sbt("masks", [128, 12, 512], BF16); Rc = Res()
        ident = sbt("ident", [128, 128], BF16)
        pm = sbt("pm", [128, 128], BF16)
        ones = sbt("ones", [128, 128], BF16)
        tinc = sbt("tinc", [128, 128], BF16)
        trev = sbt("trev", [128, 128], BF16)
        glam = sbt("glam", [128, 128], BF16)
        triu = sbt("triu", [128, 128], F32)
        bds = sbt("bds", [128, 256], BF16)
        bdq = sbt("bdq", [128, 4, 128], BF16)
        wd = sbt("wd", [128, 127], F32)
        ropec = sbt("ropec", [128, 2], F32)
        mo = sbt("mo", [128, 2, 64], BF16)
        for t_, k in ((masks, "masks"), (ident, "ident"), (pm, "pm"), (ones, "ones"), (tinc, "tinc"),
                      (trev, "trev"), (glam, "glam"), (bds, "bds"), (bdq, "bdq"), (mo, "mo")):
            S.dma("pool", t_[:], cst_d[k], writes=[Rc])
        for t_, k in ((triu, "triu"), (wd, "wd"), (ropec, "ropec")):
            S.dma("sp", t_[:], cst_d[k], writes=[Rc])
        condT = sbt("condT", [128, 8], BF16)
        cond_bc = sbt("cond_bc", [128, 8, 128], BF16)
        modcol = sbt("modcol", [128, 48], F32); Rmod = Res()
        A1 = sbt("A1", [128, 8], F32)
        A2 = sbt("A2", [128, 8], F32)
        ngc = sbt("ngc", [128, 4, 8], F32)
        bmc = sbt("bmc", [128, 48], F32)
        gainc = sbt("gainc", [128, 8], F32)
        convw = sbt("convw", [128, 2, 3], F32)
        gbT = sbt("gbT", [128, 4], F32)
        c1col = sbt("c1col", [128, 2], F32)
        small = sbt("small", [128, 64], F32); Rsm = Res()
        Rw = Res()

        PA.reset()
        posi = PA.get([128, 512], I32)
        ang = PA.get([128, 512], F32)
        kf = PA.get([128, 512], F32)
        tS = PA.get([128, 512], F32)
        tC = PA.get([128, 512], F32)
        Rs = Res()
        TWO_PI = float(2 * np.pi)
        for pc in range(8):
            cs = slice(pc * 512, (pc + 1) * 512)
            S.dma("sp", posi, pos_in[:, cs].partition_broadcast(128), writes=[Rs])
            cp("dve", ang, posi, [Rs], [Rs])
            ts("dve", ang, ang, ropec[:, 0:1], None, ALU.mult, None, [Rs, Rc], [Rs])
            ts("dve", kf, ang, 1.0 / TWO_PI, None, ALU.mult, None, [Rs], [Rs])
            cp("dve", posi, kf, [Rs], [Rs])
            cp("dve", kf, posi, [Rs], [Rs])
            stt("dve", ang, kf, -TWO_PI, ang, ALU.mult, ALU.add, [Rs], [Rs])
            ts("dve", ang, ang, 3.1415925, -3.1415925, ALU.min, ALU.max, [Rs], [Rs])
            act(tS, ang, AF.Sin, [Rs], [Rs])
            ts("dve", tS, tS, ropec[:, 1:2], None, ALU.mult, None, [Rs, Rc], [Rs])
            stt("dve", kf, ang, -1.0, ang, ALU.mult, ALU.max, [Rs], [Rs])
            act(tC, kf, AF.Sin, [Rs], [Rs], scale=-1.0, bias=float(np.pi / 2))
            S.dma("sp", ropeS_d[:, cs], tS, reads=[Rs])
            S.dma("sp", ropeC_d[:, cs], tC, reads=[Rs])
        ccol = PA.get([128, 8], F32)
        S.dma("sp", ccol, ccol_in, writes=[Rs])
        act(condT[:], ccol, AF.Silu, [Rs], [Rmod])
        cp("dve", cond_bc[:], condT[:].unsqueeze(2).to_broadcast([128, 8, 128]), [Rmod], [Rmod])
        Rrope = Res()
        S.barrier()

        def done():
            S.barrier()
            S.emit()
            return nc
        if stop == "setup":
            return done()

        xsrc = x_in
        for l in range(n_layers):
            if l > 0:
                S.new_epoch()
            xmid = xa_d
            xdst = out_d
            PA.reset()
            WB.reset()
            G1 = PA.get([128, 1024], F32)
            G2 = PA.get([128, 1024], F32)
            RG = Res()
            PA_base = PA.off
            wm = [WB.get([128, 8, 512], BF16) for _ in range(2)]
            Rwm = [Res(), Res()]
            brow = WB.get([128, 1024], F32)
            grow = WB.get([128, 1024], F32)
            Rrow = Res()
            for t_, src in ((ngc, ngc_d[l]), (bmc, bmc_d[l]), (gainc, gain_d[l]), (convw, convw_d[l]),
                            (gbT, gb_d[l])):
                S.dma("sp", t_[:], src, writes=[Rw])
            pcol, Rpcol = pAUX.get()
            wmv = w_mod_d[l].rearrange("(k p) n -> p k n", p=128)
            for pc in range(12):
                b = pc % 2
                S.dma("pool", wm[b], wmv[:, :, pc * 512:(pc + 1) * 512], writes=[Rwm[b]])
                for jj in range(4):
                    col = pc * 4 + jj
                    for k in range(8):
                        mm(pcol[:, col:col + 1], wm[b][:, k, jj * 128:(jj + 1) * 128], condT[:, k:k + 1],
                           k == 0, k == 7, [Rwm[b], Rmod], [Rpcol])
                if pc in (4, 5, 10, 11):
                    Gt = G1 if pc < 6 else G2
                    half = pc % 2
                    hs = slice(half * 512, (half + 1) * 512)
                    gi = 1 if pc < 6 else 3
                    pg, Rpg = pMM.get()
                    for k in range(8):
                        mm(pg[:], cond_bc[:, k, :], wm[b][:, k, :], k == 0, k == 7, [Rwm[b], Rmod], [Rpg])
                    if half == 0:
                        S.dma("sp", brow, bmr_d[l:l + 1, (pc // 2) * 1024:(pc // 2) * 1024 + 1024].partition_broadcast(128),
                              writes=[Rrow])
                        S.dma("sp", grow, ngr_d[l, gi:gi + 1, :].partition_broadcast(128), writes=[Rrow])
                    tt("dve", Gt[:, hs], pg[:], brow[:, hs], ALU.add, [Rpg, Rrow], [RG])
                    tt("dve", Gt[:, hs], Gt[:, hs], grow[:, hs], ALU.mult, [RG, Rrow], [RG])
            tt("dve", modcol[:], pcol[:, 0:48], bmc[:], ALU.add, [Rpcol, Rw], [Rmod])
            stt("dve", A1[:], modcol[:, 8:16], 1.0, ngc[:, 0, :], ALU.add, ALU.mult, [Rmod, Rw], [Rmod])
            stt("dve", A2[:], modcol[:, 32:40], 1.0, ngc[:, 2, :], ALU.add, ALU.mult, [Rmod, Rw], [Rmod])
            sh1 = modcol[:, 0:8]
            sh2 = modcol[:, 24:32]
            S.barrier()
            if stop == "mods":
                return done()

            WB.reset()
            PA.reset(PA_base)
            w_in = WB.get([128, 8, NCOL], BF16)
            w_o = WB.get([128, 8, D], BF16)
            RW1i = [Res() for _ in range(8)]
            RW1o = [Res() for _ in range(4)]
            wiv = w_in_d[l].rearrange("(k p) n -> p k n", p=128)
            for k in range(8):
                S.dma("pool", w_in[:, k, :], wiv[:, k, :], writes=[RW1i[k]])
            wov = w_o_d[l].rearrange("(k p) n -> p k n", p=128)
            for k in range(0, 8, 2):
                S.dma("pool", w_o[:, k:k + 2, :], wov[:, k:k + 2, :], writes=[RW1o[k // 2]])
            ksA = PA.get([128, 4096], BF16)
            kwT = PA.get([128, 1024], BF16)
            vs1 = PA.get([128, 32, 65], BF16)
            vw1 = PA.get([128, 8, 65], BF16)
            kvc = PA.get([128, 528], BF16)
            kcT = PA.get([128, 256], BF16)
            hTv = PA.get([128, 256], BF16)
            vcM = PA.get([128, 2, 128], BF16)
            W1 = PA.get([128, 32, 128], BF16)
            w2k = PA.get([128, 64], BF16)
            w2v = PA.get([128, 64], BF16)
            posT = PA.get([128, 32], BF16)
            gwT = PA.get([128, 4, 128], BF16)
            gw2b = PA.get([32, 128], BF16)
            Sst = PA.get([128, 256], F32)
            zbuf = [PA.get([128, 514], F32) for _ in range(2)]
            lr1T = PA.get([32, 512], BF16)
            ropeCt = PA.get([128, 512], F32)
            ropeSt = PA.get([128, 512], F32)
            Rks, Rkw, Rvs, Rvw, Rkvc, Rkc, RhTv, RvcM, RS, Rlr, Rrt = (Res() for _ in range(11))
            Rz = [Res(), Res()]
            xt = [BUF([128, 1024], F32)]; Rxt = [Res()]
            xs = BUF([128, 4, 1024], BF16); Rxs = Res()
            mixT = xs.rearrange("p a b -> p (a b)").rearrange("p (k t) -> p k t", k=8)
            hT = BUF([128, 8, 512], BF16); RhT = Res()
            QA = [BUF([128, 512], BF16) for _ in range(4)]; RQA = [Res() for _ in range(4)]
            qraw = BUF([128, 512], BF16); Rqr = Res()
            kraw = BUF([128, 512], BF16); Rkr = Res()
            pT = [BUF([128, 512], BF16) for _ in range(3)]; RpT = [Res() for _ in range(3)]
            cvy = BUF([128, 512], F32); Rcvy = Res()
            cvo12 = BUF([128, 1024], F32)
            cvo = [cvo12[:, 0:512], cvo12[:, 512:1024]]; Rcvo = [Res(), Res()]
            glq = BUF([128, 512], F32); glk = BUF([128, 512], F32); Rgl = Res()
            gts = BUF([128, 4, 12], F32); Rgts = Res()
            nrm = BUF([128, 4, 768], BF16); Rnrm = Res()
            nsa = BUF([128, 4, 256], F32); Rnsa = Res()
            pslc = BUF([128, 4, 64], F32); Rpslc = Res()
            selb = BUF([128, 128], BF16); Rselb = Res()
            t12 = BUF([128, 1024], F32)
            t1 = t12[:, 0:512]; t2 = t12[:, 512:1024]; Rt1 = Res(); Rt2 = Res()
            gwT32 = t1.rearrange("p (a b) -> p a b", a=4)
            g_e = BUF([128, 128], F32); g_lsp = BUF([128, 128], BF16)
            g_b = BUF([128, 128], F32); g_tmp = g_e
            g_e1 = BUF([128, 128], F32); g_e2 = BUF([128, 128], F32); g_e3 = g_e1
            g_qe = BUF([128, 128], BF16); g_ke = BUF([128, 128], BF16); g_qb = BUF([128, 128], BF16)
            g_qeb = BUF([128, 4, 128], BF16); g_ed = BUF([128, 128], F32); g_kd = BUF([128, 128], BF16)
            g_att = BUF([128, 4, 128], BF16); g_sbf = [BUF([128, 256], BF16) for _ in range(2)]
            g_kt = BUF([128, 128], F32); g_lv = BUF([128, 256], BF16); g_lg = BUF([128, 256], F32)
            g_sq = BUF([128, 256], F32); g_o = BUF([128, 256], F32)
            m_u = BUF([128, 256], F32); m_v = BUF([128, 256], F32); m_sq = g_sq
            m_vln = BUF([128, 256], BF16)
            Rg = Res()
            Rs1, RsG, RsA, Rs7 = Res(), Res(), Res(), Res()
            xbufs = [(xt[0], [Rxt[0]]), (t12, [Rt1, Rt2]), (nsa.rearrange("p a b -> p (a b)"), [Rnsa]),
                     (cvo12, [Rcvo[0], Rcvo[1]])]
            pY = Pool([3, 4, 5, 6, 7])
            ysb = BUF([128, 1024], F32); Rysb = Res()
            sqc = [BUF([128, 512], BF16) for _ in range(2)]; Rsqc = Res()
            S.dma("pool", ksA[64:128, :], cst_d["ew"], writes=[Rks])
            S.dma("pool", W1[0:64], w1_d[l, 0].rearrange("(p d) h -> d p h", d=64), writes=[Rw])
            S.dma("pool", W1[64:128], w1_d[l, 1].rearrange("(p d) h -> d p h", d=64), writes=[Rw])
            S.dma("pool", w2k, w2_d[l, 0], writes=[Rw])
            S.dma("pool", w2v, w2_d[l, 1], writes=[Rw])
            S.dma("pool", posT, posT_d[l], writes=[Rw])
            S.dma("pool", gw2b[0:17, :], gw2b_d[l], writes=[Rw])
            S.dma("sp", gwT32, gws_d[l], writes=[Rt1])
            tt("dve", gwT, gwT32, triu[:].unsqueeze(1).to_broadcast([128, 4, 128]), ALU.mult, [Rt1, Rc], [Rw])
            memset("dve", vs1[:, :, 64:65], 1.0, [Rvs])
            memset("dve", vw1[:, :, 64:65], 1.0, [Rvw])
            memset("dve", kvc[:, 0:16], 0.0, [Rkvc])
            memset("dve", kcT, 0.0, [Rkc])
            memset("dve", kwT[64:128, :], 0.0, [Rkw])
            memset("dve", hTv, 0.0, [RhTv])
            memset("dve", Sst, 0.0, [RS])
            memset("dve", lr1T, 1.0, [Rlr])
            for c2 in range(2):
                memset("dve", zbuf[c2][:, 0:2], 0.0, [Rz[c2]])
            cp("dve", vcM[:, :, 64:128], mo[:], [Rc], [RvcM])
            for jkv in range(2):
                pc1, Rpc1 = pAUX.get() if jkv == 0 else pACC.get()
                rs_ = slice(64 * jkv, 64 * jkv + 64)
                for p_ in range(32):
                    mm(pc1[:, 0:1], W1[rs_, p_, :], posT[rs_, p_:p_ + 1], p_ == 0, p_ == 31, [Rw], [Rpc1])
                cp("dve", c1col[:, jkv:jkv + 1], pc1[:, 0:1], [Rpc1], [Rw])
            if stop == "p1init":
                return done()

            xsv = xview(xsrc)
            xmv = xview(xmid)
            for tb in range(NB):
                bc = slice(tb * 512, (tb + 1) * 512)
                S.dma("sp", ropeCt, ropeC_d[:, bc], writes=[Rrt])
                S.dma("sp", ropeSt, ropeS_d[:, bc], writes=[Rrt])
                Rsm = Rs1
                for j in range(4):
                    S.dma("sp", xbufs[j][0], xsv[:, 4 * tb + j, :], writes=xbufs[j][1])
                for j in range(4):
                    xb, Rxb = xbufs[j]
                    act(xs[:, j, :], xb, AF.Square, Rxb, [Rxs, Rsm], accum_out=small[:, j:j + 1])
                    rsqrt(small[:, 4 + j:5 + j], small[:, j:j + 1], 1.0 / D, [Rsm], [Rsm])
                    ts("dve", xs[:, j, :], xb, small[:, 4 + j:5 + j], None, ALU.mult, None, Rxb + [Rsm], [Rxs])
                for k in range(8):
                    pb, Rpb = pMM.get()
                    pbv = pb[:].bitcast(BF16)
                    for j in range(4):
                        tr(pbv[:, j * 128:(j + 1) * 128], xs[:, j, k * 128:(k + 1) * 128], ident[:], [Rxs, Rc], [Rpb])
                    if k % 2 == 0:
                        act(hT[:, k, :], pbv[:, 0:512], AF.Identity, [Rpb, Rmod], [RhT],
                            scale=A1[:, k:k + 1], bias=sh1[:, k:k + 1])
                    else:
                        ts("dve", hT[:, k, :], pbv[:, 0:512], A1[:, k:k + 1], sh1[:, k:k + 1], ALU.mult, ALU.add,
                           [Rpb, Rmod], [RhT])
                if l == 0 and tb == 0:
                    tap("hT", hT.rearrange("p k t -> p (k t)"), [RhT])

                if stop == "b0s2":
                    return done()
                def fm(ch):
                    pb, Rpb = pMM.get()
                    w = FM_W[ch]
                    c0 = FM_OFF[ch]
                    for k in range(8):
                        mm(pb[0:w, :], w_in[:, k, c0:c0 + w], hT[:, k, :], k == 0, k == 7, [RW1i[k], RhT], [Rpb])
                    return pb, Rpb

                def tmc(ti, jc):
                    pb, Rpb = pMM.get()
                    for k in range(8):
                        mm(pb[:, 0:T_W[ti]], hT[:, k, jc], w_in[:, k, T_OFF[ti]:T_OFF[ti] + T_W[ti]],
                           k == 0, k == 7, [RW1i[k], RhT], [Rpb])
                    return pb, Rpb

                def rope(src_ps, Rsrc, raw, Rraw, dst, Rdst):
                    cp("act", raw[:], src_ps[:], [Rsrc], [Rraw])
                    p2, Rp2 = pMM.get()
                    mm(p2[:], pm[:], raw[:], True, True, [Rc, Rraw], [Rp2])
                    if stop == "q0c":
                        return "STOP"
                    tt("dve", t1, src_ps[:], ropeCt, ALU.mult, [Rsrc, Rrt], [Rt1])
                    if stop == "q0d":
                        return "STOP"
                    tt("dve", t2, p2[:], ropeSt, ALU.mult, [Rp2, Rrt], [Rt2])
                    if stop == "q0e":
                        return "STOP"
                    tt("dve", dst, t1[0:64], t2[0:64], ALU.add, [Rt1, Rt2], [Rdst])

                for c2 in range(2):
                    z = zbuf[c2]
                    pb, Rpb = fm(C_AH + c2)
                    cp("act", z[:, 2:514], pb[:], [Rpb], [Rz[c2]])
                    pb, Rpb = fm(C_AC + c2)
                    tt("dve", z[:, 2:514], pb[:], z[:, 2:514], ALU.mult, [Rpb, Rz[c2]], [Rz[c2]])
                    ts("pool", cvy, z[:, 2:514], convw[:, c2, 2:3], None, ALU.mult, None, [Rz[c2], Rw], [Rcvy])
                    stt("dve", cvy, z[:, 1:513], convw[:, c2, 1:2], cvy, ALU.mult, ALU.add, [Rz[c2], Rw, Rcvy], [Rcvy])
                    stt("dve", cvy, z[:, 0:512], convw[:, c2, 0:1], cvy, ALU.mult, ALU.add, [Rz[c2], Rw, Rcvy], [Rcvy])
                    cp("pool", z[:, 0:2], z[:, 512:514], [Rz[c2]], [Rz[c2]])
                    pb, Rpb = fm(C_AB + c2)
                    tt("dve", cvo[c2], pb[:], cvy, ALU.mult, [Rpb, Rcvy], [Rcvo[c2]])
                    act(sqc[c2], cvo[c2], AF.Square, [Rcvo[c2]], [Rsqc])
                if stop == "s3a":
                    return done()
                pb, Rpb = fm(C_KVC)
                cp("act", kvc[:, 16:528], pb[:], [Rpb], [Rkvc])
                if stop == "s3b1":
                    return done()
                phk, Rphk = pAUX.get()
                phv, Rphv = pACC.get()
                for jkv in range(2):
                    rs_ = slice(64 * jkv, 64 * jkv + 64)
                    for p_ in range(32):
                        mm((phk, phv)[jkv][:, 0:32], W1[rs_, p_, :], kvc[rs_, p_:p_ + 497:16], p_ == 0, p_ == 31,
                           [Rw, Rkvc], [(Rphk, Rphv)[jkv]])
                if stop == "s3b2":
                    return done()
                hk = t1[:, 0:16].bitcast(BF16)
                act(hk, phk[:, 0:32], AF.Silu, [Rphk, Rw], [Rt1], bias=c1col[:, 0:1])
                sl = slice(32 * tb, 32 * tb + 32)
                act(hTv[:, sl], phv[:, 0:32], AF.Silu, [Rphv, Rw], [RhTv], bias=c1col[:, 1:2])
                if tb == 0:
                    memset("dve", hTv[:, 0:1], 0.0, [RhTv])
                if stop == "s3b3":
                    return done()
                cp("pool", kvc[:, 0:16], kvc[:, 512:528], [Rkvc], [Rkvc])
                if stop == "s3b4":
                    return done()
                pk2, Rpk2 = pAUX.get()
                mm(pk2[0:64, 0:32], w2k, hk, True, True, [Rw, Rt1], [Rpk2])
                cch = tb // 4
                mm(pk2[:, 64:128], hTv[:, 128 * cch:128 * cch + 128], w2v, True, True, [RhTv, Rw], [Rpk2])
                cp("act", kcT[0:64, sl], pk2[0:64, 0:32], [Rpk2], [Rkc])
                cp("act", vcM[:, cch, 0:64], pk2[:, 64:128], [Rpk2], [RvcM])
                if stop == "s3b":
                    return done()
                for j in range(4):
                    J = 4 * tb + j
                    pb, Rpb = tmc(0, slice(j * 128, (j + 1) * 128))
                    cp("act", vs1[:, J, 0:64], pb[:, 0:64], [Rpb], [Rvs])
                    cp("act", vw1[:, J % 8, 0:64], pb[:, 64:128], [Rpb], [Rvw])
                    act(gts[:, j, :], pb[:, 128:140], AF.Sigmoid, [Rpb], [Rgts])
                if stop == "s3c":
                    return done()
                Rsm = RsA
                nch = 1 if tb < 4 else 2
                for h in range(4):
                    pb, Rpb = fm(C_Q + h)
                    if rope(pb, Rpb, qraw, Rqr, QA[h][0:64, :], RQA[h]) == "STOP":
                        return done()
                    if l == 0 and tb == 0 and h == 0:
                        tap("qa0", QA[0][0:64, :], [RQA[0]])
                        tap("qraw0", qraw[0:64, :], [Rqr])
                        tap("ropeC", ropeCt, [Rrt])
                        tap("ropeS", ropeSt, [Rrt])
                    if stop == "q1":
                        return done()
                    for c in range(nch):
                        pb, Rpb = pMM.get()
                        masked = (c == nch - 1)
                        mm(pb[:], kcT[:, 128 * c:128 * c + 128], qraw[:, :], True, not masked,
                           [Rkc, Rqr], [Rpb])
                        if masked:
                            mm(pb[:], ident[:], masks[:, 8 + (tb % 4), :], False, True, [Rc], [Rpb])
                        act(pT[c], pb[:], AF.Exp, [Rpb], [RpT[c]], scale=0.125)
                    if stop == "q2":
                        return done()
                    pacc, Rpacc = pACC.get()
                    pa3 = pacc[:].rearrange("p (j c) -> p j c", j=4)
                    for j in range(4):
                        for c in range(nch):
                            mm(pa3[:, j, :], pT[c][:, j * 128:(j + 1) * 128], vcM[:, c, :], c == 0, c == nch - 1,
                               [RpT[c], RvcM], [Rpacc])
                    if stop == "q3":
                        return done()
                    den = small[:, 32:36]
                    red(den, pa3[:, :, 64:128], [Rpacc], [Rsm])
                    ts("dve", den, den, 0.5, 1e-30, ALU.mult, ALU.max, [Rsm], [Rsm])
                    recip(den, den, [Rsm], [Rsm])
                    u3 = t1[:, 0:256].rearrange("p (j c) -> p j c", j=4)
                    tt("dve", u3, pa3[:, :, 64:128], den.unsqueeze(2).to_broadcast([128, 4, 64]), ALU.mult,
                       [Rpacc, Rsm], [Rt1])
                    if h == 0:
                        cp("pool", pslc, u3, [Rt1], [Rpslc])
                    else:
                        tt("pool", pslc, pslc, u3, ALU.add, [Rt1, Rpslc], [Rpslc])
                    tt("dve", small[:, 36:40], den, gts[:, :, 3 * h], ALU.mult, [Rsm, Rgts], [Rsm])
                    tt("dve", nsa[:, :, h * 64:(h + 1) * 64], pa3[:, :, 0:64],
                       small[:, 36:40].unsqueeze(2).to_broadcast([128, 4, 64]), ALU.mult, [Rpacc, Rsm], [Rnsa])
                if stop == "s3d":
                    return done()
                if l == 0:
                    for j in range(4):
                        tap("nsac", nsa[:, j, :], [Rnsa], lambda d, J=4 * tb + j: d[J * 128:(J + 1) * 128, :])
                pb, Rpb = fm(C_KS)
                rope(pb, Rpb, kraw, Rkr, ksA[0:64, bc], Rks)
                if l == 0 and tb == 0:
                    tap("ks0", ksA[0:64, 0:512], [Rks])
                pb, Rpb = fm(C_KW)
                rope(pb, Rpb, kraw, Rkr, kwT[0:64, (tb % 2) * 512:(tb % 2) * 512 + 512], Rkw)
                pb, Rpb = fm(C_LQ)
                cp("act", glq, pb[:], [Rpb], [Rgl])
                pb, Rpb = fm(C_LK)
                cp("act", glk, pb[:], [Rpb], [Rgl])
                pb, Rpb = fm(C_LR)
                cp("act", lr1T[0:16, :], pb[0:16, :], [Rpb], [Rlr])

                if stop == "b0s3":
                    return done()
                Rsm = RsA
                psl, Rpsl = pMM.get()
                pslv = psl[:].bitcast(BF16)
                for j in range(4):
                    J = 4 * tb + j
                    sc_ = t2[:, 0:64]
                    tt("dve", sc_, pslc[:, j, :], wd[:, 62 - 2 * J:126 - 2 * J], ALU.add, [Rpslc, Rc], [Rt2])
                    ts("dve", sc_[:, 0:1], sc_[:, 0:1], 3e4, None, ALU.add, None, [Rt2], [Rt2])
                    m8 = small[:, 40:56]
                    S.op("dve", lambda e, sc_=sc_, m8=m8: e.max(out=m8[:, 0:8], in_=sc_), [Rt2], [Rsm])
                    wk_ = t2[:, 64:128]
                    S.op("dve", lambda e, sc_=sc_, m8=m8, wk_=wk_: e.match_replace(
                        out=wk_, in_to_replace=m8[:, 0:8], in_values=sc_, imm_value=-3e38), [Rt2, Rsm], [Rt2])
                    S.op("dve", lambda e, m8=m8, wk_=wk_: e.max(out=m8[:, 8:16], in_=wk_), [Rt2], [Rsm])
                    if j == 0:
                        memset("dve", selb[:, 0:64], 0.0, [Rselb])
                    ts("dve", selb[:, 64:128], sc_, m8[:, 15:16], NEG, ALU.is_lt, ALU.mult, [Rt2, Rsm], [Rselb])
                    tr(pslv[:, j * 128:(j + 1) * 128], selb[:], ident[:], [Rselb, Rc], [Rpsl])
                for h in range(4):
                    cp("act" if h % 2 else "dve", QA[h][64:128, :], pslv[64:128, 0:512], [Rpsl], [RQA[h]])

                jobs = []
                for h in range(4):
                    jobs.append(dict(h=h, kts=list(range(0, 4 * tb + 4)), keyT=ksA, Rk=Rks, v1=vs1, Rv=Rvs,
                                     mask_of=(lambda kt: (kt - 4 * tb) if kt >= 4 * tb else None),
                                     rel_of=(lambda kt, j: kt <= 4 * tb + j), gcol=3 * h + 1, kmod=32))
                    jobs.append(dict(h=h, kts=list(range(max(0, 4 * tb - 4), 4 * tb + 4)), keyT=kwT, Rk=Rkw, v1=vw1, Rv=Rvw,
                                     mask_of=(lambda kt: (kt - 4 * tb) if kt >= 4 * tb else (4 + (kt - 4 * tb + 4))),
                                     rel_of=(lambda kt, j: (kt <= 4 * tb + j) and (kt >= 4 * tb + j - 4)),
                                     gcol=3 * h + 2, kmod=8))
                units = []
                for jb_ in jobs:
                    jb_["last"] = {}
                    for kt in jb_["kts"]:
                        for j in range(4):
                            if jb_["rel_of"](kt, j):
                                jb_["last"][j] = kt
                    jb_["started"] = False
                    for n, kt in enumerate(jb_["kts"]):
                        units.append((jb_, n, kt))
                LOOK = 2

                def emit_score(i):
                    jb_, n, kt = units[i]
                    h = jb_["h"]
                    js = [j for j in range(4) if jb_["rel_of"](kt, j)]
                    cs_ = slice(128 * js[0], 128 * (js[-1] + 1))
                    pb, Rpb = pMM.get()
                    mk = jb_["mask_of"](kt)
                    km = jb_["kmod"]
                    kc_ = slice((kt % km) * 128, (kt % km + 1) * 128)
                    mm(pb[:, cs_], jb_["keyT"][:, kc_], QA[h][:, cs_], True, mk is None, [jb_["Rk"], RQA[h]], [Rpb])
                    if mk is not None:
                        mm(pb[:, cs_], ident[:], masks[:, mk, cs_], False, True, [Rc], [Rpb])
                    act(pT[i % 3][:, cs_], pb[:, cs_], AF.Exp, [Rpb], [RpT[i % 3]], scale=0.125)

                def emit_pv(i):
                    jb_, n, kt = units[i]
                    h = jb_["h"]
                    if n == 0:
                        jb_["pacc"] = pACC.get()
                    pacc, Rpacc = jb_["pacc"]
                    pa3 = pacc[:, 0:260].rearrange("p (j c) -> p j c", j=4)
                    km = jb_["kmod"]
                    pt_ = pT[i % 3]
                    for j in range(4):
                        if jb_["rel_of"](kt, j):
                            mm(pa3[:, j, :], pt_[:, j * 128:(j + 1) * 128], jb_["v1"][:, kt % km, :],
                               not jb_["started"], kt == jb_["last"][j], [RpT[i % 3], jb_["Rv"]], [Rpacc],
                               skip_group_check=True)
                            jb_["started"] = True
                    if n == len(jb_["kts"]) - 1:
                        den = small[:, 32:36]
                        ts("dve", den, pa3[:, :, 64], 1e-30, None, ALU.max, None, [Rpacc], [Rsm])
                        recip(den, den, [Rsm], [Rsm])
                        tt("dve", small[:, 36:40], den, gts[:, :, jb_["gcol"]], ALU.mult, [Rsm, Rgts], [Rsm])
                        u3 = t1[:, 0:256].rearrange("p (j c) -> p j c", j=4)
                        tt("dve", u3, pa3[:, :, 0:64], small[:, 36:40].unsqueeze(2).to_broadcast([128, 4, 64]),
                           ALU.mult, [Rpacc, Rsm], [Rt1])
                        tt("pool", nsa[:, :, h * 64:(h + 1) * 64], nsa[:, :, h * 64:(h + 1) * 64], u3, ALU.add,
                           [Rt1, Rnsa], [Rnsa])

                def gen_B():
                    for j in range(4):
                        J = 4 * tb + j
                        yield
                        jc = slice(j * 128, (j + 1) * 128)
                        yield
                        pb, Rpb = tmc(1, jc)
                        yield
                        cp("dve", g_kt, pb[:, 0:128], [Rpb], [Rg])
                        yield
                        cp("dve", g_lv, pb[:, 128:384], [Rpb], [Rg])
                        yield
                        pb, Rpb = tmc(2, jc)
                        yield
                        act(g_lg, pb[:, 0:256], AF.Silu, [Rpb], [Rg])
                        yield
                        cp("dve", m_u, pb[:, 256:512], [Rpb], [Rg])
                        yield
                        pb, Rpb = tmc(3, jc)
                        yield
                        cp("dve", m_v, pb[:, 0:256], [Rpb], [Rg])
                        yield
                        pa, Rpa = pAUX.get()
                        yield
                        mm(pa[:, 0:128], lr1T[0:17, jc], gw2b[0:17, :], True, True, [Rlr, Rw], [Rpa])
                        yield
                        act(g_e, pa[:, 0:128], AF.Exp, [Rpa], [Rg], scale=-1.0)
                        yield
                        act(g_lsp, g_e, AF.Ln, [Rg], [Rg], bias=1.0)
                        yield
                        pbT, RpbT = pAUX.get()
                        yield
                        mm(pbT[:, 0:128], g_lsp, tinc[:], True, True, [Rg, Rc], [RpbT])
                        yield
                        mm(pbT[:, 128:256], trev[:], g_lsp, True, True, [Rg, Rc], [RpbT])
                        yield
                        cp("dve", g_b, pbT[:, 0:128], [RpbT], [Rg])
                        yield
                        gb3 = g_b.rearrange("p (c t) -> p c t", c=2)
                        yield
                        tt("dve", g_tmp.rearrange("p (c t) -> p c t", c=2), gb3, gb3[:, :, 32:33].to_broadcast([128, 2, 64]),
                           ALU.subtract, [Rg], [Rg])
                        yield
                        qsc = float(32 ** -0.5)
                        yield
                        act(g_e1, g_tmp, AF.Exp, [Rg], [Rg])
                        yield
                        act(g_e2, g_tmp, AF.Exp, [Rg], [Rg], scale=-1.0)
                        yield
                        stt("dve", g_qe, glq[:, jc], qsc, g_e1, ALU.mult, ALU.mult, [Rgl, Rg], [Rg])
                        yield
                        tt("dve", g_ke, glk[:, jc], g_e2, ALU.mult, [Rgl, Rg], [Rg])
                        yield
                        act(g_e3, g_b, AF.Exp, [Rg], [Rg])
                        yield
                        act(small[:, 8:10], gb3[:, :, 63], AF.Exp, [Rg], [RsG])
                        yield
                        act(g_ed, pbT[:, 128:256], AF.Exp, [RpbT], [Rg])
                        yield
                        stt("dve", g_qb, glq[:, jc], qsc, g_e3, ALU.mult, ALU.mult, [Rgl, Rg], [Rg])
                        yield
                        tt("dve", g_qeb, bdq[:], g_qe.unsqueeze(1).to_broadcast([128, 4, 128]), ALU.mult, [Rc, Rg], [Rg])
                        yield
                        tt("dve", g_kd, g_kt, g_ed, ALU.mult, [Rg], [Rg])
                        yield
                        patt, Rpatt = pMM.get()
                        yield
                        mm(patt[:], g_ke, g_qeb.rearrange("p a b -> p (a b)"), True, True, [Rg], [Rpatt])
                        yield
                        tt("dve", g_att, patt[:].rearrange("p (a b) -> p a b", a=4),
                           glam[:].unsqueeze(1).to_broadcast([128, 4, 128]), ALU.mult, [Rpatt, Rc], [Rg])
                        yield
                        pkvs = [pAUX.get(), pAUX.get()]
                        yield
                        for c in range(2):
                            rs_ = slice(64 * c, 64 * c + 64)
                            mm(pkvs[c][0][:, 0:256], g_kd[rs_, :], g_lv[rs_, :], True, True, [Rg], [pkvs[c][1]])
                        yield
                        for c in range(2):
                            tt("dve", g_sbf[c], Sst, bds[:], ALU.mult, [RS, Rc], [Rg])
                            stt("dve", Sst, Sst, small[:, 8 + c:9 + c], pkvs[c][0][:, 0:256], ALU.mult, ALU.add,
                                [RS, RsG, pkvs[c][1]], [RS])
                        yield
                        po, Rpo = pAUX.get()
                        yield
                        for c in range(2):
                            rs_ = slice(64 * c, 64 * c + 64)
                            mm(po[rs_, 0:256], g_qb[:, rs_], g_sbf[c], True, False, [Rg], [Rpo], skip_group_check=True)
                        yield
                        for hh in range(4):
                            mm(po[:, hh * 64:(hh + 1) * 64], g_att[:, hh, :], g_lv[:, hh * 64:(hh + 1) * 64], False, True,
                               [Rg], [Rpo], skip_group_check=True)
                        yield
                        act(g_sq, po[:, 0:256], AF.Square, [Rpo], [Rg])
                        yield
                        red(small[:, 12:16], g_sq.rearrange("p (h d) -> p h d", h=4), [Rg], [RsG])
                        yield
                        rsqrt(small[:, 12:16], small[:, 12:16], 1.0 / 64, [RsG], [RsG])
                        yield
                        tt("dve", g_o.rearrange("p (h d) -> p h d", h=4), po[:, 0:256].rearrange("p (h d) -> p h d", h=4),
                           small[:, 12:16].unsqueeze(2).to_broadcast([128, 4, 64]), ALU.mult, [Rpo, RsG], [Rg])
                        yield
                        tt("pool", g_o, g_o, g_lg, ALU.mult, [Rg], [Rg])
                        yield
                        if l == 0:
                            tap("gla", g_o, [Rg], lambda d, J=J: d[J * 128:(J + 1) * 128, :])
                        yield
                        act(g_sq, g_o, AF.Square, [Rg], [Rg, RsG], accum_out=small[:, 16:17])
                        yield
                        rsqrt(small[:, 16:17], small[:, 16:17], 1.0 / 256, [RsG], [RsG])
                        yield
                        ts("dve", nrm[:, j, 256:512], g_o, small[:, 16:17], None, ALU.mult, None, [Rg, RsG], [Rnrm])
                        yield
                        mv3 = m_v.rearrange("p (g d) -> p g d", g=4)
                        yield
                        red(small[:, 20:24], mv3, [Rg], [RsG])
                        yield
                        tt("pool", m_sq, m_v, m_v, ALU.mult, [Rg], [Rg])
                        yield
                        red(small[:, 24:28], m_sq.rearrange("p (g d) -> p g d", g=4), [Rg], [RsG])
                        yield
                        ts("dve", small[:, 20:24], small[:, 20:24], 1.0 / 64, None, ALU.mult, None, [RsG], [RsG])
                        yield
                        tt("dve", small[:, 28:32], small[:, 20:24], small[:, 20:24], ALU.mult, [RsG], [RsG])
                        yield
                        stt("dve", small[:, 24:28], small[:, 24:28], 1.0 / 64, small[:, 28:32], ALU.mult, ALU.subtract,
                            [RsG], [RsG])
                        yield
                        rsqrt(small[:, 24:28], small[:, 24:28], 1.0, [RsG], [RsG])
                        yield
                        tt("dve", mv3, mv3, small[:, 20:24].unsqueeze(2).to_broadcast([128, 4, 64]), ALU.subtract,
                           [Rg, RsG], [Rg])
                        yield
                        tt("dve", m_vln.rearrange("p (g d) -> p g d", g=4), mv3,
                           small[:, 24:28].unsqueeze(2).to_broadcast([128, 4, 64]), ALU.mult, [Rg, RsG], [Rg])
                        yield
                        pgm, Rpgm = pAUX.get()
                        yield
                        for g in range(4):
                            mm(pgm[:, g * 64:(g + 1) * 64], gwT[:, g, :], m_vln[:, g * 64:(g + 1) * 64], True, True,
                               [Rw, Rg], [Rpgm])
                        yield
                        mo3 = m_sq.rearrange("p (g d) -> p g d", g=4)
                        yield
                        tt("dve", mo3, pgm[:, 0:256].rearrange("p (g d) -> p g d", g=4),
                           gbT[:].unsqueeze(2).to_broadcast([128, 4, 64]), ALU.add, [Rpgm, Rw], [Rg])
                        yield
                        tt("pool", m_sq, m_sq, m_u, ALU.mult, [Rg], [Rg])
                        yield
                        if l == 0:
                            tap("gmlp", m_sq, [Rg], lambda d, J=J: d[J * 128:(J + 1) * 128, :])
                        yield
                        act(m_v, m_sq, AF.Square, [Rg], [Rg, RsG], accum_out=small[:, 17:18])
                        yield
                        rsqrt(small[:, 17:18], small[:, 17:18], 1.0 / 256, [RsG], [RsG])
                        yield
                        ts("dve", nrm[:, j, 512:768], m_sq, small[:, 17:18], None, ALU.mult, None, [Rg, RsG], [Rnrm])
                        yield
                def gen_A():
                    for i in range(len(units) + LOOK):
                        if i < len(units):
                            emit_score(i)
                        if i >= LOOK:
                            emit_pv(i - LOOK)
                        yield

                gA, gB = gen_A(), gen_B()
                per = max(1, -(-(4 * 68) // (len(units) + LOOK)))
                aliveA = aliveB = True
                while aliveA or aliveB:
                    if aliveA:
                        try:
                            next(gA)
                        except StopIteration:
                            aliveA = False
                    nb_ = per if aliveA else 1 << 30
                    while aliveB and nb_ > 0:
                        nb_ -= 1
                        try:
                            next(gB)
                        except StopIteration:
                            aliveB = False
                if stop == "b0s4":
                    return done()
                for j in range(4):
                    J = 4 * tb + j
                    if l == 0:
                        tap("nsa", nsa[:, j, :], [Rnsa], lambda d, J=J: d[J * 128:(J + 1) * 128, :])
                    act(t2[:, 0:256], nsa[:, j, :], AF.Square, [Rnsa], [Rt2, Rsm], accum_out=small[:, 18:19])
                    rsqrt(small[:, 18:19], small[:, 18:19], 1.0 / 256, [Rsm], [Rsm])
                    ts("dve", nrm[:, j, 0:256], nsa[:, j, :], small[:, 18:19], None, ALU.mult, None, [Rnsa, Rsm], [Rnrm])

                if stop == "b0s5":
                    return done()
                pss, Rpss = pMM.get()
                for c2 in range(2):
                    mm(pss[:], ones[:], sqc[c2], c2 == 0, c2 == 1, [Rc, Rsqc], [Rpss])
                rsqrt(t1, pss[:], 1.0 / 256, [Rpss], [Rt1])
                for c2 in range(2):
                    if l == 0:
                        tap("conv", cvo[c2], [Rcvo[c2]], lambda d, c2=c2, bc=bc: d[c2 * 128:(c2 + 1) * 128, bc])
                    stt("dve", mixT[:, c2, :], cvo[c2], gainc[:, c2:c2 + 1], t1, ALU.mult, ALU.mult,
                        [Rcvo[c2], Rw, Rt1], [Rxs])
                for cch6 in range(6):
                    pb, Rpb = pMM.get()
                    pbv = pb[:].bitcast(BF16)
                    for j in range(4):
                        tr(pbv[:, j * 128:(j + 1) * 128], nrm[:, j, cch6 * 128:(cch6 + 1) * 128], ident[:],
                           [Rnrm, Rc], [Rpb])
                    act(mixT[:, 2 + cch6, :], pbv[:, 0:512], AF.Copy, [Rpb, Rw], [Rxs], scale=gainc[:, 2 + cch6:3 + cch6])

                Rsm = Rs7
                for j in range(4):
                    S.dma("sp", xbufs[j][0], xsv[:, 4 * tb + j, :], writes=xbufs[j][1])
                for j in range(4):
                    J = 4 * tb + j
                    xb, Rxb = xbufs[j]
                    pys = []
                    for half in range(2):
                        py, Rpy = pY.get()
                        for k in range(8):
                            mm(py[:], mixT[:, k, j * 128:(j + 1) * 128], w_o[:, k, half * 512:(half + 1) * 512],
                               k == 0, k == 7, [Rxs, RW1o[k // 2]], [Rpy])
                        act(sqc[half], py[:], AF.Square, [Rpy], [Rsqc, Rsm],
                            accum_out=small[:, 56 + half:57 + half])
                        pys.append((py, Rpy))
                    tt("dve", small[:, 58:59], small[:, 56:57], small[:, 57:58], ALU.add, [Rsm], [Rsm])
                    rsqrt(small[:, 58:59], small[:, 58:59], 1.0 / D, [Rsm], [Rsm])
                    for half in range(2):
                        py, Rpy = pys[half]
                        hs = slice(half * 512, (half + 1) * 512)
                        stt("dve", ysb[:, hs], py[:], small[:, 58:59], G1[:, hs], ALU.mult, ALU.mult,
                            [Rpy, Rsm, RG], [Rysb])
                        tt("pool", xb[:, hs], xb[:, hs], ysb[:, hs], ALU.add, Rxb + [Rysb], Rxb)
                    S.dma("sp", xmv[:, J, :], xb, reads=Rxb)
                if stop == "b0":
                    return done()
            S.barrier()
            if stop == "p1":
                return done()

            WB.reset()
            PA.reset(PA_base)
            w_up = WB.get([128, 8, DFF], BF16)
            w_dn = WB.get([128, 32, D], BF16)
            RW2u = [Res() for _ in range(8)]
            RW2d = [Res() for _ in range(8)]
            wuv = w_up_d[l].rearrange("(k p) n -> p k n", p=128)
            for k in range(8):
                S.dma("pool", w_up[:, k, :], wuv[:, k, :], writes=[RW2u[k]])
            wdv = w_dn_d[l].rearrange("(k p) n -> p k n", p=128)
            for k in range(0, 32, 4):
                S.dma("pool", w_dn[:, k:k + 4, :], wdv[:, k:k + 4, :], writes=[RW2d[k // 4]])
            xp = PA.get([128, 1024], F32); Rxp = Res()
            xe = [PA.get([128, 1024], F32) for _ in range(2)]; Rxe = [Res(), Res()]
            xs2 = [PA.get([128, 2, 1024], BF16) for _ in range(2)]; Rxs2 = [Res(), Res()]
            hT2 = [PA.get([128, 8, 256], BF16) for _ in range(2)]; RhT2 = [Res(), Res()]
            actT = PA.get([128, 32, 256], BF16); Ract = Res()
            ysb = PA.get([128, 1024], F32); Rysb = Res()
            junk = PA.get([128, 512], BF16); Rjunk = Res()
            xdv = xview(xdst)
            NB2 = S_ // 256

            def prologue(tb):
                q = tb % 2
                for j in range(2):
                    J = 2 * tb + j
                    S.dma("sp", xp, xmv[:, J, :], writes=[Rxp])
                    act(xs2[q][:, j, :], xp, AF.Square, [Rxp], [Rxs2[q], Rs1], accum_out=small[:, j:j + 1])
                    rsqrt(small[:, 4 + j:5 + j], small[:, j:j + 1], 1.0 / D, [Rs1], [Rs1])
                    ts("dve", xs2[q][:, j, :], xp, small[:, 4 + j:5 + j], None, ALU.mult, None, [Rxp, Rs1], [Rxs2[q]])
                for k in range(8):
                    pb, Rpb = pMM.get()
                    pbv = pb[:].bitcast(BF16)
                    for j in range(2):
                        tr(pbv[:, j * 128:(j + 1) * 128], xs2[q][:, j, k * 128:(k + 1) * 128], ident[:],
                           [Rxs2[q], Rc], [Rpb])
                    if k % 2 == 0:
                        act(hT2[q][:, k, :], pbv[:, 0:256], AF.Identity, [Rpb, Rmod], [RhT2[q]],
                            scale=A2[:, k:k + 1], bias=sh2[:, k:k + 1])
                    else:
                        ts("dve", hT2[q][:, k, :], pbv[:, 0:256], A2[:, k:k + 1], sh2[:, k:k + 1], ALU.mult, ALU.add,
                           [Rpb, Rmod], [RhT2[q]])

            def up(tb):
                q = tb % 2
                for j in range(2):
                    S.dma("sp", xe[j], xmv[:, 2 * tb + j, :], writes=[Rxe[j]])
                for hc in range(0, 32, 2):
                    pb, Rpb = pMM.get()
                    for u in range(2):
                        for k in range(8):
                            mm(pb[:, u * 256:(u + 1) * 256], w_up[:, k, (hc + u) * 128:(hc + u + 1) * 128],
                               hT2[q][:, k, :], k == 0, k == 7, [RW2u[k], RhT2[q]], [Rpb])
                    av = actT[:, hc:hc + 2, :].rearrange("p a b -> p (a b)")
                    act(av, pb[:], AF.Relu, [Rpb], [Ract])
                    tt("pool" if (hc // 2) % 2 else "dve", av, av, av, ALU.mult, [Ract], [Ract])

            def down(tb):
                for j in range(2):
                    J = 2 * tb + j
                    pys = []
                    for half in range(2):
                        py, Rpy = pAUX.get() if half else pACC.get()
                        for k in range(32):
                            mm(py[:], actT[:, k, j * 128:(j + 1) * 128], w_dn[:, k, half * 512:(half + 1) * 512],
                               k == 0, k == 31, [Ract, RW2d[k // 4]], [Rpy])
                        act(junk, py[:], AF.Square, [Rpy], [Rjunk, Rs7], accum_out=small[:, 56 + half:57 + half])
                        pys.append((py, Rpy))
                    tt("dve", small[:, 58:59], small[:, 56:57], small[:, 57:58], ALU.add, [Rs7], [Rs7])
                    rsqrt(small[:, 58:59], small[:, 58:59], 1.0 / D, [Rs7], [Rs7])
                    for half in range(2):
                        py, Rpy = pys[half]
                        hs = slice(half * 512, (half + 1) * 512)
                        stt("dve", ysb[:, hs], py[:], small[:, 58:59], G2[:, hs], ALU.mult, ALU.mult,
                            [Rpy, Rs7, RG], [Rysb])
                        tt("pool", xe[j][:, hs], xe[j][:, hs], ysb[:, hs], ALU.add, [Rxe[j], Rysb], [Rxe[j]])
                    S.dma("sp", xdv[:, J, :], xe[j], reads=[Rxe[j]])

            prologue(0)
            for tb in range(NB2):
                up(tb)
                if tb + 1 < NB2:
                    prologue(tb + 1)
                down(tb)
            S.barrier()
            xsrc = out_d
        S.barrier()
        S.emit()
    return nc


def prep_shared(inp):
    f = lambda a: np.ascontiguousarray(np.asarray(a, dtype=np.float32))
    perm = _perm()
    sh = {}
    sh["w_in"] = f(np.asarray(inp["w_in"])[:, :, perm])
    sh["w_o"] = f(inp["w_o"])
    sh["w_up"] = f(inp["w_up"])
    sh["w_down"] = f(inp["w_down"])
    sh["w_mod"] = f(inp["w_mod"])
    cw = np.asarray(inp["conv_w"])
    sh["convw"] = f(cw.reshape(DEPTH, 3, 2, 128).transpose(0, 3, 2, 1))
    sh["cmp_w1"] = f(inp["cmp_w1"])
    sh["cmp_w2"] = f(inp["cmp_w2"])
    cpz = np.asarray(inp["cmp_pos"])
    sh["cmp_posT"] = f(cpz.transpose(0, 1, 3, 2).reshape(DEPTH, 128, 32))
    sh["gw2b"] = f(np.concatenate([np.asarray(inp["gla_gate_w2"]), np.asarray(inp["gla_gate_b"])[:, None, :]], axis=1))
    sh["gmlp_wsT"] = f(np.asarray(inp["gmlp_ws"]).transpose(0, 3, 1, 2))
    sh["gmlp_bT"] = f(np.asarray(inp["gmlp_b"]).transpose(0, 2, 1))
    sh["gaincol"] = f(np.asarray(inp["grp_gain"]).reshape(DEPTH, 8, 128).transpose(0, 2, 1))
    ng = np.asarray(inp["norm_g"])
    sh["normg_col"] = f(ng.reshape(DEPTH, 4, 8, 128).transpose(0, 3, 1, 2))
    sh["normg_row"] = f(ng)
    bm = np.asarray(inp["b_mod"])
    sh["bmod_col"] = f(bm.reshape(DEPTH, 48, 128).transpose(0, 2, 1))
    sh["bmod_row"] = f(bm)
    for k, v in _consts().items():
        sh["c_" + k] = f(v)
    return sh


def core_inputs(inp, b, sh):
    d = dict(sh)
    d["x"] = np.ascontiguousarray(np.asarray(inp["x"][b], dtype=np.float32))
    d["ccol"] = np.ascontiguousarray(np.asarray(inp["c"][b], dtype=np.float32).reshape(8, 128).T)
    d["pos"] = np.ascontiguousarray(np.asarray(inp["positions"][b], dtype=np.int32).reshape(1, S_))
    return d


def kernel(**inputs):
    sh = prep_shared(inputs)
    nc = build()
    in_maps = [core_inputs(inputs, b, sh) for b in range(8)]
    res = run_bass_kernel_spmd(nc, in_maps, core_ids=list(range(8)))
    return np.stack([np.asarray(r["out"], dtype=np.float32) for r in res.results], axis=0)
```

```python
from contextlib import ExitStack
import numpy as np
import concourse.bass as bass
import concourse.mybir as mybir
from concourse.bass_utils import run_bass_kernel_spmd

F32 = mybir.dt.float32
BF16 = mybir.dt.bfloat16
I32 = mybir.dt.int32
ALU = mybir.AluOpType
AF = mybir.ActivationFunctionType
AX = mybir.AxisListType

D = 1024
S_ = 4096
DEPTH = 4
DFF = 4096
NT = S_ // 128
NB = S_ // 512
EPS = 1e-6
NEG = -30000.0
NCOL = 2972
FM_W = [128] * 15 + [16]
C_AH, C_AB, C_AC, C_Q, C_KVC, C_KS, C_KW, C_LQ, C_LK, C_LR = 0, 2, 4, 6, 10, 11, 12, 13, 14, 15
TM0 = 1936


class Res:
    __slots__ = ("w", "rs", "excl")

    def __init__(self, excl=False):
        self.w = None
        self.rs = {}
        self.excl = excl


class Sched:
    CE = ("pe", "act", "dve", "pool")

    def __init__(self, nc, stack, n_dma_sems=(("sp", 24), ("pool", 16), ("act", 4))):
        self.nc = nc
        self.prog = {k: [] for k in ("pe", "act", "dve", "pool", "sp")}
        self.semobj = {}
        self.cnt = {}
        self.ckey = {}
        self.keyeng = {}
        self.stack = stack
        self.epoch = 0
        for k in self.CE:
            self.semobj[k] = stack.enter_context(nc.semaphore("s_" + k))
            self.cnt[k] = 0
            self.ckey[k] = k
            self.keyeng[k] = k
        self.dq, self.dval, self.dnext = {}, {}, {}
        for q, n in n_dma_sems:
            keys = []
            for i in range(n):
                key = f"d_{q}{i}"
                self.semobj[key] = stack.enter_context(nc.semaphore(key))
                self.dval[key] = 0
                keys.append(key)
            self.dq[q] = keys
            self.dnext[q] = 0
        self.seen = {k: {} for k in self.prog}
        self.n_ops = 0

    def _wait(self, e, dep):
        key, val = dep
        if self.seen[e].get(key, 0) >= val:
            return
        self.seen[e][key] = val
        s = self.semobj[key]
        self.prog[e].append(lambda eng, s=s, v=val: eng.wait_ge(s, v))

    def _deps(self, e, reads, writes, is_dma):
        for r in reads:
            if r.w is not None:
                self._wait(e, r.w)
            if r.excl:
                for k, v in r.rs.items():
                    if self.keyeng.get(k) != e:
                        self._wait(e, (k, v))
        for w in writes:
            if w.w is not None and (is_dma or self.keyeng.get(w.w[0]) != e):
                self._wait(e, w.w)
            for k, v in w.rs.items():
                if is_dma or self.keyeng.get(k) != e:
                    self._wait(e, (k, v))

    def _commit(self, rec, reads, writes):
        for r in reads:
            if r.rs.get(rec[0], 0) < rec[1]:
                r.rs[rec[0]] = rec[1]
        for w in writes:
            w.w = rec
            w.rs = {}

    def op(self, e, fn, reads=(), writes=()):
        self._deps(e, reads, writes, False)
        self.cnt[e] += 1
        rec = (self.ckey[e], self.cnt[e])
        s = self.semobj[self.ckey[e]]
        self.prog[e].append(lambda eng, f=fn, s=s: f(eng).then_inc(s, 1))
        self._commit(rec, reads, writes)
        self.n_ops += 1
        return rec

    def dma(self, q, out, in_, reads=(), writes=(), **kw):
        self._deps(q, reads, writes, True)
        keys = self.dq[q]
        key = keys[self.dnext[q] % len(keys)]
        self.dnext[q] += 1
        prev = self.dval[key]
        if prev > 0:
            self._wait(q, (key, prev))
        tgt = prev + 16
        self.dval[key] = tgt
        s = self.semobj[key]
        self.prog[q].append(
            lambda eng, o=out, i=in_, s=s, kw=kw: eng.dma_start(out=o, in_=i, **kw).then_inc(s, 16))
        rec = (key, tgt)
        self._commit(rec, reads, writes)
        self.n_ops += 1
        return rec

    def barrier(self):
        for e in self.prog:
            for k in self.CE:
                if self.cnt[k] > 0:
                    self._wait(e, (self.ckey[k], self.cnt[k]))
            for key, v in self.dval.items():
                if v > 0:
                    self._wait(e, (key, v))

    def new_epoch(self):
        self.barrier()
        self.epoch += 1
        for k in self.CE:
            key = f"{k}@{self.epoch}"
            self.semobj[key] = self.stack.enter_context(self.nc.semaphore(f"s_{k}_{self.epoch}"))
            self.ckey[k] = key
            self.keyeng[key] = k
            self.cnt[k] = 0

    def emit(self):
        nc = self.nc
        with nc.Block() as block:
            @block.tensor
            def _(eng):
                for f in self.prog["pe"]:
                    f(eng)

            @block.scalar
            def _(eng):
                for f in self.prog["act"]:
                    f(eng)

            @block.vector
            def _(eng):
                for f in self.prog["dve"]:
                    f(eng)

            @block.gpsimd
            def _(eng):
                for f in self.prog["pool"]:
                    f(eng)

            @block.sync
            def _(eng):
                for f in self.prog["sp"]:
                    f(eng)


class Arena:
    def __init__(self, tile, nbytes, elsize):
        self.tile, self.nbytes, self.elsize = tile, nbytes, elsize
        self.off = 0

    def reset(self, off=0):
        self.off = off

    def get(self, shape, dt):
        es = 4 if dt in (F32, I32) else 2
        n = int(np.prod(shape[1:]))
        nb = (n * es + 31) // 32 * 32
        assert self.off + nb <= self.nbytes, f"arena overflow {self.off}+{nb}>{self.nbytes}"
        a, b = self.off // self.elsize, (self.off + nb) // self.elsize
        self.off += nb
        v = self.tile[:, a:b]
        if dt != self.tile.dtype:
            v = v.bitcast(dt)
        v = v[:, 0:n]
        if len(shape) == 3:
            v = v.rearrange("p (a b) -> p a b", a=shape[1])
        elif len(shape) == 4:
            v = v.rearrange("p (a b c) -> p a b c", a=shape[1], b=shape[2])
        if shape[0] < 128:
            v = v[0:shape[0]]
        return v


def _consts():
    c = {}
    kp = np.arange(128)[:, None]
    tq = np.arange(512)[None, :]
    masks = np.zeros((128, 12, 512), np.float32)
    for i in range(4):
        masks[:, i, :] = np.where(kp + 128 * i <= tq, 0.0, NEG)
    for n, i in enumerate(range(-4, 0)):
        masks[:, 4 + n, :] = np.where(tq < kp + 128 * i + 512, 0.0, NEG)
    for pat in range(4):
        masks[:, 8 + pat, :] = np.where(16 * kp + 15 - 512 * pat <= tq, 0.0, NEG)
    c["masks"] = masks
    c["ident"] = np.eye(128, dtype=np.float32)
    pm = np.zeros((128, 128), np.float32)
    for m in range(8):
        pm[m + 8, m] = 1.0
        pm[m, m + 8] = 1.0
    c["pm"] = pm
    inv_freq = (500000.0 ** (-np.arange(0, 16, 2, dtype=np.float32) / 16)).astype(np.float32)
    rc = np.zeros((128, 2), np.float32)
    rc[:16, 0] = np.concatenate([inv_freq, inv_freq])
    rc[:8, 1] = -1.0
    rc[8:16, 1] = 1.0
    c["ropec"] = rc
    ew = np.zeros((64, 4096), np.float32)
    for jb in range(64):
        ew[jb, jb * 64:(jb + 1) * 64] = 1.0
    c["ew"] = ew
    mo = np.zeros((256, 64), np.float32)
    for s in range(1, 256):
        n = s - 1
        cs = n * 16
        for j in range(64):
            ov = min(cs + 32, j * 64 + 64) - max(cs, j * 64)
            mo[s, j] = max(ov, 0) / 16.0
    c["mo"] = mo.reshape(2, 128, 64).transpose(1, 0, 2).copy()
    wd = np.zeros((128, 127), np.float32)
    cl = (np.arange(128) // 64)[:, None]
    m = (np.arange(127) - 62)[None, :]
    wd = np.where(m > cl, -1e30, np.where(m == cl, 2e4, np.where(m == cl - 1, 1e4, 0.0))).astype(np.float32)
    c["wd"] = wd
    s = np.arange(128)[:, None]
    t = np.arange(128)[None, :]
    same = (s // 64) == (t // 64)
    c["tinc"] = np.where(same & (s <= t), -1.0 / 16, 0.0).astype(np.float32)
    c["trev"] = np.where(same & (s > t), -1.0 / 16, 0.0).astype(np.float32)
    c["glam"] = np.where(same & (s <= t), 1.0, 0.0).astype(np.float32)
    c["triu"] = np.where(s <= t, 1.0, 0.0).astype(np.float32)
    p = np.arange(128)[:, None]
    c["bds"] = ((p // 32) == (np.arange(256)[None, :] // 64)).astype(np.float32)
    c["bdq"] = np.repeat(((p // 32) == np.arange(4)[None, :]).astype(np.float32)[:, :, None], 128, axis=2).copy()
    c["ones"] = np.ones((128, 128), np.float32)
    return c


def _perm():
    r = lambda a, b: list(range(a, b))
    cols = []
    cols += r(0, 256) + r(256, 512) + r(512, 768)
    for h in range(4):
        cols += r(768 + 64 * h, 832 + 64 * h) * 2
    cols += r(1024, 1152)
    cols += r(1152, 1216) * 2
    cols += r(1280, 1344) * 2
    cols += r(1420, 1548)
    cols += r(1548, 1676)
    cols += r(2188, 2204)
    assert len(cols) == TM0
    cols += r(1216, 1280) + r(1344, 1408) + r(1408, 1420)
    cols += r(1548, 1676) + r(1676, 1932)
    cols += r(1932, 2188) + r(2204, 2460)
    cols += r(2460, 2716)
    return np.array(cols, np.int64)


NCOL = TM0 + 140 + 384 + 512 + 256
FM_OFF = [128 * i for i in range(15)] + [1920]
T_OFF = [TM0, TM0 + 140, TM0 + 524, TM0 + 1036]
T_W = [140, 384, 512, 256]


def build(n_layers=DEPTH, taps=(), stop=None):
    nc = bass.Bass("TRN2", target_bir_lowering=False)
    CST = _consts()

    def din(name, shape, dt=F32):
        return nc.dram_tensor(name, list(shape), dt, kind="ExternalInput").ap()

    x_in = din("x", [S_, D])
    ccol_in = din("ccol", [128, 8])
    pos_in = din("pos", [1, S_], I32)
    w_in_d = din("w_in", [DEPTH, D, NCOL])
    w_o_d = din("w_o", [DEPTH, D, D])
    w_up_d = din("w_up", [DEPTH, D, DFF])
    w_dn_d = din("w_down", [DEPTH, DFF, D])
    w_mod_d = din("w_mod", [DEPTH, D, 6 * D])
    convw_d = din("convw", [DEPTH, 128, 2, 3])
    w1_d = din("cmp_w1", [DEPTH, 2, 2048, 128])
    w2_d = din("cmp_w2", [DEPTH, 2, 128, 64])
    posT_d = din("cmp_posT", [DEPTH, 128, 32])
    gw2b_d = din("gw2b", [DEPTH, 17, 128])
    gws_d = din("gmlp_wsT", [DEPTH, 128, 4, 128])
    gb_d = din("gmlp_bT", [DEPTH, 128, 4])
    gain_d = din("gaincol", [DEPTH, 128, 8])
    ngc_d = din("normg_col", [DEPTH, 128, 4, 8])
    ngr_d = din("normg_row", [DEPTH, 4, D])
    bmc_d = din("bmod_col", [DEPTH, 128, 48])
    bmr_d = din("bmod_row", [DEPTH, 6 * D])
    cst_d = {k: din("c_" + k, v.shape) for k, v in CST.items()}
    out_d = nc.dram_tensor("out", [S_, D], F32, kind="ExternalOutput").ap()
    xa_d = nc.dram_tensor("xa", [S_, D], F32).ap()
    ropeC_d = nc.dram_tensor("ropeC", [128, S_], F32).ap()
    ropeS_d = nc.dram_tensor("ropeS", [128, S_], F32).ap()
    tap_d = {}
    for name, shape in taps:
        tap_d[name] = nc.dram_tensor("tap_" + name, list(shape), F32, kind="ExternalOutput").ap()

    xview = lambda ap: ap.rearrange("(n p) d -> p n d", p=128)

    with ExitStack() as st:
        S = Sched(nc, st)
        build.last_sched = S
        sbt = lambda name, shape, dt: st.enter_context(nc.sbuf_tensor("sb_" + name, shape, dt))
        WB_t = sbt("WB", [128, 65536], BF16)
        PA_t = sbt("PA", [128, 14848], F32)
        WB = Arena(WB_t, 131072, 2)
        PA = Arena(PA_t, 59392, 4)

        def BUF(shape, dt):
            es = 4 if dt in (F32, I32) else 2
            nb = (int(np.prod(shape[1:])) * es + 31) // 32 * 32
            if WB.off + nb <= WB.nbytes:
                return WB.get(shape, dt)
            return PA.get(shape, dt)
        ps = [st.enter_context(nc.psum_tensor(f"ps{i}", [128, 512], F32)) for i in range(8)]
        psR = [Res(excl=True) for _ in range(8)]

        class Pool:
            def __init__(self, ids):
                self.ids, self.i = ids, 0

            def get(self):
                b = self.ids[self.i % len(self.ids)]
                self.i += 1
                return ps[b], psR[b]
        pMM = Pool([0, 1, 2])
        pACC = Pool([3, 4])
        pAUX = Pool([5, 6, 7])

        def mm(out, lhsT, rhs, start, stop, r, w, **kw):
            S.op("pe", lambda e: e.matmul(out, lhsT, rhs, start=start, stop=stop, **kw), r, w)

        def tr(out, in_, ident, r, w):
            S.op("pe", lambda e: e.transpose(out, in_, ident), r, w)

        def act(out, in_, func, r, w, **kw):
            S.op("act", lambda e: e.activation(out, in_, func, **kw), r, w)

        def ts(eng, out, in0, s1, s2, op0, op1, r, w):
            if op1 is None:
                S.op(eng, lambda e: e.tensor_scalar(out, in0, s1, None, op0), r, w)
            else:
                S.op(eng, lambda e: e.tensor_scalar(out, in0, s1, s2, op0, op1), r, w)

        def stt(eng, out, in0, sc, in1, op0, op1, r, w):
            S.op(eng, lambda e: e.scalar_tensor_tensor(out, in0, sc, in1, op0, op1), r, w)

        def tt(eng, out, in0, in1, op, r, w):
            S.op(eng, lambda e: e.tensor_tensor(out, in0, in1, op), r, w)

        def cp(eng, out, in_, r, w):
            if eng == "act":
                S.op("act", lambda e: e.copy(out, in_), r, w)
            else:
                S.op(eng, lambda e: e.tensor_copy(out, in_), r, w)

        def red(out, in_, r, w, op=ALU.add):
            S.op("dve", lambda e: e.tensor_reduce(out, in_, AX.X, op), r, w)

        def recip(out, in_, r, w):
            S.op("dve", lambda e: e.reciprocal(out, in_), r, w)

        def memset(eng, ap, v, w):
            S.op(eng, lambda e: e.memset(ap, v), (), w)

        def rsqrt(out, in_, scale, r, w):
            act(out, in_, AF.Sqrt, r, w, bias=EPS, scale=scale)
            recip(out, out, w, w)

        def tap(name, src_ap, res, dst_slice=None):
            if name in tap_d:
                dst = tap_d[name] if dst_slice is None else dst_slice(tap_d[name])
                S.dma("pool", dst, src_ap, reads=res)

        masks = sbt("masks", [128, 12, 512], BF16); Rc = Res()
        ident = sbt("ident", [128, 128], BF16)
        pm = sbt("pm", [128, 128], BF16)
        ones = sbt("ones", [128, 128], BF16)
        tinc = sbt("tinc", [128, 128], BF16)
        trev = sbt("trev", [128, 128], BF16)
        glam = sbt("glam", [128, 128], BF16)
        triu = sbt("triu", [128, 128], F32)
        bds = sbt("bds", [128, 256], BF16)
        bdq = sbt("bdq", [128, 4, 128], BF16)
        wd = sbt("wd", [128, 127], F32)
        ropec = sbt("ropec", [128, 2], F32)
        mo = sbt("mo", [128, 2, 64], BF16)
        for t_, k in ((masks, "masks"), (ident, "ident"), (pm, "pm"), (ones, "ones"), (tinc, "tinc"),
                      (trev, "trev"), (glam, "glam"), (bds, "bds"), (bdq, "bdq"), (mo, "mo")):
            S.dma("pool", t_[:], cst_d[k], writes=[Rc])
        for t_, k in ((triu, "triu"), (wd, "wd"), (ropec, "ropec")):
            S.dma("sp", t_[:], cst_d[k], writes=[Rc])
        condT = sbt("condT", [128, 8], BF16)
        cond_bc = sbt("cond_bc", [128, 8, 128], BF16)
        modcol = sbt("modcol", [128, 48], F32); Rmod = Res()
        A1 = sbt("A1", [128, 8], F32)
        A2 = sbt("A2", [128, 8], F32)
        ngc = sbt("ngc", [128, 4, 8], F32)
        bmc = sbt("bmc", [128, 48], F32)
        gainc = sbt("gainc", [128, 8], F32)
        convw = sbt("convw", [128, 2, 3], F32)
        gbT = sbt("gbT", [128, 4], F32)
        c1col = sbt("c1col", [128, 2], F32)
        small = sbt("small", [128, 64], F32); Rsm = Res()
        Rw = Res()

        PA.reset()
        posi = PA.get([128, 512], I32)
        ang = PA.get([128, 512], F32)
        kf = PA.get([128, 512], F32)
        tS = PA.get([128, 512], F32)
        tC = PA.get([128, 512], F32)
        Rs = Res()
        TWO_PI = float(2 * np.pi)
        for pc in range(8):
            cs = slice(pc * 512, (pc + 1) * 512)
            S.dma("sp", posi, pos_in[:, cs].partition_broadcast(128), writes=[Rs])
            cp("dve", ang, posi, [Rs], [Rs])
            ts("dve", ang, ang, ropec[:, 0:1], None, ALU.mult, None, [Rs, Rc], [Rs])
            ts("dve", kf, ang, 1.0 / TWO_PI, None, ALU.mult, None, [Rs], [Rs])
            cp("dve", posi, kf, [Rs], [Rs])
            cp("dve", kf, posi, [Rs], [Rs])
            stt("dve", ang, kf, -TWO_PI, ang, ALU.mult, ALU.add, [Rs], [Rs])
            ts("dve", ang, ang, 3.1415925, -3.1415925, ALU.min, ALU.max, [Rs], [Rs])
            act(tS, ang, AF.Sin, [Rs], [Rs])
            ts("dve", tS, tS, ropec[:, 1:2], None, ALU.mult, None, [Rs, Rc], [Rs])
            stt("dve", kf, ang, -1.0, ang, ALU.mult, ALU.max, [Rs], [Rs])
            act(tC, kf, AF.Sin, [Rs], [Rs], scale=-1.0, bias=float(np.pi / 2))
            S.dma("sp", ropeS_d[:, cs], tS, reads=[Rs])
            S.dma("sp", ropeC_d[:, cs], tC, reads=[Rs])
        ccol = PA.get([128, 8], F32)
        S.dma("sp", ccol, ccol_in, writes=[Rs])
        act(condT[:], ccol, AF.Silu, [Rs], [Rmod])
        cp("dve", cond_bc[:], condT[:].unsqueeze(2).to_broadcast([128, 8, 128]), [Rmod], [Rmod])
        Rrope = Res()
        S.barrier()

        def done():
            S.barrier()
            S.emit()
            return nc
        if stop == "setup":
            return done()

        xsrc = x_in
        for l in range(n_layers):
            if l > 0:
                S.new_epoch()
            xmid = xa_d
            xdst = out_d
            PA.reset()
            WB.reset()
            G1 = PA.get([128, 1024], F32)
            G2 = PA.get([128, 1024], F32)
            RG = Res()
            PA_base = PA.off
            wm = [WB.get([128, 8, 512], BF16) for _ in range(2)]
            Rwm = [Res(), Res()]
            brow = WB.get([128, 1024], F32)
            grow = WB.get([128, 1024], F32)
            Rrow = Res()
            for t_, src in ((ngc, ngc_d[l]), (bmc, bmc_d[l]), (gainc, gain_d[l]), (convw, convw_d[l]),
                            (gbT, gb_d[l])):
                S.dma("sp", t_[:], src, writes=[Rw])
            pcol, Rpcol = pAUX.get()
            wmv = w_mod_d[l].rearrange("(k p) n -> p k n", p=128)
            for pc in range(12):
                b = pc % 2
                S.dma("pool", wm[b], wmv[:, :, pc * 512:(pc + 1) * 512], writes=[Rwm[b]])
                for jj in range(4):
                    col = pc * 4 + jj
                    for k in range(8):
                        mm(pcol[:, col:col + 1], wm[b][:, k, jj * 128:(jj + 1) * 128], condT[:, k:k + 1],
                           k == 0, k == 7, [Rwm[b], Rmod], [Rpcol])
                if pc in (4, 5, 10, 11):
                    Gt = G1 if pc < 6 else G2
                    half = pc % 2
                    hs = slice(half * 512, (half + 1) * 512)
                    gi = 1 if pc < 6 else 3
                    pg, Rpg = pMM.get()
                    for k in range(8):
                        mm(pg[:], cond_bc[:, k, :], wm[b][:, k, :], k == 0, k == 7, [Rwm[b], Rmod], [Rpg])
                    if half == 0:
                        S.dma("sp", brow, bmr_d[l:l + 1, (pc // 2) * 1024:(pc // 2) * 1024 + 1024].partition_broadcast(128),
                              writes=[Rrow])
                        S.dma("sp", grow, ngr_d[l, gi:gi + 1, :].partition_broadcast(128), writes=[Rrow])
                    tt("dve", Gt[:, hs], pg[:], brow[:, hs], ALU.add, [Rpg, Rrow], [RG])
                    tt("dve", Gt[:, hs], Gt[:, hs], grow[:, hs], ALU.mult, [RG, Rrow], [RG])
            tt("dve", modcol[:], pcol[:, 0:48], bmc[:], ALU.add, [Rpcol, Rw], [Rmod])
            stt("dve", A1[:], modcol[:, 8:16], 1.0, ngc[:, 0, :], ALU.add, ALU.mult, [Rmod, Rw], [Rmod])
            stt("dve", A2[:], modcol[:, 32:40], 1.0, ngc[:, 2, :], ALU.add, ALU.mult, [Rmod, Rw], [Rmod])
            sh1 = modcol[:, 0:8]
            sh2 = modcol[:, 24:32]
            S.barrier()
            if stop == "mods":
                return done()

            WB.reset()
            PA.reset(PA_base)
            w_in = WB.get([128, 8, NCOL], BF16)
            w_o = WB.get([128, 8, D], BF16)
            RW1i = [Res() for _ in range(8)]
            RW1o = [Res() for _ in range(4)]
            wiv = w_in_d[l].rearrange("(k p) n -> p k n", p=128)
            for k in range(8):
                S.dma("pool", w_in[:, k, :], wiv[:, k, :], writes=[RW1i[k]])
            wov = w_o_d[l].rearrange("(k p) n -> p k n", p=128)
            for k in range(0, 8, 2):
                S.dma("pool", w_o[:, k:k + 2, :], wov[:, k:k + 2, :], writes=[RW1o[k // 2]])
            ksA = PA.get([128, 4096], BF16)
            kwT = PA.get([128, 1024], BF16)
            vs1 = PA.get([128, 32, 65], BF16)
            vw1 = PA.get([128, 8, 65], BF16)
            kvc = PA.get([128, 528], BF16)
            kcT = PA.get([128, 256], BF16)
            hTv = PA.get([128, 256], BF16)
            vcM = PA.get([128, 2, 128], BF16)
            W1 = PA.get([128, 32, 128], BF16)
            w2k = PA.get([128, 64], BF16)
            w2v = PA.get([128, 64], BF16)
            posT = PA.get([128, 32], BF16)
            gwT = PA.get([128, 4, 128], BF16)
            gw2b = PA.get([32, 128], BF16)
            Sst = PA.get([128, 256], F32)
            zbuf = [PA.get([128, 514], F32) for _ in range(2)]
            lr1T = PA.get([32, 512], BF16)
            ropeCt = PA.get([128, 512], F32)
            ropeSt = PA.get([128, 512], F32)
            Rks, Rkw, Rvs, Rvw, Rkvc, Rkc, RhTv, RvcM, RS, Rlr, Rrt = (Res() for _ in range(11))
            Rz = [Res(), Res()]
            xt = [BUF([128, 1024], F32)]; Rxt = [Res()]
            xs = BUF([128, 4, 1024], BF16); Rxs = Res()
            mixT = xs.rearrange("p a b -> p (a b)").rearrange("p (k t) -> p k t", k=8)
            hT = BUF([128, 8, 512], BF16); RhT = Res()
            QA = [BUF([128, 512], BF16) for _ in range(4)]; RQA = [Res() for _ in range(4)]
            qraw = BUF([128, 512], BF16); Rqr = Res()
            kraw = BUF([128, 512], BF16); Rkr = Res()
            pT = [BUF([128, 512], BF16) for _ in range(3)]; RpT = [Res() for _ in range(3)]
            cvy = BUF([128, 512], F32); Rcvy = Res()
            cvo12 = BUF([128, 1024], F32)
            cvo = [cvo12[:, 0:512], cvo12[:, 512:1024]]; Rcvo = [Res(), Res()]
            glq = BUF([128, 512], F32); glk = BUF([128, 512], F32); Rgl = Res()
            gts = BUF([128, 4, 12], F32); Rgts = Res()
            nrm = BUF([128, 4, 768], BF16); Rnrm = Res()
            nsa = BUF([128, 4, 256], F32); Rnsa = Res()
            pslc = BUF([128, 4, 64], F32); Rpslc = Res()
            selb = BUF([128, 128], BF16); Rselb = Res()
            t12 = BUF([128, 1024], F32)
            t1 = t12[:, 0:512]; t2 = t12[:, 512:1024]; Rt1 = Res(); Rt2 = Res()
            gwT32 = t1.rearrange("p (a b) -> p a b", a=4)
            g_e = BUF([128, 128], F32); g_lsp = BUF([128, 128], BF16)
            g_b = BUF([128, 128], F32); g_tmp = g_e
            g_e1 = BUF([128, 128], F32); g_e2 = BUF([128, 128], F32); g_e3 = g_e1
            g_qe = BUF([128, 128], BF16); g_ke = BUF([128, 128], BF16); g_qb = BUF([128, 128], BF16)
            g_qeb = BUF([128, 4, 128], BF16); g_ed = BUF([128, 128], F32); g_kd = BUF([128, 128], BF16)
            g_att = BUF([128, 4, 128], BF16); g_sbf = [BUF([128, 256], BF16) for _ in range(2)]
            g_kt = BUF([128, 128], F32); g_lv = BUF([128, 256], BF16); g_lg = BUF([128, 256], F32)
            g_sq = BUF([128, 256], F32); g_o = BUF([128, 256], F32)
            m_u = BUF([128, 256], F32); m_v = BUF([128, 256], F32); m_sq = g_sq
            m_vln = BUF([128, 256], BF16)
            Rg = Res()
            Rs1, RsG, RsA, Rs7 = Res(), Res(), Res(), Res()
            xbufs = [(xt[0], [Rxt[0]]), (t12, [Rt1, Rt2]), (nsa.rearrange("p a b -> p (a b)"), [Rnsa]),
                     (cvo12, [Rcvo[0], Rcvo[1]])]
            pY = Pool([3, 4, 5, 6, 7])
            pB = Pool([5, 6, 7])
            ysb = BUF([128, 1024], F32); Rysb = Res()
            sqc = [BUF([128, 512], BF16) for _ in range(2)]; Rsqc = Res()
            S.dma("pool", ksA[64:128, :], cst_d["ew"], writes=[Rks])
            S.dma("pool", W1[0:64], w1_d[l, 0].rearrange("(p d) h -> d p h", d=64), writes=[Rw])
            S.dma("pool", W1[64:128], w1_d[l, 1].rearrange("(p d) h -> d p h", d=64), writes=[Rw])
            S.dma("pool", w2k, w2_d[l, 0], writes=[Rw])
            S.dma("pool", w2v, w2_d[l, 1], writes=[Rw])
            S.dma("pool", posT, posT_d[l], writes=[Rw])
            S.dma("pool", gw2b[0:17, :], gw2b_d[l], writes=[Rw])
            S.dma("sp", gwT32, gws_d[l], writes=[Rt1])
            tt("dve", gwT, gwT32, triu[:].unsqueeze(1).to_broadcast([128, 4, 128]), ALU.mult, [Rt1, Rc], [Rw])
            memset("dve", vs1[:, :, 64:65], 1.0, [Rvs])
            memset("dve", vw1[:, :, 64:65], 1.0, [Rvw])
            memset("dve", kvc[:, 0:16], 0.0, [Rkvc])
            memset("dve", kcT, 0.0, [Rkc])
            memset("dve", kwT[64:128, :], 0.0, [Rkw])
            memset("dve", hTv, 0.0, [RhTv])
            memset("dve", Sst, 0.0, [RS])
            memset("dve", lr1T, 1.0, [Rlr])
            for c2 in range(2):
                memset("dve", zbuf[c2][:, 0:2], 0.0, [Rz[c2]])
            cp("dve", vcM[:, :, 64:128], mo[:], [Rc], [RvcM])
            for jkv in range(2):
                pc1, Rpc1 = pAUX.get() if jkv == 0 else pACC.get()
                rs_ = slice(64 * jkv, 64 * jkv + 64)
                for p_ in range(32):
                    mm(pc1[:, 0:1], W1[rs_, p_, :], posT[rs_, p_:p_ + 1], p_ == 0, p_ == 31, [Rw], [Rpc1])
                cp("dve", c1col[:, jkv:jkv + 1], pc1[:, 0:1], [Rpc1], [Rw])
            if stop == "p1init":
                return done()

            xsv = xview(xsrc)
            xmv = xview(xmid)
            for tb in range(NB):
                bc = slice(tb * 512, (tb + 1) * 512)
                S.dma("sp", ropeCt, ropeC_d[:, bc], writes=[Rrt])
                S.dma("sp", ropeSt, ropeS_d[:, bc], writes=[Rrt])
                Rsm = Rs1
                for j in range(4):
                    S.dma("sp", xbufs[j][0], xsv[:, 4 * tb + j, :], writes=xbufs[j][1])
                for j in range(4):
                    xb, Rxb = xbufs[j]
                    act(xs[:, j, :], xb, AF.Square, Rxb, [Rxs, Rsm], accum_out=small[:, j:j + 1])
                    rsqrt(small[:, 4 + j:5 + j], small[:, j:j + 1], 1.0 / D, [Rsm], [Rsm])
                    ts("dve", xs[:, j, :], xb, small[:, 4 + j:5 + j], None, ALU.mult, None, Rxb + [Rsm], [Rxs])
                for k in range(8):
                    pb, Rpb = pMM.get()
                    pbv = pb[:].bitcast(BF16)
                    for j in range(4):
                        tr(pbv[:, j * 128:(j + 1) * 128], xs[:, j, k * 128:(k + 1) * 128], ident[:], [Rxs, Rc], [Rpb])
                    if k % 2 == 0:
                        act(hT[:, k, :], pbv[:, 0:512], AF.Identity, [Rpb, Rmod], [RhT],
                            scale=A1[:, k:k + 1], bias=sh1[:, k:k + 1])
                    else:
                        ts("dve", hT[:, k, :], pbv[:, 0:512], A1[:, k:k + 1], sh1[:, k:k + 1], ALU.mult, ALU.add,
                           [Rpb, Rmod], [RhT])
                if l == 0 and tb == 0:
                    tap("hT", hT.rearrange("p k t -> p (k t)"), [RhT])

                if stop == "b0s2":
                    return done()
                def fm(ch):
                    pb, Rpb = pMM.get()
                    w = FM_W[ch]
                    c0 = FM_OFF[ch]
                    for k in range(8):
                        mm(pb[0:w, :], w_in[:, k, c0:c0 + w], hT[:, k, :], k == 0, k == 7, [RW1i[k], RhT], [Rpb])
                    return pb, Rpb

                def tmc(ti, jc, pool=None):
                    pb, Rpb = (pool or pMM).get()
                    for k in range(8):
                        mm(pb[:, 0:T_W[ti]], hT[:, k, jc], w_in[:, k, T_OFF[ti]:T_OFF[ti] + T_W[ti]],
                           k == 0, k == 7, [RW1i[k], RhT], [Rpb])
                    return pb, Rpb

                def rope(src_ps, Rsrc, raw, Rraw, dst, Rdst):
                    cp("act", raw[:], src_ps[:], [Rsrc], [Rraw])
                    p2, Rp2 = pMM.get()
                    mm(p2[:], pm[:], raw[:], True, True, [Rc, Rraw], [Rp2])
                    if stop == "q0c":
                        return "STOP"
                    tt("dve", t1, src_ps[:], ropeCt, ALU.mult, [Rsrc, Rrt], [Rt1])
                    if stop == "q0d":
                        return "STOP"
                    tt("dve", t2, p2[:], ropeSt, ALU.mult, [Rp2, Rrt], [Rt2])
                    if stop == "q0e":
                        return "STOP"
                    tt("dve", dst, t1[0:64], t2[0:64], ALU.add, [Rt1, Rt2], [Rdst])

                pb, Rpb = fm(C_LQ)
                cp("act", glq, pb[:], [Rpb], [Rgl])
                pb, Rpb = fm(C_LK)
                cp("act", glk, pb[:], [Rpb], [Rgl])
                pb, Rpb = fm(C_LR)
                cp("act", lr1T[0:16, :], pb[0:16, :], [Rpb], [Rlr])
                def gen_B():
                    for j in range(4):
                        J = 4 * tb + j
                        yield
                        jc = slice(j * 128, (j + 1) * 128)
                        yield
                        pb, Rpb = tmc(1, jc, pB)
                        yield
                        cp("dve", g_kt, pb[:, 0:128], [Rpb], [Rg])
                        yield
                        cp("dve", g_lv, pb[:, 128:384], [Rpb], [Rg])
                        yield
                        pb, Rpb = tmc(2, jc, pB)
                        yield
                        act(g_lg, pb[:, 0:256], AF.Silu, [Rpb], [Rg])
                        yield
                        cp("dve", m_u, pb[:, 256:512], [Rpb], [Rg])
                        yield
                        pb, Rpb = tmc(3, jc, pB)
                        yield
                        cp("dve", m_v, pb[:, 0:256], [Rpb], [Rg])
                        yield
                        pa, Rpa = pB.get()
                        yield
                        mm(pa[:, 0:128], lr1T[0:17, jc], gw2b[0:17, :], True, True, [Rlr, Rw], [Rpa])
                        yield
                        act(g_e, pa[:, 0:128], AF.Exp, [Rpa], [Rg], scale=-1.0)
                        yield
                        act(g_lsp, g_e, AF.Ln, [Rg], [Rg], bias=1.0)
                        yield
                        pbT, RpbT = pB.get()
                        yield
                        mm(pbT[:, 0:128], g_lsp, tinc[:], True, True, [Rg, Rc], [RpbT])
                        yield
                        mm(pbT[:, 128:256], trev[:], g_lsp, True, True, [Rg, Rc], [RpbT])
                        yield
                        cp("dve", g_b, pbT[:, 0:128], [RpbT], [Rg])
                        yield
                        gb3 = g_b.rearrange("p (c t) -> p c t", c=2)
                        yield
                        tt("dve", g_tmp.rearrange("p (c t) -> p c t", c=2), gb3, gb3[:, :, 32:33].to_broadcast([128, 2, 64]),
                           ALU.subtract, [Rg], [Rg])
                        yield
                        qsc = float(32 ** -0.5)
                        yield
                        act(g_e1, g_tmp, AF.Exp, [Rg], [Rg])
                        yield
                        act(g_e2, g_tmp, AF.Exp, [Rg], [Rg], scale=-1.0)
                        yield
                        stt("dve", g_qe, glq[:, jc], qsc, g_e1, ALU.mult, ALU.mult, [Rgl, Rg], [Rg])
                        yield
                        tt("dve", g_ke, glk[:, jc], g_e2, ALU.mult, [Rgl, Rg], [Rg])
                        yield
                        act(g_e3, g_b, AF.Exp, [Rg], [Rg])
                        yield
                        act(small[:, 8:10], gb3[:, :, 63], AF.Exp, [Rg], [RsG])
                        yield
                        act(g_ed, pbT[:, 128:256], AF.Exp, [RpbT], [Rg])
                        yield
                        stt("dve", g_qb, glq[:, jc], qsc, g_e3, ALU.mult, ALU.mult, [Rgl, Rg], [Rg])
                        yield
                        tt("dve", g_qeb, bdq[:], g_qe.unsqueeze(1).to_broadcast([128, 4, 128]), ALU.mult, [Rc, Rg], [Rg])
                        yield
                        tt("dve", g_kd, g_kt, g_ed, ALU.mult, [Rg], [Rg])
                        yield
                        patt, Rpatt = pB.get()
                        yield
                        mm(patt[:], g_ke, g_qeb.rearrange("p a b -> p (a b)"), True, True, [Rg], [Rpatt])
                        yield
                        tt("dve", g_att, patt[:].rearrange("p (a b) -> p a b", a=4),
                           glam[:].unsqueeze(1).to_broadcast([128, 4, 128]), ALU.mult, [Rpatt, Rc], [Rg])
                        yield
                        pkvs = [pB.get(), pB.get()]
                        yield
                        for c in range(2):
                            rs_ = slice(64 * c, 64 * c + 64)
                            mm(pkvs[c][0][:, 0:256], g_kd[rs_, :], g_lv[rs_, :], True, True, [Rg], [pkvs[c][1]])
                        yield
                        for c in range(2):
                            tt("dve", g_sbf[c], Sst, bds[:], ALU.mult, [RS, Rc], [Rg])
                            stt("dve", Sst, Sst, small[:, 8 + c:9 + c], pkvs[c][0][:, 0:256], ALU.mult, ALU.add,
                                [RS, RsG, pkvs[c][1]], [RS])
                        yield
                        po, Rpo = pB.get()
                        yield
                        for c in range(2):
                            rs_ = slice(64 * c, 64 * c + 64)
                            mm(po[rs_, 0:256], g_qb[:, rs_], g_sbf[c], True, False, [Rg], [Rpo], skip_group_check=True)
                        yield
                        for hh in range(4):
                            mm(po[:, hh * 64:(hh + 1) * 64], g_att[:, hh, :], g_lv[:, hh * 64:(hh + 1) * 64], False, True,
                               [Rg], [Rpo], skip_group_check=True)
                        yield
                        act(g_sq, po[:, 0:256], AF.Square, [Rpo], [Rg])
                        yield
                        red(small[:, 12:16], g_sq.rearrange("p (h d) -> p h d", h=4), [Rg], [RsG])
                        yield
                        rsqrt(small[:, 12:16], small[:, 12:16], 1.0 / 64, [RsG], [RsG])
                        yield
                        tt("dve", g_o.rearrange("p (h d) -> p h d", h=4), po[:, 0:256].rearrange("p (h d) -> p h d", h=4),
                           small[:, 12:16].unsqueeze(2).to_broadcast([128, 4, 64]), ALU.mult, [Rpo, RsG], [Rg])
                        yield
                        tt("pool", g_o, g_o, g_lg, ALU.mult, [Rg], [Rg])
                        yield
                        if l == 0:
                            tap("gla", g_o, [Rg], lambda d, J=J: d[J * 128:(J + 1) * 128, :])
                        yield
                        act(g_sq, g_o, AF.Square, [Rg], [Rg, RsG], accum_out=small[:, 16:17])
                        yield
                        rsqrt(small[:, 16:17], small[:, 16:17], 1.0 / 256, [RsG], [RsG])
                        yield
                        ts("dve", nrm[:, j, 256:512], g_o, small[:, 16:17], None, ALU.mult, None, [Rg, RsG], [Rnrm])
                        yield
                        mv3 = m_v.rearrange("p (g d) -> p g d", g=4)
                        yield
                        red(small[:, 20:24], mv3, [Rg], [RsG])
                        yield
                        tt("pool", m_sq, m_v, m_v, ALU.mult, [Rg], [Rg])
                        yield
                        red(small[:, 24:28], m_sq.rearrange("p (g d) -> p g d", g=4), [Rg], [RsG])
                        yield
                        ts("dve", small[:, 20:24], small[:, 20:24], 1.0 / 64, None, ALU.mult, None, [RsG], [RsG])
                        yield
                        tt("dve", small[:, 28:32], small[:, 20:24], small[:, 20:24], ALU.mult, [RsG], [RsG])
                        yield
                        stt("dve", small[:, 24:28], small[:, 24:28], 1.0 / 64, small[:, 28:32], ALU.mult, ALU.subtract,
                            [RsG], [RsG])
                        yield
                        rsqrt(small[:, 24:28], small[:, 24:28], 1.0, [RsG], [RsG])
                        yield
                        tt("dve", mv3, mv3, small[:, 20:24].unsqueeze(2).to_broadcast([128, 4, 64]), ALU.subtract,
                           [Rg, RsG], [Rg])
                        yield
                        tt("dve", m_vln.rearrange("p (g d) -> p g d", g=4), mv3,
                           small[:, 24:28].unsqueeze(2).to_broadcast([128, 4, 64]), ALU.mult, [Rg, RsG], [Rg])
                        yield
                        pgm, Rpgm = pB.get()
                        yield
                        for g in range(4):
                            mm(pgm[:, g * 64:(g + 1) * 64], gwT[:, g, :], m_vln[:, g * 64:(g + 1) * 64], True, True,
                               [Rw, Rg], [Rpgm])
                        yield
                        mo3 = m_sq.rearrange("p (g d) -> p g d", g=4)
                        yield
                        tt("dve", mo3, pgm[:, 0:256].rearrange("p (g d) -> p g d", g=4),
                           gbT[:].unsqueeze(2).to_broadcast([128, 4, 64]), ALU.add, [Rpgm, Rw], [Rg])
                        yield
                        tt("pool", m_sq, m_sq, m_u, ALU.mult, [Rg], [Rg])
                        yield
                        if l == 0:
                            tap("gmlp", m_sq, [Rg], lambda d, J=J: d[J * 128:(J + 1) * 128, :])
                        yield
                        act(m_v, m_sq, AF.Square, [Rg], [Rg, RsG], accum_out=small[:, 17:18])
                        yield
                        rsqrt(small[:, 17:18], small[:, 17:18], 1.0 / 256, [RsG], [RsG])
                        yield
                        ts("dve", nrm[:, j, 512:768], m_sq, small[:, 17:18], None, ALU.mult, None, [Rg, RsG], [Rnrm])
                        yield
                gB = gen_B()
                Bst = {"alive": True}

                def pumpB(n):
                    while Bst["alive"] and n > 0:
                        n -= 1
                        try:
                            next(gB)
                        except StopIteration:
                            Bst["alive"] = False

                for c2 in range(2):
                    z = zbuf[c2]
                    pb, Rpb = fm(C_AH + c2)
                    cp("act", z[:, 2:514], pb[:], [Rpb], [Rz[c2]])
                    pb, Rpb = fm(C_AC + c2)
                    tt("dve", z[:, 2:514], pb[:], z[:, 2:514], ALU.mult, [Rpb, Rz[c2]], [Rz[c2]])
                    ts("pool", cvy, z[:, 2:514], convw[:, c2, 2:3], None, ALU.mult, None, [Rz[c2], Rw], [Rcvy])
                    stt("dve", cvy, z[:, 1:513], convw[:, c2, 1:2], cvy, ALU.mult, ALU.add, [Rz[c2], Rw, Rcvy], [Rcvy])
                    stt("dve", cvy, z[:, 0:512], convw[:, c2, 0:1], cvy, ALU.mult, ALU.add, [Rz[c2], Rw, Rcvy], [Rcvy])
                    cp("pool", z[:, 0:2], z[:, 512:514], [Rz[c2]], [Rz[c2]])
                    pb, Rpb = fm(C_AB + c2)
                    tt("dve", cvo[c2], pb[:], cvy, ALU.mult, [Rpb, Rcvy], [Rcvo[c2]])
                    act(sqc[c2], cvo[c2], AF.Square, [Rcvo[c2]], [Rsqc])
                    pumpB(16)
                if stop == "s3a":
                    return done()
                pb, Rpb = fm(C_KVC)
                cp("act", kvc[:, 16:528], pb[:], [Rpb], [Rkvc])
                if stop == "s3b1":
                    return done()
                phk, Rphk = pMM.get()
                phv, Rphv = pACC.get()
                for jkv in range(2):
                    rs_ = slice(64 * jkv, 64 * jkv + 64)
                    for p_ in range(32):
                        mm((phk, phv)[jkv][:, 0:32], W1[rs_, p_, :], kvc[rs_, p_:p_ + 497:16], p_ == 0, p_ == 31,
                           [Rw, Rkvc], [(Rphk, Rphv)[jkv]])
                if stop == "s3b2":
                    return done()
                hk = t1[:, 0:16].bitcast(BF16)
                act(hk, phk[:, 0:32], AF.Silu, [Rphk, Rw], [Rt1], bias=c1col[:, 0:1])
                sl = slice(32 * tb, 32 * tb + 32)
                act(hTv[:, sl], phv[:, 0:32], AF.Silu, [Rphv, Rw], [RhTv], bias=c1col[:, 1:2])
                if tb == 0:
                    memset("dve", hTv[:, 0:1], 0.0, [RhTv])
                if stop == "s3b3":
                    return done()
                cp("pool", kvc[:, 0:16], kvc[:, 512:528], [Rkvc], [Rkvc])
                if stop == "s3b4":
                    return done()
                pk2, Rpk2 = pMM.get()
                mm(pk2[0:64, 0:32], w2k, hk, True, True, [Rw, Rt1], [Rpk2])
                cch = tb // 4
                mm(pk2[:, 64:128], hTv[:, 128 * cch:128 * cch + 128], w2v, True, True, [RhTv, Rw], [Rpk2])
                cp("act", kcT[0:64, sl], pk2[0:64, 0:32], [Rpk2], [Rkc])
                cp("act", vcM[:, cch, 0:64], pk2[:, 64:128], [Rpk2], [RvcM])
                pumpB(16)
                if stop == "s3b":
                    return done()
                for j in range(4):
                    J = 4 * tb + j
                    pb, Rpb = tmc(0, slice(j * 128, (j + 1) * 128))
                    cp("act", vs1[:, J, 0:64], pb[:, 0:64], [Rpb], [Rvs])
                    cp("act", vw1[:, J % 8, 0:64], pb[:, 64:128], [Rpb], [Rvw])
                    act(gts[:, j, :], pb[:, 128:140], AF.Sigmoid, [Rpb], [Rgts])
                    pumpB(6)
                if stop == "s3c":
                    return done()
                Rsm = RsA
                nch = 1 if tb < 4 else 2
                for h in range(4):
                    pb, Rpb = fm(C_Q + h)
                    if rope(pb, Rpb, qraw, Rqr, QA[h][0:64, :], RQA[h]) == "STOP":
                        return done()
                    if l == 0 and tb == 0 and h == 0:
                        tap("qa0", QA[0][0:64, :], [RQA[0]])
                        tap("qraw0", qraw[0:64, :], [Rqr])
                        tap("ropeC", ropeCt, [Rrt])
                        tap("ropeS", ropeSt, [Rrt])
                    if stop == "q1":
                        return done()
                    for c in range(nch):
                        pb, Rpb = pMM.get()
                        masked = (c == nch - 1)
                        mm(pb[:], kcT[:, 128 * c:128 * c + 128], qraw[:, :], True, not masked,
                           [Rkc, Rqr], [Rpb])
                        if masked:
                            mm(pb[:], ident[:], masks[:, 8 + (tb % 4), :], False, True, [Rc], [Rpb])
                        act(pT[c], pb[:], AF.Exp, [Rpb], [RpT[c]], scale=0.125)
                    if stop == "q2":
                        return done()
                    pacc, Rpacc = pACC.get()
                    pa3 = pacc[:].rearrange("p (j c) -> p j c", j=4)
                    for j in range(4):
                        for c in range(nch):
                            mm(pa3[:, j, :], pT[c][:, j * 128:(j + 1) * 128], vcM[:, c, :], c == 0, c == nch - 1,
                               [RpT[c], RvcM], [Rpacc])
                    if stop == "q3":
                        return done()
                    den = small[:, 32:36]
                    red(den, pa3[:, :, 64:128], [Rpacc], [Rsm])
                    ts("dve", den, den, 0.5, 1e-30, ALU.mult, ALU.max, [Rsm], [Rsm])
                    recip(den, den, [Rsm], [Rsm])
                    u3 = t1[:, 0:256].rearrange("p (j c) -> p j c", j=4)
                    tt("dve", u3, pa3[:, :, 64:128], den.unsqueeze(2).to_broadcast([128, 4, 64]), ALU.mult,
                       [Rpacc, Rsm], [Rt1])
                    if h == 0:
                        cp("pool", pslc, u3, [Rt1], [Rpslc])
                    else:
                        tt("pool", pslc, pslc, u3, ALU.add, [Rt1, Rpslc], [Rpslc])
                    tt("dve", small[:, 36:40], den, gts[:, :, 3 * h], ALU.mult, [Rsm, Rgts], [Rsm])
                    tt("dve", nsa[:, :, h * 64:(h + 1) * 64], pa3[:, :, 0:64],
                       small[:, 36:40].unsqueeze(2).to_broadcast([128, 4, 64]), ALU.mult, [Rpacc, Rsm], [Rnsa])
                    pumpB(16)
                if stop == "s3d":
                    return done()
                if l == 0:
                    for j in range(4):
                        tap("nsac", nsa[:, j, :], [Rnsa], lambda d, J=4 * tb + j: d[J * 128:(J + 1) * 128, :])
                pb, Rpb = fm(C_KS)
                rope(pb, Rpb, kraw, Rkr, ksA[0:64, bc], Rks)
                if l == 0 and tb == 0:
                    tap("ks0", ksA[0:64, 0:512], [Rks])
                pb, Rpb = fm(C_KW)
                rope(pb, Rpb, kraw, Rkr, kwT[0:64, (tb % 2) * 512:(tb % 2) * 512 + 512], Rkw)

                if stop == "b0s3":
                    return done()
                Rsm = RsA
                psl, Rpsl = pMM.get()
                pslv = psl[:].bitcast(BF16)
                for j in range(4):
                    J = 4 * tb + j
                    sc_ = t2[:, 0:64]
                    tt("dve", sc_, pslc[:, j, :], wd[:, 62 - 2 * J:126 - 2 * J], ALU.add, [Rpslc, Rc], [Rt2])
                    ts("dve", sc_[:, 0:1], sc_[:, 0:1], 3e4, None, ALU.add, None, [Rt2], [Rt2])
                    m8 = small[:, 40:56]
                    S.op("dve", lambda e, sc_=sc_, m8=m8: e.max(out=m8[:, 0:8], in_=sc_), [Rt2], [Rsm])
                    wk_ = t2[:, 64:128]
                    S.op("dve", lambda e, sc_=sc_, m8=m8, wk_=wk_: e.match_replace(
                        out=wk_, in_to_replace=m8[:, 0:8], in_values=sc_, imm_value=-3e38), [Rt2, Rsm], [Rt2])
                    S.op("dve", lambda e, m8=m8, wk_=wk_: e.max(out=m8[:, 8:16], in_=wk_), [Rt2], [Rsm])
                    if j == 0:
                        memset("dve", selb[:, 0:64], 0.0, [Rselb])
                    ts("dve", selb[:, 64:128], sc_, m8[:, 15:16], NEG, ALU.is_lt, ALU.mult, [Rt2, Rsm], [Rselb])
                    tr(pslv[:, j * 128:(j + 1) * 128], selb[:], ident[:], [Rselb, Rc], [Rpsl])
                for h in range(4):
                    cp("act" if h % 2 else "dve", QA[h][64:128, :], pslv[64:128, 0:512], [Rpsl], [RQA[h]])

                jobs = []
                for h in range(4):
                    jobs.append(dict(h=h, kts=list(range(0, 4 * tb + 4)), keyT=ksA, Rk=Rks, v1=vs1, Rv=Rvs,
                                     mask_of=(lambda kt: (kt - 4 * tb) if kt >= 4 * tb else None),
                                     rel_of=(lambda kt, j: kt <= 4 * tb + j), gcol=3 * h + 1, kmod=32))
                    jobs.append(dict(h=h, kts=list(range(max(0, 4 * tb - 4), 4 * tb + 4)), keyT=kwT, Rk=Rkw, v1=vw1, Rv=Rvw,
                                     mask_of=(lambda kt: (kt - 4 * tb) if kt >= 4 * tb else (4 + (kt - 4 * tb + 4))),
                                     rel_of=(lambda kt, j: (kt <= 4 * tb + j) and (kt >= 4 * tb + j - 4)),
                                     gcol=3 * h + 2, kmod=8))
                units = []
                for jb_ in jobs:
                    jb_["last"] = {}
                    for kt in jb_["kts"]:
                        for j in range(4):
                            if jb_["rel_of"](kt, j):
                                jb_["last"][j] = kt
                    jb_["started"] = False
                    for n, kt in enumerate(jb_["kts"]):
                        units.append((jb_, n, kt))
                LOOK = 2

                def emit_score(i):
                    jb_, n, kt = units[i]
                    h = jb_["h"]
                    js = [j for j in range(4) if jb_["rel_of"](kt, j)]
                    cs_ = slice(128 * js[0], 128 * (js[-1] + 1))
                    pb, Rpb = pMM.get()
                    mk = jb_["mask_of"](kt)
                    km = jb_["kmod"]
                    kc_ = slice((kt % km) * 128, (kt % km + 1) * 128)
                    mm(pb[:, cs_], jb_["keyT"][:, kc_], QA[h][:, cs_], True, mk is None, [jb_["Rk"], RQA[h]], [Rpb])
                    if mk is not None:
                        mm(pb[:, cs_], ident[:], masks[:, mk, cs_], False, True, [Rc], [Rpb])
                    act(pT[i % 3][:, cs_], pb[:, cs_], AF.Exp, [Rpb], [RpT[i % 3]], scale=0.125)

                def emit_pv(i):
                    jb_, n, kt = units[i]
                    h = jb_["h"]
                    if n == 0:
                        jb_["pacc"] = pACC.get()
                    pacc, Rpacc = jb_["pacc"]
                    pa3 = pacc[:, 0:260].rearrange("p (j c) -> p j c", j=4)
                    km = jb_["kmod"]
                    pt_ = pT[i % 3]
                    for j in range(4):
                        if jb_["rel_of"](kt, j):
                            mm(pa3[:, j, :], pt_[:, j * 128:(j + 1) * 128], jb_["v1"][:, kt % km, :],
                               not jb_["started"], kt == jb_["last"][j], [RpT[i % 3], jb_["Rv"]], [Rpacc],
                               skip_group_check=True)
                            jb_["started"] = True
                    if n == len(jb_["kts"]) - 1:
                        den = small[:, 32:36]
                        ts("dve", den, pa3[:, :, 64], 1e-30, None, ALU.max, None, [Rpacc], [Rsm])
                        recip(den, den, [Rsm], [Rsm])
                        tt("dve", small[:, 36:40], den, gts[:, :, jb_["gcol"]], ALU.mult, [Rsm, Rgts], [Rsm])
                        u3 = t1[:, 0:256].rearrange("p (j c) -> p j c", j=4)
                        tt("dve", u3, pa3[:, :, 0:64], small[:, 36:40].unsqueeze(2).to_broadcast([128, 4, 64]),
                           ALU.mult, [Rpacc, Rsm], [Rt1])
                        tt("pool", nsa[:, :, h * 64:(h + 1) * 64], nsa[:, :, h * 64:(h + 1) * 64], u3, ALU.add,
                           [Rt1, Rnsa], [Rnsa])

                def gen_A():
                    for i in range(len(units) + LOOK):
                        if i < len(units):
                            emit_score(i)
                        if i >= LOOK:
                            emit_pv(i - LOOK)
                        yield

                gA = gen_A()
                per = max(1, -(-(4 * 68) // (len(units) + LOOK)))
                aliveA = True
                aliveB = Bst["alive"]
                while aliveA or aliveB:
                    if aliveA:
                        try:
                            next(gA)
                        except StopIteration:
                            aliveA = False
                    nb_ = per if aliveA else 1 << 30
                    while aliveB and nb_ > 0:
                        nb_ -= 1
                        try:
                            next(gB)
                        except StopIteration:
                            aliveB = False
                if stop == "b0s4":
                    return done()
                for j in range(4):
                    J = 4 * tb + j
                    if l == 0:
                        tap("nsa", nsa[:, j, :], [Rnsa], lambda d, J=J: d[J * 128:(J + 1) * 128, :])
                    act(t2[:, 0:256], nsa[:, j, :], AF.Square, [Rnsa], [Rt2, Rsm], accum_out=small[:, 18:19])
                    rsqrt(small[:, 18:19], small[:, 18:19], 1.0 / 256, [Rsm], [Rsm])
                    ts("dve", nrm[:, j, 0:256], nsa[:, j, :], small[:, 18:19], None, ALU.mult, None, [Rnsa, Rsm], [Rnrm])

                if stop == "b0s5":
                    return done()
                pss, Rpss = pMM.get()
                for c2 in range(2):
                    mm(pss[:], ones[:], sqc[c2], c2 == 0, c2 == 1, [Rc, Rsqc], [Rpss])
                rsqrt(t1, pss[:], 1.0 / 256, [Rpss], [Rt1])
                for c2 in range(2):
                    if l == 0:
                        tap("conv", cvo[c2], [Rcvo[c2]], lambda d, c2=c2, bc=bc: d[c2 * 128:(c2 + 1) * 128, bc])
                    stt("dve", mixT[:, c2, :], cvo[c2], gainc[:, c2:c2 + 1], t1, ALU.mult, ALU.mult,
                        [Rcvo[c2], Rw, Rt1], [Rxs])
                for cch6 in range(6):
                    pb, Rpb = pMM.get()
                    pbv = pb[:].bitcast(BF16)
                    for j in range(4):
                        tr(pbv[:, j * 128:(j + 1) * 128], nrm[:, j, cch6 * 128:(cch6 + 1) * 128], ident[:],
                           [Rnrm, Rc], [Rpb])
                    act(mixT[:, 2 + cch6, :], pbv[:, 0:512], AF.Copy, [Rpb, Rw], [Rxs], scale=gainc[:, 2 + cch6:3 + cch6])

                Rsm = Rs7
                for j in range(4):
                    S.dma("sp", xbufs[j][0], xsv[:, 4 * tb + j, :], writes=xbufs[j][1])
                for j in range(4):
                    J = 4 * tb + j
                    xb, Rxb = xbufs[j]
                    pys = []
                    for half in range(2):
                        py, Rpy = pY.get()
                        for k in range(8):
                            mm(py[:], mixT[:, k, j * 128:(j + 1) * 128], w_o[:, k, half * 512:(half + 1) * 512],
                               k == 0, k == 7, [Rxs, RW1o[k // 2]], [Rpy])
                        act(sqc[half], py[:], AF.Square, [Rpy], [Rsqc, Rsm],
                            accum_out=small[:, 56 + half:57 + half])
                        pys.append((py, Rpy))
                    tt("dve", small[:, 58:59], small[:, 56:57], small[:, 57:58], ALU.add, [Rsm], [Rsm])
                    rsqrt(small[:, 58:59], small[:, 58:59], 1.0 / D, [Rsm], [Rsm])
                    for half in range(2):
                        py, Rpy = pys[half]
                        hs = slice(half * 512, (half + 1) * 512)
                        stt("dve", ysb[:, hs], py[:], small[:, 58:59], G1[:, hs], ALU.mult, ALU.mult,
                            [Rpy, Rsm, RG], [Rysb])
                        tt("pool", xb[:, hs], xb[:, hs], ysb[:, hs], ALU.add, Rxb + [Rysb], Rxb)
                    S.dma("sp", xmv[:, J, :], xb, reads=Rxb)
                if stop == "b0":
                    return done()
            S.barrier()
            if stop == "p1":
                return done()

            WB.reset()
            PA.reset(PA_base)
            w_up = WB.get([128, 8, DFF], BF16)
            w_dn = WB.get([128, 32, D], BF16)
            RW2u = [Res() for _ in range(8)]
            RW2d = [Res() for _ in range(8)]
            wuv = w_up_d[l].rearrange("(k p) n -> p k n", p=128)
            for k in range(8):
                S.dma("pool", w_up[:, k, :], wuv[:, k, :], writes=[RW2u[k]])
            wdv = w_dn_d[l].rearrange("(k p) n -> p k n", p=128)
            for k in range(0, 32, 4):
                S.dma("pool", w_dn[:, k:k + 4, :], wdv[:, k:k + 4, :], writes=[RW2d[k // 4]])
            xp = PA.get([128, 1024], F32); Rxp = Res()
            xe = [PA.get([128, 1024], F32) for _ in range(2)]; Rxe = [Res(), Res()]
            xs2 = [PA.get([128, 2, 1024], BF16) for _ in range(2)]; Rxs2 = [Res(), Res()]
            hT2 = [PA.get([128, 8, 256], BF16) for _ in range(2)]; RhT2 = [Res(), Res()]
            actT = PA.get([128, 32, 256], BF16); Ract = Res()
            ysb = PA.get([128, 1024], F32); Rysb = Res()
            junk = PA.get([128, 512], BF16); Rjunk = Res()
            xdv = xview(xdst)
            NB2 = S_ // 256

            def prologue(tb):
                q = tb % 2
                for j in range(2):
                    J = 2 * tb + j
                    S.dma("sp", xp, xmv[:, J, :], writes=[Rxp])
                    act(xs2[q][:, j, :], xp, AF.Square, [Rxp], [Rxs2[q], Rs1], accum_out=small[:, j:j + 1])
                    rsqrt(small[:, 4 + j:5 + j], small[:, j:j + 1], 1.0 / D, [Rs1], [Rs1])
                    ts("dve", xs2[q][:, j, :], xp, small[:, 4 + j:5 + j], None, ALU.mult, None, [Rxp, Rs1], [Rxs2[q]])
                for k in range(8):
                    pb, Rpb = pMM.get()
                    pbv = pb[:].bitcast(BF16)
                    for j in range(2):
                        tr(pbv[:, j * 128:(j + 1) * 128], xs2[q][:, j, k * 128:(k + 1) * 128], ident[:],
                           [Rxs2[q], Rc], [Rpb])
                    if k % 2 == 0:
                        act(hT2[q][:, k, :], pbv[:, 0:256], AF.Identity, [Rpb, Rmod], [RhT2[q]],
                            scale=A2[:, k:k + 1], bias=sh2[:, k:k + 1])
                    else:
                        ts("dve", hT2[q][:, k, :], pbv[:, 0:256], A2[:, k:k + 1], sh2[:, k:k + 1], ALU.mult, ALU.add,
                           [Rpb, Rmod], [RhT2[q]])

            def up(tb):
                q = tb % 2
                for j in range(2):
                    S.dma("sp", xe[j], xmv[:, 2 * tb + j, :], writes=[Rxe[j]])
                for hc in range(0, 32, 2):
                    pb, Rpb = pMM.get()
                    for u in range(2):
                        for k in range(8):
                            mm(pb[:, u * 256:(u + 1) * 256], w_up[:, k, (hc + u) * 128:(hc + u + 1) * 128],
                               hT2[q][:, k, :], k == 0, k == 7, [RW2u[k], RhT2[q]], [Rpb])
                    av = actT[:, hc:hc + 2, :].rearrange("p a b -> p (a b)")
                    act(av, pb[:], AF.Relu, [Rpb], [Ract])
                    tt("pool" if (hc // 2) % 2 else "dve", av, av, av, ALU.mult, [Ract], [Ract])

            def down(tb):
                for j in range(2):
                    J = 2 * tb + j
                    pys = []
                    for half in range(2):
                        py, Rpy = pAUX.get() if half else pACC.get()
                        for k in range(32):
                            mm(py[:], actT[:, k, j * 128:(j + 1) * 128], w_dn[:, k, half * 512:(half + 1) * 512],
                               k == 0, k == 31, [Ract, RW2d[k // 4]], [Rpy])
                        act(junk, py[:], AF.Square, [Rpy], [Rjunk, Rs7], accum_out=small[:, 56 + half:57 + half])
                        pys.append((py, Rpy))
                    tt("dve", small[:, 58:59], small[:, 56:57], small[:, 57:58], ALU.add, [Rs7], [Rs7])
                    rsqrt(small[:, 58:59], small[:, 58:59], 1.0 / D, [Rs7], [Rs7])
                    for half in range(2):
                        py, Rpy = pys[half]
                        hs = slice(half * 512, (half + 1) * 512)
                        stt("dve", ysb[:, hs], py[:], small[:, 58:59], G2[:, hs], ALU.mult, ALU.mult,
                            [Rpy, Rs7, RG], [Rysb])
                        tt("pool", xe[j][:, hs], xe[j][:, hs], ysb[:, hs], ALU.add, [Rxe[j], Rysb], [Rxe[j]])
                    S.dma("sp", xdv[:, J, :], xe[j], reads=[Rxe[j]])

            prologue(0)
            for tb in range(NB2):
                up(tb)
                if tb + 1 < NB2:
                    prologue(tb + 1)
                down(tb)
            S.barrier()
            xsrc = out_d
        S.barrier()
        S.emit()
    return nc


def prep_shared(inp):
    f = lambda a: np.ascontiguousarray(np.asarray(a, dtype=np.float32))
    perm = _perm()
    sh = {}
    sh["w_in"] = f(np.asarray(inp["w_in"])[:, :, perm])
    sh["w_o"] = f(inp["w_o"])
    sh["w_up"] = f(inp["w_up"])
    sh["w_down"] = f(inp["w_down"])
    sh["w_mod"] = f(inp["w_mod"])
    cw = np.asarray(inp["conv_w"])
    sh["convw"] = f(cw.reshape(DEPTH, 3, 2, 128).transpose(0, 3, 2, 1))
    sh["cmp_w1"] = f(inp["cmp_w1"])
    sh["cmp_w2"] = f(inp["cmp_w2"])
    cpz = np.asarray(inp["cmp_pos"])
    sh["cmp_posT"] = f(cpz.transpose(0, 1, 3, 2).reshape(DEPTH, 128, 32))
    sh["gw2b"] = f(np.concatenate([np.asarray(inp["gla_gate_w2"]), np.asarray(inp["gla_gate_b"])[:, None, :]], axis=1))
    sh["gmlp_wsT"] = f(np.asarray(inp["gmlp_ws"]).transpose(0, 3, 1, 2))
    sh["gmlp_bT"] = f(np.asarray(inp["gmlp_b"]).transpose(0, 2, 1))
    sh["gaincol"] = f(np.asarray(inp["grp_gain"]).reshape(DEPTH, 8, 128).transpose(0, 2, 1))
    ng = np.asarray(inp["norm_g"])
    sh["normg_col"] = f(ng.reshape(DEPTH, 4, 8, 128).transpose(0, 3, 1, 2))
    sh["normg_row"] = f(ng)
    bm = np.asarray(inp["b_mod"])
    sh["bmod_col"] = f(bm.reshape(DEPTH, 48, 128).transpose(0, 2, 1))
    sh["bmod_row"] = f(bm)
    for k, v in _consts().items():
        sh["c_" + k] = f(v)
    return sh


def core_inputs(inp, b, sh):
    d = dict(sh)
    d["x"] = np.ascontiguousarray(np.asarray(inp["x"][b], dtype=np.float32))
    d["ccol"] = np.ascontiguousarray(np.asarray(inp["c"][b], dtype=np.float32).reshape(8, 128).T)
    d["pos"] = np.ascontiguousarray(np.asarray(inp["positions"][b], dtype=np.int32).reshape(1, S_))
    return d


def kernel(**inputs):
    sh = prep_shared(inputs)
    nc = build()
    in_maps = [core_inputs(inputs, b, sh) for b in range(8)]
    res = run_bass_kernel_spmd(nc, in_maps, core_ids=list(range(8)))
    return np.stack([np.asarray(r["out"], dtype=np.float32) for r in res.results], axis=0)
```
